# Optimizing a Trainium2 kernel written in Bass

```python
import jax, jax.numpy as jnp
from jax import lax
import numpy as np

D_MODEL = 1024
BATCH = 8
SEQ = 2048
DEPTH = 2

FNET_GROUPS = 4
FNET_GROUP_DIM = 128
FNET_WIDTH = FNET_GROUPS * FNET_GROUP_DIM
ATTN_HEADS = 8
ATTN_KV_HEADS = 2
HEAD_DIM = 64
ATTN_WIDTH = ATTN_HEADS * HEAD_DIM
KV_WIDTH = ATTN_KV_HEADS * HEAD_DIM
WINDOW = 128
BLOCK = 128
REL_BUCKETS = 32
REL_MAX_DIST = 128
MIX_IN_WIDTH = FNET_WIDTH + ATTN_WIDTH + 2 * KV_WIDTH
MIX_OUT_WIDTH = FNET_WIDTH + ATTN_WIDTH
D_FF = 2816
D_INNER = 2 * D_MODEL
SSM_HEAD_DIM = 64
SSM_HEADS = D_INNER // SSM_HEAD_DIM
SSM_GROUPS = 4
D_STATE = 128
CONV_WIDTH = 5
SSD_CHUNK = 128
CONV_DIM = D_INNER + 2 * SSM_GROUPS * D_STATE
SSM_IN_WIDTH = D_INNER + CONV_DIM + 2 * SSM_HEADS
N_EXPERTS = 8
TOP_K = 2
D_FF_EXPERT = 3584
N_EVEN = (DEPTH + 1) // 2
N_ODD = DEPTH // 2
EPS = 1e-6

kernel_name = "hybrid_fnet_swa_ssd_moe_adaln"


def rms_norm(x, w):
    xf = x.astype(jnp.float32)
    y = xf * lax.rsqrt(jnp.mean(xf * xf, axis=-1, keepdims=True) + EPS)
    return (y * w.astype(jnp.float32)).astype(x.dtype)


def modulate(x, w, shift, scale):
    return rms_norm(x, w) * (1 + scale[:, None, :]) + shift[:, None, :]


def swiglu(t, w1, w3, w2):
    return (jax.nn.silu(t @ w1) * (t @ w3)) @ w2


def t5_band_buckets():
    i = np.arange(BLOCK)[:, None]
    j = np.arange(3 * BLOCK)[None, :]
    rel = (j - BLOCK) - i
    half = REL_BUCKETS // 2
    max_exact = half // 2
    n = np.abs(rel)
    large = max_exact + (np.log(np.maximum(n, 1) / max_exact)
                         / np.log(REL_MAX_DIST / max_exact) * (half - max_exact)).astype(np.int32)
    large = np.minimum(large, half - 1)
    bucket = (rel > 0).astype(np.int32) * half + np.where(n < max_exact, n, large)
    return bucket.astype(np.int32), rel


def band_mask(n_blk):
    _, rel = t5_band_buckets()
    kpos = (np.arange(n_blk)[:, None, None] * BLOCK
            + np.arange(3 * BLOCK)[None, None, :] - BLOCK)
    return (np.abs(rel)[None] <= WINDOW) & (kpos >= 0) & (kpos < n_blk * BLOCK)


def fourier_mix(u):
    b, s, _ = u.shape
    ug = u.astype(jnp.float32).reshape(b, s, FNET_GROUPS, FNET_GROUP_DIM)
    y = jnp.fft.fft2(ug, axes=(1, 3), norm="ortho").real
    return y.reshape(b, s, FNET_WIDTH).astype(u.dtype)


def window_gqa(q, k, v, rel_bias, sink):
    b, s = q.shape[0], q.shape[1]
    nb = s // BLOCK
    g = ATTN_HEADS // ATTN_KV_HEADS
    qb = q.reshape(b, nb, BLOCK, ATTN_KV_HEADS, g, HEAD_DIM)

    def band(t):
        tp = jnp.pad(t, ((0, 0), (BLOCK, BLOCK), (0, 0), (0, 0)))
        tp = tp.reshape(b, nb + 2, BLOCK, ATTN_KV_HEADS, HEAD_DIM)
        return jnp.concatenate([tp[:, :-2], tp[:, 1:-1], tp[:, 2:]], axis=2)

    kw, vw = band(k), band(v)
    bucket, _ = t5_band_buckets()
    bias = jnp.transpose(rel_bias[bucket].astype(jnp.float32), (2, 0, 1))
    bias = bias.reshape(ATTN_KV_HEADS, g, BLOCK, 3 * BLOCK)
    logits = jnp.einsum('bnqkgd,bnskd->bnkgqs', qb, kw).astype(jnp.float32) * (HEAD_DIM ** -0.5) + bias
    logits = jnp.where(band_mask(nb)[None, :, None, None], logits, -jnp.inf)
    sk = sink.astype(jnp.float32).reshape(ATTN_KV_HEADS, g)[None, None, :, :, None, None]
    m = jnp.maximum(jnp.max(logits, axis=-1, keepdims=True), sk)
    p = jnp.exp(logits - m)
    probs = p / (jnp.sum(p, axis=-1, keepdims=True) + jnp.exp(sk - m))
    out = jnp.einsum('bnkgqs,bnskd->bnqkgd', probs.astype(v.dtype), vw)
    return out.reshape(b, s, ATTN_WIDTH)


def even_mixer(h, in_w, q_norm_w, k_norm_w, sink, out_w, rel_bias):
    b, s, _ = h.shape
    proj = h @ in_w
    u, q, k, v = jnp.split(proj, [FNET_WIDTH, FNET_WIDTH + ATTN_WIDTH,
                                  FNET_WIDTH + ATTN_WIDTH + KV_WIDTH], axis=-1)
    q = rms_norm(q.reshape(b, s, ATTN_HEADS, HEAD_DIM), q_norm_w)
    k = rms_norm(k.reshape(b, s, ATTN_KV_HEADS, HEAD_DIM), k_norm_w)
    v = v.reshape(b, s, ATTN_KV_HEADS, HEAD_DIM)
    y = jnp.concatenate([fourier_mix(u), window_gqa(q, k, v, rel_bias, sink)], axis=-1)
    return y @ out_w


def ssd_scan(x, dt, A, Bm, Cm):
    b, s, h, p = x.shape
    nc = s // SSD_CHUNK
    r = SSM_HEADS // SSM_GROUPS
    X = (x * dt[..., None]).reshape(b, nc, SSD_CHUNK, SSM_GROUPS, r, p)
    a = (A * dt).reshape(b, nc, SSD_CHUNK, SSM_GROUPS, r).transpose(0, 3, 4, 1, 2)
    Bc = Bm.reshape(b, nc, SSD_CHUNK, SSM_GROUPS, D_STATE)
    Cc = Cm.reshape(b, nc, SSD_CHUNK, SSM_GROUPS, D_STATE)
    a_cum = jnp.cumsum(a, axis=-1)
    tril = np.tril(np.ones((SSD_CHUNK, SSD_CHUNK), dtype=bool))
    Lmat = jnp.exp(jnp.where(tril, a_cum[..., :, None] - a_cum[..., None, :], -jnp.inf))
    CB = jnp.einsum('bclgn,bcsgn->bgcls', Cc, Bc)
    y_diag = jnp.einsum('bgrcls,bcsgrp->bclgrp', CB[:, :, None] * Lmat, X)
    decay_states = jnp.exp(a_cum[..., -1:] - a_cum)
    states = jnp.einsum('bclgn,bgrcl,bclgrp->cbgrpn', Bc, decay_states, X)
    chunk_decay = jnp.exp(a_cum[..., -1]).transpose(3, 0, 1, 2)

    def step(hc, inp):
        st, dec = inp
        return hc * dec[..., None, None] + st, hc

    h0 = jnp.zeros(states.shape[1:], jnp.float32)
    _, prev = lax.scan(step, h0, (states, chunk_decay))
    y_off = jnp.einsum('bclgn,cbgrpn,bgrcl->bclgrp', Cc, prev, jnp.exp(a_cum))
    return (y_diag + y_off).reshape(b, s, h, p)


def ssd_mixer(h, in_w, conv_w, conv_b, dt_bias_f, dt_bias_b, A_log_f, A_log_b, D, gnorm_w, out_w):
    b, s, _ = h.shape
    proj = h @ in_w
    z, xbc, dt = jnp.split(proj, [D_INNER, D_INNER + CONV_DIM], axis=-1)
    xbc = lax.conv_general_dilated(xbc, conv_w[:, None, :], (1,),
                                   ((CONV_WIDTH // 2, CONV_WIDTH // 2),),
                                   dimension_numbers=('NWC', 'WIO', 'NWC'),
                                   feature_group_count=CONV_DIM)
    xbc = jax.nn.silu(xbc + conv_b)
    xs, Bm, Cm = jnp.split(xbc, [D_INNER, D_INNER + SSM_GROUPS * D_STATE], axis=-1)
    xs = xs.reshape(b, s, SSM_HEADS, SSM_HEAD_DIM).astype(jnp.float32)
    Bm = Bm.reshape(b, s, SSM_GROUPS, D_STATE).astype(jnp.float32)
    Cm = Cm.reshape(b, s, SSM_GROUPS, D_STATE).astype(jnp.float32)
    dt = dt.astype(jnp.float32)
    dt_f = jax.nn.softplus(dt[..., :SSM_HEADS] + dt_bias_f.astype(jnp.float32))
    dt_b = jax.nn.softplus(dt[..., SSM_HEADS:] + dt_bias_b.astype(jnp.float32))
    A_f = -jnp.exp(A_log_f.astype(jnp.float32))
    A_b = -jnp.exp(A_log_b.astype(jnp.float32))
    flip = lambda t: jnp.flip(t, axis=1)
    y_f = ssd_scan(xs, dt_f, A_f, Bm, Cm)
    y_b = flip(ssd_scan(flip(xs), flip(dt_b), A_b, flip(Bm), flip(Cm)))
    y = (y_f + y_b + D.astype(jnp.float32)[:, None] * xs).reshape(b, s, D_INNER)
    y = rms_norm(y * jax.nn.silu(z.astype(jnp.float32)), gnorm_w).astype(h.dtype)
    return y @ out_w


def moe_swiglu(h, router_w, router_b, w1, w3, w2):
    b, s, d = h.shape
    t = h.reshape(b * s, d)
    logits = (t @ router_w).astype(jnp.float32) + router_b.astype(jnp.float32)
    top_v, top_i = lax.top_k(logits, TOP_K)
    top_w = jax.nn.softmax(top_v, axis=-1)
    gates = jnp.sum(jax.nn.one_hot(top_i, N_EXPERTS, dtype=jnp.float32) * top_w[..., None], axis=1)
    out = jnp.zeros((b * s, d), jnp.float32)
    for e in range(N_EXPERTS):
        out = out + gates[:, e:e + 1] * swiglu(t, w1[e], w3[e], w2[e]).astype(jnp.float32)
    return out.reshape(b, s, d).astype(h.dtype)


def setup_inputs(seed: int = 0) -> dict:
    key = jax.random.key(seed)
    ks = iter(jax.random.split(key, 64))
    f32 = jnp.float32

    def nrm(shape, scale):
        return jax.random.normal(next(ks), shape, f32) * scale

    def gain(shape):
        return 1.0 + nrm(shape, 0.05)

    def dt_bias(shape):
        u = jax.random.uniform(next(ks), shape, f32, np.log(1e-3), np.log(1e-1))
        dt = jnp.exp(u)
        return dt + jnp.log(-jnp.expm1(-dt))

    def a_log(shape):
        return jnp.log(jax.random.uniform(next(ks), shape, f32, 1.0, 16.0))

    d = D_MODEL
    return {
        "x": nrm((BATCH, SEQ, d), 1.0),
        "c": nrm((BATCH, d), 1.0),
        "rel_bias": nrm((REL_BUCKETS, ATTN_HEADS), 0.5),
        "ev_ada_w": nrm((N_EVEN, d, 6 * d), 0.5 * d ** -0.5),
        "ev_ada_b": nrm((N_EVEN, 6 * d), 0.02),
        "ev_norm1_w": gain((N_EVEN, d)),
        "ev_in_w": nrm((N_EVEN, d, MIX_IN_WIDTH), d ** -0.5),
        "ev_q_norm_w": gain((N_EVEN, HEAD_DIM)),
        "ev_k_norm_w": gain((N_EVEN, HEAD_DIM)),
        "ev_sink": nrm((N_EVEN, ATTN_HEADS), 0.5),
        "ev_out_w": nrm((N_EVEN, MIX_OUT_WIDTH, d), MIX_OUT_WIDTH ** -0.5),
        "ev_norm2_w": gain((N_EVEN, d)),
        "ev_ffn_w1": nrm((N_EVEN, d, D_FF), d ** -0.5),
        "ev_ffn_w3": nrm((N_EVEN, d, D_FF), d ** -0.5),
        "ev_ffn_w2": nrm((N_EVEN, D_FF, d), D_FF ** -0.5),
        "od_ada_w": nrm((N_ODD, d, 6 * d), 0.5 * d ** -0.5),
        "od_ada_b": nrm((N_ODD, 6 * d), 0.02),
        "od_norm1_w": gain((N_ODD, d)),
        "od_in_w": nrm((N_ODD, d, SSM_IN_WIDTH), d ** -0.5),
        "od_conv_w": nrm((N_ODD, CONV_WIDTH, CONV_DIM), CONV_WIDTH ** -0.5),
        "od_conv_b": nrm((N_ODD, CONV_DIM), 0.02),
        "od_dt_bias_f": dt_bias((N_ODD, SSM_HEADS)),
        "od_dt_bias_b": dt_bias((N_ODD, SSM_HEADS)),
        "od_A_log_f": a_log((N_ODD, SSM_HEADS)),
        "od_A_log_b": a_log((N_ODD, SSM_HEADS)),
        "od_D": gain((N_ODD, SSM_HEADS)),
        "od_gnorm_w": gain((N_ODD, D_INNER)),
        "od_out_w": nrm((N_ODD, D_INNER, d), D_INNER ** -0.5),
        "od_norm2_w": gain((N_ODD, d)),
        "od_router_w": nrm((N_ODD, d, N_EXPERTS), d ** -0.5),
        "od_router_b": nrm((N_ODD, N_EXPERTS), 0.01),
        "od_moe_w1": nrm((N_ODD, N_EXPERTS, d, D_FF_EXPERT), d ** -0.5),
        "od_moe_w3": nrm((N_ODD, N_EXPERTS, d, D_FF_EXPERT), d ** -0.5),
        "od_moe_w2": nrm((N_ODD, N_EXPERTS, D_FF_EXPERT, d), D_FF_EXPERT ** -0.5),
    }


def reference(x, c, rel_bias,
              ev_ada_w, ev_ada_b, ev_norm1_w, ev_in_w, ev_q_norm_w, ev_k_norm_w, ev_sink,
              ev_out_w, ev_norm2_w, ev_ffn_w1, ev_ffn_w3, ev_ffn_w2,
              od_ada_w, od_ada_b, od_norm1_w, od_in_w, od_conv_w, od_conv_b,
              od_dt_bias_f, od_dt_bias_b, od_A_log_f, od_A_log_b, od_D, od_gnorm_w, od_out_w,
              od_norm2_w, od_router_w, od_router_b, od_moe_w1, od_moe_w3, od_moe_w2):
    cs = jax.nn.silu(c)
    for i in range(DEPTH):
        j = i // 2
        if i % 2 == 0:
            mod = cs @ ev_ada_w[j] + ev_ada_b[j]
            sh1, sc1, g1, sh2, sc2, g2 = jnp.split(mod, 6, axis=-1)
            hm = even_mixer(modulate(x, ev_norm1_w[j], sh1, sc1), ev_in_w[j], ev_q_norm_w[j],
                            ev_k_norm_w[j], ev_sink[j], ev_out_w[j], rel_bias)
            x = x + g1[:, None, :] * hm
            hf = swiglu(modulate(x, ev_norm2_w[j], sh2, sc2), ev_ffn_w1[j], ev_ffn_w3[j], ev_ffn_w2[j])
            x = x + g2[:, None, :] * hf
        else:
            mod = cs @ od_ada_w[j] + od_ada_b[j]
            sh1, sc1, g1, sh2, sc2, g2 = jnp.split(mod, 6, axis=-1)
            hm = ssd_mixer(modulate(x, od_norm1_w[j], sh1, sc1), od_in_w[j], od_conv_w[j], od_conv_b[j],
                           od_dt_bias_f[j], od_dt_bias_b[j], od_A_log_f[j], od_A_log_b[j], od_D[j],
                           od_gnorm_w[j], od_out_w[j])
            x = x + g1[:, None, :] * hm
            hf = moe_swiglu(modulate(x, od_norm2_w[j], sh2, sc2), od_router_w[j], od_router_b[j],
                            od_moe_w1[j], od_moe_w3[j], od_moe_w2[j])
            x = x + g2[:, None, :] * hf
    return x
```

```python
import numpy as np
import ml_dtypes
from contextlib import ExitStack
import concourse.bass as bass
import concourse.mybir as mybir
from concourse.bass_utils import run_bass_kernel_spmd

F32 = mybir.dt.float32
BF16 = mybir.dt.bfloat16
ALU = mybir.AluOpType
AF = mybir.ActivationFunctionType
ENGS = ("pe", "act", "dve", "pool", "sp")
S_LEN = 2048
D = 1024
EPS = 1e-6
NEG = -30000.0


class Dep:
    __slots__ = ("w", "r", "dsem", "dcnt", "wq")

    def __init__(self):
        self.wq = None
        self.w = None
        self.r = []
        self.dsem = None
        self.dcnt = 0


def deps(n):
    return [Dep() for _ in range(n)]


class Sched:
    def __init__(self, nc, stack):
        self.nc = nc
        self.stack = stack
        self.q = {e: [] for e in ENGS}
        self.cnt = {e: 0 for e in ENGS}
        self.sem = {e: stack.enter_context(nc.semaphore("s_" + e)) for e in ENGS}
        self.known = {e: {} for e in ENGS}
        self.same_wait = {"pe": False, "act": True, "dve": True, "pool": True, "sp": False}
        self.nsem = 0
        self.dma_deps = []
        self.dead = False

    def _waits(self, eng, reads, writes):
        evs = []
        for d in reads:
            if d.w is not None:
                evs.append(d.w)
        for d in writes:
            if d.w is not None:
                evs.append(d.w)
            evs.extend(d.r)
        waits = {}
        kn = self.known[eng]
        for (sem, val, e) in evs:
            if e == eng and not self.same_wait[eng]:
                continue
            if kn.get(id(sem), 0) >= val:
                continue
            cur = waits.get(id(sem))
            if cur is None or cur[1] < val:
                waits[id(sem)] = (sem, val)
        for k, (sem, val) in waits.items():
            kn[k] = val
        return list(waits.values())

    def op(self, eng, fn, reads=(), writes=()):
        if self.dead:
            return
        waits = self._waits(eng, reads, writes)
        self.cnt[eng] += 1
        sem = self.sem[eng]
        ev = (sem, self.cnt[eng], eng)

        def emit(e, waits=waits, fn=fn, sem=sem):
            for (s, v) in waits:
                e.wait_ge(s, v)
            fn(e).then_inc(sem, 1)

        self.q[eng].append(emit)
        for d in writes:
            d.w = ev
            d.r = []
        for d in reads:
            if d not in writes:
                d.r.append(ev)

    def dma(self, eng, out_ap, in_ap, reads=(), writes=(), **kw):
        if self.dead:
            return
        dst = writes[0]
        if dst.w is not None and dst.w[2] is None and dst.w[0] is dst.dsem and not dst.r and dst.wq == eng:
            waits = self._waits(eng, reads, ())
        else:
            waits = self._waits(eng, reads, writes)
        if dst.dsem is None:
            dst.dsem = self.stack.enter_context(self.nc.semaphore("d%d" % self.nsem))
            self.nsem += 1
            self.dma_deps.append(dst)
        dst.dcnt += 16
        dst.wq = eng
        ev = (dst.dsem, dst.dcnt, None)
        dsem = dst.dsem

        def emit(e, waits=waits, dsem=dsem, out_ap=out_ap, in_ap=in_ap, kw=kw):
            for (s, v) in waits:
                e.wait_ge(s, v)
            e.dma_start(out=out_ap, in_=in_ap, **kw).then_inc(dsem, 16)

        self.q[eng].append(emit)
        for d in writes:
            d.w = ev
            d.r = []
        for d in reads:
            d.r.append(ev)

    def barrier(self):
        if self.dead:
            return
        evs = [(self.sem[e], self.cnt[e]) for e in ENGS if self.cnt[e] > 0]
        evs += [(d.dsem, d.dcnt) for d in self.dma_deps]
        for eng in ENGS:
            kn = self.known[eng]
            waits = []
            for (sem, val) in evs:
                if sem is self.sem[eng]:
                    continue
                if kn.get(id(sem), 0) >= val:
                    continue
                kn[id(sem)] = val
                waits.append((sem, val))

            def emit(e, waits=waits):
                for (s, v) in waits:
                    e.wait_ge(s, v)

            self.q[eng].append(emit)

    def emit_all(self, block):
        m = {"pe": block.tensor, "act": block.scalar, "dve": block.vector, "pool": block.gpsimd, "sp": block.sync}
        for eng in ENGS:
            lst = self.q[eng]

            def body(e, lst=lst):
                for f in lst:
                    f(e)

            m[eng](body)

    def mm(self, out, lhsT, rhs, start, stop, r, w):
        self.op("pe", lambda e: e.matmul(out, lhsT=lhsT, rhs=rhs, start=start, stop=stop), r, w)

    def tr(self, out, in_, ident, r, w):
        self.op("pe", lambda e: e.transpose(out, in_, ident), r, w)

    def act(self, out, in_, func, r, w, bias=None, scale=None):
        kw = {}
        if bias is not None:
            kw["bias"] = bias
        if scale is not None:
            kw["scale"] = scale
        self.op("act", lambda e: e.activation(out=out, in_=in_, func=func, **kw), r, w)

    def copy(self, eng, out, in_, r, w):
        if eng == "act":
            self.op("act", lambda e: e.copy(out=out, in_=in_), r, w)
        else:
            self.op(eng, lambda e: e.tensor_copy(out=out, in_=in_), r, w)

    def tt(self, eng, out, in0, in1, op, r, w):
        self.op(eng, lambda e: e.tensor_tensor(out=out, in0=in0, in1=in1, op=op), r, w)

    def ts(self, eng, out, in0, s1, s2, op0, op1, r, w):
        if s2 is None:
            self.op(eng, lambda e: e.tensor_scalar(out=out, in0=in0, scalar1=s1, scalar2=None, op0=op0), r, w)
        else:
            self.op(eng, lambda e: e.tensor_scalar(out=out, in0=in0, scalar1=s1, scalar2=s2, op0=op0, op1=op1), r, w)

    def stt(self, eng, out, in0, scalar, in1, op0, op1, r, w):
        self.op(eng, lambda e: e.scalar_tensor_tensor(out=out, in0=in0, scalar=scalar, in1=in1, op0=op0, op1=op1), r, w)

    def memset(self, eng, ap, val, w):
        self.op(eng, lambda e: e.memset(ap, val), (), w)

    def recip(self, out, in_, r, w):
        self.op("dve", lambda e: e.reciprocal(out=out, in_=in_), r, w)


class _Stop(Exception):
    pass


STAGE = 99


def stage(S, k):
    if STAGE == k or (STAGE == 221 and k == 22):
        S.barrier()
        S.dead = True


class Ring:
    def __init__(self, items):
        self.items = items
        self.i = 0

    def next(self):
        it = self.items[self.i % len(self.items)]
        self.i += 1
        return it


def _bf(a):
    return np.ascontiguousarray(a.astype(np.float32)).astype(ml_dtypes.bfloat16)


def host_consts():
    c = {}
    c["identf"] = np.eye(128, dtype=np.float32)
    c["identb"] = _bf(np.eye(128))
    c["onesb"] = _bf(np.ones((128, 128)))
    c["onesf"] = np.ones((128, 128), np.float32)
    bd = np.zeros((128, 128), np.float32)
    bd[:64, :64] = 1
    bd[64:, 64:] = 1
    c["bd64"] = _bf(bd)
    k = np.arange(128)
    ang = 2 * np.pi * np.outer(k, k) / 128.0
    c["ccsc"] = _bf(np.concatenate([np.cos(ang), -np.sin(ang)], 1) / np.sqrt(128.0))
    n = np.arange(S_LEN)
    jk = np.outer(n, n) % S_LEN
    ang = 2 * np.pi * jk / float(S_LEN)
    c["cs_mat"] = _bf(np.cos(ang) / np.sqrt(float(S_LEN)))
    c["ss_mat"] = _bf(np.sin(ang) / np.sqrt(float(S_LEN)))
    i = np.arange(512)
    rel = 255 - i
    half, max_exact = 16, 8
    na = np.abs(rel)
    large = max_exact + (np.log(np.maximum(na, 1) / max_exact) / np.log(128 / max_exact) * (half - max_exact)).astype(np.int32)
    large = np.minimum(large, half - 1)
    bucket = (rel > 0).astype(np.int32) * half + np.where(na < max_exact, na, large)
    oh = np.zeros((32, 512), np.float32)
    oh[bucket, i] = 1.0
    oh[:, 511] = 0.0
    c["ohr"] = oh
    p = np.arange(128)[:, None]
    cc = np.arange(384)[None, :]
    relm = 128 + p - cc
    c["amask"] = np.where(np.abs(relm) <= 128, 0.0, NEG).astype(np.float32)
    s = np.arange(128)[:, None]
    l = np.arange(128)[None, :]
    c["trif"] = (s <= l).astype(np.float32)
    c["trib"] = (s >= l).astype(np.float32)
    c["maskf"] = _bf((l >= s).astype(np.float32))
    c["maskb"] = _bf((l <= s).astype(np.float32))
    c["negf"] = np.where(l >= s, 0.0, NEG).astype(np.float32)
    c["negb"] = np.where(l <= s, 0.0, NEG).astype(np.float32)
    sl = np.zeros((128, 128), np.float32)
    sl[127, :] = 1
    c["sellast"] = sl
    sf = np.zeros((128, 128), np.float32)
    sf[0, :] = 1
    c["selfirst"] = sf
    ee = np.zeros((8, 8, 128), np.float32)
    for e in range(8):
        ee[e, e, :] = 1
    c["esel"] = ee.reshape(8, 1024)
    return c


CONST_SHAPES = {
    "identf": ([128, 128], F32), "identb": ([128, 128], BF16), "onesb": ([128, 128], BF16), "onesf": ([128, 128], F32),
    "bd64": ([128, 128], BF16), "ccsc": ([128, 256], BF16), "cs_mat": ([2048, 2048], BF16), "ss_mat": ([2048, 2048], BF16),
    "ohr": ([32, 512], F32), "amask": ([128, 384], F32), "trif": ([128, 128], F32), "trib": ([128, 128], F32),
    "maskf": ([128, 128], BF16), "maskb": ([128, 128], BF16), "sellast": ([128, 128], F32), "selfirst": ([128, 128], F32),
    "esel": ([8, 1024], F32), "negf": ([128, 128], F32), "negb": ([128, 128], F32),
}

PARAM_SHAPES = {
    "x": [2048, 1024], "c": [8, 128], "rel_bias": [32, 8],
    "ev_ada_w": [1024, 6144], "ev_ada_b": [1, 6144], "ev_norm1_w": [8, 128], "ev_in_w": [1024, 1280],
    "ev_q_norm_w": [64, 1], "ev_k_norm_w": [64, 1], "ev_sink": [1, 8], "ev_out_w": [1024, 1024], "ev_norm2_w": [8, 128],
    "ev_ffn_w1": [1024, 2816], "ev_ffn_w3": [1024, 2816], "ev_ffn_w2": [2816, 1024],
    "od_ada_w": [1024, 6144], "od_ada_b": [1, 6144], "od_norm1_w": [8, 128], "od_in_w": [1024, 5184],
    "od_conv_w": [120, 128], "od_conv_b": [24, 128], "od_dt_bias_f": [1, 32], "od_dt_bias_b": [1, 32],
    "od_A_log_f": [1, 32], "od_A_log_b": [1, 32], "od_D": [1, 32], "od_gnorm_w": [16, 128], "od_out_w": [2048, 1024],
    "od_norm2_w": [8, 128], "od_router_w": [1024, 8], "od_router_b": [1, 8],
    "od_moe_w1": [8, 1024, 3584], "od_moe_w3": [8, 1024, 3584], "od_moe_w2": [8, 3584, 1024],
}


def build(p0=0, p1=4, dump=False):
    nc = bass.Bass("TRN2", target_bir_lowering=False)
    class _Lazy(dict):
        def __missing__(self, k):
            if k in PARAM_SHAPES:
                v = nc.dram_tensor(k, list(PARAM_SHAPES[k]), F32, kind="ExternalInput").ap()
            else:
                v = nc.dram_tensor(k, list(CONST_SHAPES[k][0]), CONST_SHAPES[k][1], kind="ExternalInput").ap()
            self[k] = v
            return v

    P = _Lazy()
    C = P
    out_d = nc.dram_tensor("out", [2048, 1024], F32, kind="ExternalOutput").ap()
    dump_d = [nc.dram_tensor("dump%d" % i, [2048, 1024], F32, kind="ExternalOutput").ap() for i in range(4)] if dump else None
    tscr = nc.dram_tensor("tscr", [8, 128, 512], F32, kind="Internal").ap()
    ygscr = nc.dram_tensor("ygscr", [2048, 2048], BF16, kind="ExternalOutput" if dump else "Internal").ap()
    acscr = nc.dram_tensor("acscr", [64, 2048], F32, kind="Internal").ap()
    xscr = nc.dram_tensor("xscr", [128, 8, 2048], F32, kind="Internal").ap()

    with ExitStack() as st:
        S = Sched(nc, st)

        def sb(stack, name, shape, dt):
            return stack.enter_context(nc.sbuf_tensor("sb_" + name, shape, dt))

        xT = sb(st, "xT", [128, 8, S_LEN], F32)
        dxT = [deps(4) for _ in range(8)]
        hT = sb(st, "hT", [128, 8, S_LEN], BF16)
        dhT = [deps(4) for _ in range(8)]
        cst = {}
        dcst = {}
        for k in ("identf", "identb", "onesb", "onesf", "bd64"):
            cst[k] = sb(st, "c_" + k, CONST_SHAPES[k][0], CONST_SHAPES[k][1])
            dcst[k] = Dep()
        modcol = [sb(st, "modcol%d" % i, [128, 48], F32) for i in range(2)]
        dmod = [Dep(), Dep()]
        pcol = sb(st, "pcol", [128, 72], F32)
        dpcol = Dep()
        acol = sb(st, "acol", [128, 4, 8], F32)
        dacol = Dep()
        cscol = sb(st, "cscol", [128, 8], F32)
        dcs = Dep()
        epsc = sb(st, "epsc", [128, 1], F32)
        depsc = Dep()
        wring_t = [sb(st, "wring%d" % i, [128, 8, 512], BF16) for i in range(3)]
        wring = Ring([(wring_t[i], Dep()) for i in range(3)])
        psf = [st.enter_context(nc.psum_tensor("psf%d" % i, [128, 512], F32)) for i in range(7)]
        psb_t = st.enter_context(nc.psum_tensor("psb", [128, 1024], BF16))
        dpsb = Dep()
        PSM = Ring([(psf[i], Dep()) for i in range(4)])
        PSA = Ring([(psf[i], Dep()) for i in range(4, 7)])
        block = st.enter_context(nc.Block())
        evq = Ring(["act", "dve"])
        WQ = Ring(["pool"])
        lastw = [None]

        identf, identb, onesb, onesf, bd64 = (cst[k] for k in ("identf", "identb", "onesb", "onesf", "bd64"))

        for k in cst:
            S.dma("sp", cst[k][:], C[k][:, :], writes=[dcst[k]])
        S.memset("dve", epsc[:], EPS, [depsc])

        akc = [0]

        def make_adabufs(stack):
            return dict(adat=[sb(stack, "adat%d_%d" % (akc[0], i), [128, 8, 512], F32) for i in range(2)], dadat=deps(2),
                        modrow=[sb(stack, "modrow%d_%d" % (akc[0], i), [1, 512], F32) for i in range(2)], dmr=deps(2),
                        brow=[sb(stack, "brow%d_%d" % (akc[0], i), [1, 512], F32) for i in range(2)], dbrow=deps(2), k=[0], tag=akc.__setitem__(0, akc[0] + 1))

        def ada_block(layer, nb, B_):
            nm = ("ev", "od")[layer]
            k = B_["k"][0]
            B_["k"][0] += 1
            t, dt_ = B_["adat"][k % 2], B_["dadat"][k % 2]
            mr, dmr_ = B_["modrow"][k % 2], B_["dmr"][k % 2]
            br, dbr_ = B_["brow"][k % 2], B_["dbrow"][k % 2]
            S.dma("act", br[:], P[nm + "_ada_b"][:, nb * 512:(nb + 1) * 512], writes=[dbr_])
            S.dma("sp", t[:], P[nm + "_ada_w"].rearrange("(kc p) f -> p kc f", p=128)[:, :, nb * 512:(nb + 1) * 512], writes=[dt_])
            ps, dps = PSA.next()
            for kc in range(8):
                S.mm(ps[0:1, :], cscol[:, kc:kc + 1], t[:, kc, :], kc == 0, kc == 7, [dcs, dt_], [dps])
            S.tt("dve", mr[:], ps[0:1, :], br[:], ALU.add, [dps, dbr_], [dmr_])
            pc, dpc = PSA.next()
            for j4 in range(4):
                S.mm(pc[:, j4:j4 + 1], mr[0:1, j4 * 128:(j4 + 1) * 128], onesf[0:1, 0:1], True, True, [dmr_, dcst["onesf"]], [dpc])
            S.copy("dve", modcol[layer][:, nb * 4:(nb + 1) * 4], pc[:, 0:4], [dpc], [dmod[layer]])

        def ada_finish(layer):
            for sub in range(2):
                S.stt("dve", acol[:, 2 * layer + sub, :], modcol[layer][:, 8 + 24 * sub:16 + 24 * sub], 1.0,
                      pcol[:, 16 * layer + 8 * sub:16 * layer + 8 * sub + 8], ALU.add, ALU.mult, [dmod[layer], dpcol], [dacol])

        with ExitStack() as ph:
            xst = [sb(ph, "xst%d" % i, [128, 4, 1024], F32) for i in range(2)]
            dxst = [Dep(), Dep()]
            for tb in range(4):
                t, dt_ = xst[tb % 2], dxst[tb % 2]
                S.dma("sp", t[:], P["x"][tb * 512:(tb + 1) * 512, :].rearrange("(a p) f -> p a f", p=128), writes=[dt_])
                for fc in range(8):
                    ps, dps = PSM.next()
                    for a in range(4):
                        S.tr(ps[:, a * 128:(a + 1) * 128], t[:, a, fc * 128:(fc + 1) * 128], identf[:], [dt_, dcst["identf"]], [dps])
                    S.copy(evq.next(), xT[:, fc, tb * 512:(tb + 1) * 512], ps[:], [dps], [dxT[fc][tb]])

            S.barrier()
        with ExitStack() as ph:
            c8 = sb(ph, "c8", [8, 128], F32)
            dc8 = Dep()
            S.dma("sp", c8[:], P["c"][:, :], writes=[dc8])
            ps, dps = PSA.next()
            S.tr(ps[:, 0:8], c8[:], identf[0:8, 0:8], [dc8, dcst["identf"]], [dps])
            S.act(cscol[:], ps[:, 0:8], AF.Silu, [dps], [dcs])
            defer_od = (p0 <= 1 < p1)
            adabufs = make_adabufs(ph)
            for layer in range(2):
                if layer == 1 and defer_od:
                    continue
                for nb in range(12):
                    ada_block(layer, nb, adabufs)
            prow = sb(ph, "prow", [72, 128], F32)
            dprow = Dep()
            for r0, nm in ((0, "ev_norm1_w"), (8, "ev_norm2_w"), (16, "od_norm1_w"), (24, "od_norm2_w"), (32, "od_gnorm_w"), (48, "od_conv_b")):
                n = PARAM_SHAPES[nm][0]
                S.dma("sp", prow[r0:r0 + n, :], P[nm][:, :], writes=[dprow])
            ps, dps = PSA.next()
            S.tr(ps[:, 0:72], prow[:], identf[0:72, 0:72], [dprow, dcst["identf"]], [dps])
            S.copy("dve", pcol[:], ps[:, 0:72], [dps], [dpcol])
            for layer in range(2):
                if layer == 1 and defer_od:
                    continue
                ada_finish(layer)
            S.barrier()

        def modulate(ph, layer, sub, tag, fp32_out=None):
            sq = [sb(ph, "sq%s%d" % (tag, i), [128, 512], BF16) for i in range(3)]
            dsq = deps(3)
            rstd = sb(ph, "rstd" + tag, [128, S_LEN], F32)
            drstd = deps(4)
            tmp = [sb(ph, "mtmp%s%d" % (tag, i), [128, 512], F32) for i in range(2)]
            dtmp = [Dep(), Dep()]
            bcol0 = 0 + 24 * sub
            k = 0
            for tb in range(4):
                tsl = slice(tb * 512, (tb + 1) * 512)
                ps, dps = PSA.next()
                for fc in range(8):
                    i = k % 3
                    k += 1
                    S.act(sq[i][:], xT[:, fc, tsl], AF.Square, [dxT[fc][tb]], [dsq[i]])
                    S.mm(ps[:], onesb[:], sq[i][:], fc == 0, fc == 7, [dsq[i], dcst["onesb"]], [dps])
                S.act(rstd[:, tsl], ps[:], AF.Sqrt, [dps, depsc], [drstd[tb]], bias=epsc[:, 0:1], scale=1.0 / D)
                S.recip(rstd[:, tsl], rstd[:, tsl], [drstd[tb]], [drstd[tb]])
            k = 0
            for tb in range(4):
                tsl = slice(tb * 512, (tb + 1) * 512)
                for fc in range(8):
                    t, dt_ = tmp[k % 2], dtmp[k % 2]
                    k += 1
                    S.stt("dve", t[:], xT[:, fc, tsl], acol[:, 2 * layer + sub, fc:fc + 1], rstd[:, tsl], ALU.mult, ALU.mult,
                          [dxT[fc][tb], dacol, drstd[tb]], [dt_])
                    S.act(hT[:, fc, tsl], t[:], AF.Identity, [dt_, dmod[layer]], [dhT[fc][tb]],
                          bias=modcol[layer][:, bcol0 + fc:bcol0 + fc + 1], scale=1.0)
                    if fp32_out is not None:
                        fp32_out(fc, tb, t, dt_, modcol[layer][:, bcol0 + fc:bcol0 + fc + 1])

        def dbg(name, tile, shape, dt_, reads):
            if not dump:
                return
            d_ = nc.dram_tensor("dbg_" + name, list(shape), dt_, kind="ExternalOutput").ap()
            S.dma("sp", d_, tile[:], reads=reads, writes=[Dep()])

        def load_w(dram_ap_3d, kc_n, ncols):
            t, dt_ = wring.next()
            rd = [lastw[0]] if lastw[0] is not None and lastw[0] is not dt_ else []
            for kc in range(kc_n):
                S.dma(WQ.next(), t[:, kc, 0:ncols], dram_ap_3d[:, kc, :], reads=rd, writes=[dt_])
            lastw[0] = dt_
            return t, dt_

        def w_view(name, c0, c1):
            return P[name].rearrange("(kc p) f -> p kc f", p=128)[:, :, c0:c1]

        def swiglu(ph, w1v, w3v, w2v, F, upd, tag, gate=None, after_group=None):
            if getattr(ph, "_sw", None) is None:
                gT = sb(ph, "gT" + tag, [128, 4, S_LEN], BF16)
                dgT = [deps(4) for _ in range(4)]
                sl_ = [sb(ph, "sil%s%d" % (tag, i), [128, 512], BF16) for i in range(2)]
                dsl = [Dep(), Dep()]
                ph._sw = (gT, dgT, sl_, dsl)
            gT, dgT, sl_, dsl = ph._sw
            nfc_tot = F // 128
            fc0 = 0
            k = 0
            while fc0 < nfc_tot:
                nfc = min(4, nfc_tot - fc0)
                w1t, dw1 = load_w(w1v(fc0 * 128, (fc0 + nfc) * 128), 8, nfc * 128)
                w3t, dw3 = load_w(w3v(fc0 * 128, (fc0 + nfc) * 128), 8, nfc * 128)
                for j in range(nfc):
                    for tb in range(4):
                        tsl = slice(tb * 512, (tb + 1) * 512)
                        p1, dp1 = PSM.next()
                        p3, dp3 = PSM.next()
                        for kc in range(8):
                            S.mm(p1[:], w1t[:, kc, j * 128:(j + 1) * 128], hT[:, kc, tsl], kc == 0, kc == 7, [dw1, dhT[kc][tb]], [dp1])
                        for kc in range(8):
                            S.mm(p3[:], w3t[:, kc, j * 128:(j + 1) * 128], hT[:, kc, tsl], kc == 0, kc == 7, [dw3, dhT[kc][tb]], [dp3])
                        s_, ds_ = sl_[k % 2], dsl[k % 2]
                        k += 1
                        S.act(s_[:], p1[:], AF.Silu, [dp1], [ds_])
                        if gate is None:
                            S.tt("dve", gT[:, j, tsl], p3[:], s_[:], ALU.mult, [dp3, ds_], [dgT[j][tb]])
                        else:
                            gb_, dgb_ = gate(tb)
                            S.tt("dve", gT[:, j, tsl], p3[:], gb_, ALU.mult, [dp3, dgb_], [dgT[j][tb]])
                            S.tt("dve", gT[:, j, tsl], gT[:, j, tsl], s_[:], ALU.mult, [ds_, dgT[j][tb]], [dgT[j][tb]])
                w2t = []
                for half in range(2):
                    w2t.append(load_w(w2v(fc0, nfc)[:, :, half * 512:(half + 1) * 512], nfc, 512))
                for dc in range(8):
                    wt, dwt = w2t[dc // 4]
                    for tb in range(4):
                        tsl = slice(tb * 512, (tb + 1) * 512)
                        po, dpo = PSA.next()
                        for j in range(nfc):
                            S.mm(po[:], wt[:, j, (dc % 4) * 128:(dc % 4 + 1) * 128], gT[:, j, tsl], j == 0, j == nfc - 1, [dwt, dgT[j][tb]], [dpo])
                        upd(po, dpo, dc, tb)
                if after_group is not None:
                    after_group()
                fc0 += nfc

        def dump_x(idx):
            if not dump:
                return
            with ExitStack() as dph:
                ot = [sb(dph, "dot%d_%d" % (idx, i), [128, 4, 1024], F32) for i in range(2)]
                dot = [Dep(), Dep()]
                dd = Dep()
                for tb in range(4):
                    t, dt_ = ot[tb % 2], dot[tb % 2]
                    for a in range(4):
                        for fq in range(2):
                            ps, dps = PSM.next()
                            for f4 in range(4):
                                fc = fq * 4 + f4
                                S.tr(ps[:, f4 * 128:(f4 + 1) * 128], xT[:, fc, tb * 512 + a * 128: tb * 512 + (a + 1) * 128], identf[:],
                                     [dxT[fc][tb], dcst["identf"]], [dps])
                            S.copy(evq.next(), t[:, a, fq * 512:(fq + 1) * 512], ps[:], [dps], [dt_])
                    S.dma("sp", dump_d[idx][tb * 512:(tb + 1) * 512, :].rearrange("(a p) f -> p a f", p=128), t[:], reads=[dt_], writes=[dd])
                S.barrier()


        def moe_phase_inner():
            with ExitStack() as ph:
                rw = sb(ph, "rw", [128, 8, 8], F32)
                drw = Dep()
                S.dma("sp", rw[:], P["od_router_w"].rearrange("(kc p) e -> p kc e", p=128), writes=[drw])
                rbb = sb(ph, "m_rbb", [128, 8], F32)
                drbb = Dep()
                S.dma("sp", rbb[:], bass.AP(tensor=P["od_router_b"].tensor, offset=0, ap=[[0, 128], [1, 8]]), writes=[drbb])
                esel = sb(ph, "esel", [8, 8, 128], F32)
                desel = Dep()
                S.dma("sp", esel[:], P["esel"].rearrange("k (e m) -> k e m", e=8), writes=[desel])
                logit = sb(ph, "logit", [128, 16, 8], F32)
                dlogit = deps(4)
                gtok = sb(ph, "gtok", [128, 16, 8], F32)
                dgtok = deps(4)
                gT8 = sb(ph, "gT8", [8, S_LEN], F32)
                dgT8 = deps(4)
                with ExitStack() as ph2:
                    hfa = sb(ph2, "hfa", [128, 8, 512], F32)
                    dhfa = deps(8)

                    def fp32_out(fc, tb, t, dt_, bcol):
                        S.ts("dve", hfa[:, fc, :], t[:], bcol, None, ALU.add, None, [dt_, dmod[1]], [dhfa[fc]])
                        if fc == 7:
                            pl, dpl = PSM.next()
                            for tt in range(4):
                                for f2 in range(8):
                                    S.mm(pl[:, tt * 8:(tt + 1) * 8], hfa[:, f2, tt * 128:(tt + 1) * 128], rw[:, f2, :], f2 == 0, f2 == 7, [dhfa[f2], drw], [dpl])
                        if fc == 7:
                            S.tt("dve", logit[:, tb * 4:(tb + 1) * 4, :], pl[:, 0:32].rearrange("p (a b) -> p a b", b=8),
                                 rbb[:].unsqueeze(1).to_broadcast([128, 4, 8]), ALU.add, [dpl, drbb], [dlogit[tb]])

                    modulate(ph2, 1, 1, "m", fp32_out=fp32_out)
                    top8 = sb(ph2, "top8", [128, 8], F32)
                    nm1 = sb(ph2, "nm1", [128, 1], F32)
                    ex = sb(ph2, "ex", [128, 8], F32)
                    e2 = sb(ph2, "e2", [128, 1], F32)
                    msk = sb(ph2, "msk", [128, 8], F32)
                    dtp = Dep()
                    for tt in range(16):
                        lg = logit[:, tt, :]
                        dl = dlogit[tt // 4]
                        S.op("dve", lambda e, lg=lg: e.max(out=top8[:], in_=lg), [dl], [dtp])
                        S.ts("dve", nm1[:], top8[:, 0:1], -1.0, None, ALU.mult, None, [dtp], [dtp])
                        S.act(ex[:], lg, AF.Exp, [dl, dtp], [dtp], bias=nm1[:, 0:1], scale=1.0)
                        S.act(e2[:], top8[:, 1:2], AF.Exp, [dtp], [dtp], bias=nm1[:, 0:1], scale=1.0)
                        S.ts("dve", e2[:], e2[:], 1.0, None, ALU.add, None, [dtp], [dtp])
                        S.recip(e2[:], e2[:], [dtp], [dtp])
                        S.ts("dve", msk[:], lg, top8[:, 1:2], None, ALU.is_ge, None, [dl, dtp], [dtp])
                        S.stt("dve", gtok[:, tt, :], ex[:], e2[:, 0:1], msk[:], ALU.mult, ALU.mult, [dtp], [dgtok[tt // 4]])
                    for tb in range(4):
                        ps, dps = PSA.next()
                        for t4 in range(4):
                            S.tr(ps[0:8, t4 * 128:(t4 + 1) * 128], gtok[:, tb * 4 + t4, :], identf[:], [dgtok[tb], dcst["identf"]], [dps])
                        S.copy("dve", gT8[:, tb * 512:(tb + 1) * 512], ps[0:8, :], [dps], [dgT8[tb]])
                    S.barrier()
                gbc = [sb(ph, "gbc%d" % i, [128, 512], F32) for i in range(8)]
                dgbc = deps(8)
                utmp = [sb(ph, "utmp%d" % i, [128, 512], F32) for i in range(2)]
                dutmp = [Dep(), Dep()]
                ucnt = [0]
                for e in range(8):
                    base = (e % 2) * 4
                    for tb in range(4):
                        ps, dps = PSA.next()
                        S.mm(ps[:], esel[:, e, :], gT8[:, tb * 512:(tb + 1) * 512], True, True, [desel, dgT8[tb]], [dps])
                        S.copy("act", gbc[base + tb][:], ps[:], [dps], [dgbc[base + tb]])

                    def upd(po, dpo, dc, tb, base=base):
                        tsl = slice(tb * 512, (tb + 1) * 512)
                        S.stt("dve", xT[:, dc, tsl], po[:], modcol[1][:, 40 + dc:41 + dc], xT[:, dc, tsl], ALU.mult, ALU.add,
                              [dpo, dmod[1], dxT[dc][tb]], [dxT[dc][tb]])

                    def gate(tb, base=base):
                        return gbc[base + tb][:], dgbc[base + tb]

                    w1e = P["od_moe_w1"][e].rearrange("(kc p) f -> p kc f", p=128)
                    w3e = P["od_moe_w3"][e].rearrange("(kc p) f -> p kc f", p=128)
                    w2e = P["od_moe_w2"][e]
                    swiglu(ph, lambda a, b, w=w1e: w[:, :, a:b], lambda a, b, w=w3e: w[:, :, a:b],
                           lambda f0, n, w=w2e: w[f0 * 128:(f0 + n) * 128, :].rearrange("(j p) d -> p j d", p=128), 3584, upd, "m%d" % e, gate=gate)
                S.barrier()


        def ssd_main(ssacc, dssacc):
            with ExitStack() as ph:
                with ExitStack() as ph2:
                    modulate(ph2, 1, 0, "s")
                    dxscr = Dep()
                    for fc in range(8):
                        S.dma("sp", xscr[:, fc, :], xT[:, fc, :], reads=dxT[fc], writes=[dxscr])
                    S.barrier()
                cwcol = sb(ph, "cwcol", [128, 120], F32)
                dcw = Dep()
                dtb = sb(ph, "dtb", [128, 64], F32)
                abc = sb(ph, "abc", [128, 64], F32)
                dbc = sb(ph, "dbc", [128, 32], F32)
                dprm = Dep()
                cst2 = {}
                for k in ("trif", "trib", "sellast", "selfirst", "negf", "negb"):
                    cst2[k] = sb(ph, "c_" + k, [128, 128], F32)
                wdtt = sb(ph, "wdtt", [128, 8, 64], BF16)
                dwdt = Dep()
                for kc in range(8):
                    S.dma("pool", wdtt[:, kc, :], w_view("od_in_w", 5120, 5184)[:, kc, :], writes=[dwdt])
                dcst2 = Dep()
                for k in cst2:
                    S.dma("sp", cst2[k][:], P[k][:, :], writes=[dcst2])
                with ExitStack() as ph2:
                    cwrow = sb(ph2, "cwrow", [120, 128], F32)
                    dcwr = Dep()
                    S.dma("sp", cwrow[:], P["od_conv_w"][:, :], writes=[dcwr])
                    ps, dps = PSA.next()
                    S.tr(ps[:, 0:120], cwrow[:], identf[0:120, 0:120], [dcwr, dcst["identf"]], [dps])
                    S.copy("dve", cwcol[:], ps[:, 0:120], [dps], [dcw])
                    bc = lambda nm, n: bass.AP(tensor=P[nm].tensor, offset=0, ap=[[0, 128], [1, n]])
                    S.dma("sp", dtb[:, 0:32], bc("od_dt_bias_f", 32), writes=[dprm])
                    S.dma("sp", dtb[:, 32:64], bc("od_dt_bias_b", 32), writes=[dprm])
                    S.dma("sp", abc[:, 0:32], bc("od_A_log_f", 32), writes=[dprm])
                    S.dma("sp", abc[:, 32:64], bc("od_A_log_b", 32), writes=[dprm])
                    S.dma("sp", dbc[:], bc("od_D", 32), writes=[dprm])
                    S.act(abc[:], abc[:], AF.Exp, [dprm], [dprm])
                    S.ts("dve", abc[:], abc[:], -1.0, None, ALU.mult, None, [dprm], [dprm])
                    S.barrier()
                def _dtset(i):
                    if i == 0:
                        return [sb(ph, "dtset0_%d" % k, [128, 16, 16], F32) for k in range(5)]
                    return [xT[:, 6, k * 256:(k + 1) * 256].rearrange("p (a b) -> p a b", b=16) for k in range(5)]
                dtsets = [_dtset(0), _dtset(1)]
                ddts = deps(2)
                dacscrs = deps(2)
                ddtmp = Dep()
                a_tok = sb(ph, "a_tok", [128, 16, 16], F32)
                acT = [sb(ph, "acT%d" % i, [16, 512], F32) for i in range(2)]
                dacT = deps(2)
                tmpd = sb(ph, "tmpd", [128, 16], F32)
                dtmpd = Dep()
                dtbg = sb(ph, "dtbg", [128, 16], F32)
                abcg = sb(ph, "abcg", [128, 16], F32)

                def dt_path(g):
                    dt_tok, acum, ea, dec, cdb = dtsets[g % 2]
                    ddt = ddts[g % 2]
                    dacscr = dacscrs[g % 2]
                    r0 = (g % 2) * 16
                    for d in range(2):
                        S.copy("dve", dtbg[:, d * 8:(d + 1) * 8], dtb[:, d * 32 + g * 8:d * 32 + g * 8 + 8], [dprm], [ddtmp])
                        S.copy("dve", abcg[:, d * 8:(d + 1) * 8], abc[:, d * 32 + g * 8:d * 32 + g * 8 + 8], [dprm], [ddtmp])
                    for tt in range(16):
                        ps, dps = PSM.next()
                        for d in range(2):
                            for kc in range(8):
                                S.mm(ps[:, d * 8:(d + 1) * 8], hT[:, kc, tt * 128:(tt + 1) * 128], wdtt[:, kc, d * 32 + g * 8:d * 32 + g * 8 + 8], kc == 0, kc == 7,
                                     [dhT[kc][tt // 4], dwdt], [dps])
                        S.tt("dve", tmpd[:], ps[:, 0:16], dtbg[:], ALU.add, [dps, ddtmp], [dtmpd])
                        S.act(tmpd[:], tmpd[:], AF.Exp, [dtmpd], [dtmpd])
                        S.act(dt_tok[:, tt, :], tmpd[:], AF.Ln, [dtmpd, dcst["onesf"]], [ddt], bias=onesf[:, 0:1], scale=1.0)
                        S.tt("dve", a_tok[:, tt, :], dt_tok[:, tt, :], abcg[:], ALU.mult, [ddt, ddtmp], [ddtmp])
                    for c in range(16):
                        ps, dps = PSM.next()
                        S.mm(ps[:, 0:8], cst2["trif"][:], a_tok[:, c, 0:8], True, True, [dcst2, ddtmp], [dps])
                        S.mm(ps[:, 8:16], cst2["trib"][:], a_tok[:, c, 8:16], True, True, [dcst2, ddtmp], [dps])
                        S.copy("dve", acum[:, c, :], ps[:, 0:16], [dps], [ddt])
                        S.act(ea[:, c, :], ps[:, 0:16], AF.Exp, [dps], [ddt])
                        p2, dp2 = PSM.next()
                        S.mm(p2[:, 0:8], cst2["sellast"][:], acum[:, c, 0:8], True, True, [dcst2, ddt], [dp2])
                        S.mm(p2[:, 8:16], cst2["selfirst"][:], acum[:, c, 8:16], True, True, [dcst2, ddt], [dp2])
                        S.act(cdb[:, c, :], p2[:, 0:16], AF.Exp, [dp2], [ddt])
                        S.tt("dve", tmpd[:], p2[:, 0:16], acum[:, c, :], ALU.subtract, [dp2, ddt], [dtmpd])
                        S.act(dec[:, c, :], tmpd[:], AF.Exp, [dtmpd], [ddt])
                    for c4 in range(4):
                        ps, dps = PSA.next()
                        for c1 in range(4):
                            c = c4 * 4 + c1
                            S.tr(ps[0:16, c1 * 128:(c1 + 1) * 128], acum[:, c, :], identf[:], [ddt, dcst["identf"]], [dps])
                        S.copy("dve", acT[c4 % 2][:], ps[0:16, :], [dps], [dacT[c4 % 2]])
                        S.dma("sp", acscr[r0:r0 + 16, c4 * 512:(c4 + 1) * 512], acT[c4 % 2][:], reads=[dacT[c4 % 2]], writes=[dacscr])

                raw = sb(ph, "craw", [128, S_LEN + 4], BF16)
                draw = Dep()
                S.memset("pool", raw[:, 0:2], 0.0, [draw])
                S.memset("pool", raw[:, S_LEN + 2:S_LEN + 4], 0.0, [draw])

                dgs = [xT[:, 4, i * 320:(i + 1) * 320].bitcast(BF16).rearrange("p (a b) -> p a b", b=128) for i in range(2)]
                ddgs = deps(2)
                kdg = [0]

                def conv_chunk(wt, dwt, wcol0, cchunk, dst_ap, ddst):
                    i = kdg[0] % 2
                    kdg[0] += 1
                    dg, ddg = dgs[i], ddgs[i]
                    for w in range(5):
                        S.ts("dve", dg[:, w, :], identb[:], cwcol[:, w * 24 + cchunk:w * 24 + cchunk + 1], None, ALU.mult, None, [dcst["identb"], dcw], [ddg])
                    for tb in range(4):
                        tsl = slice(tb * 512, (tb + 1) * 512)
                        ps, dps = PSM.next()
                        for kc in range(8):
                            S.mm(ps[:], wt[:, kc, wcol0:wcol0 + 128], hT[:, kc, tsl], kc == 0, kc == 7, [dwt, dhT[kc][tb]], [dps])
                        S.copy("act", raw[:, 2 + tb * 512:2 + (tb + 1) * 512], ps[:], [dps], [draw])
                    for tb in range(4):
                        tsl = slice(tb * 512, (tb + 1) * 512)
                        pc, dpc = PSM.next()
                        for w in range(5):
                            S.mm(pc[:], dg[:, w, :], raw[:, w + tb * 512:w + tb * 512 + 512], w == 0, w == 4, [ddg, draw], [dpc])
                        S.act(dst_ap[:, tsl], pc[:], AF.Silu, [dpc, dpcol], ddst, bias=pcol[:, 48 + cchunk:49 + cchunk], scale=1.0)

                BT = sb(ph, "BT", [128, S_LEN], BF16)
                CT = sb(ph, "CT", [128, S_LEN], BF16)
                dBT, dCT = Dep(), Dep()
                Btok = sb(ph, "Btok", [128, 16, 128], BF16)
                dBtok = Dep()
                cbt = sb(ph, "cbt", [128, 16, 128], BF16)
                dcbt = Dep()
                xsT = sb(ph, "xsT", [128, S_LEN], BF16)
                dxsT = Dep()
                Xd = [sb(ph, "Xd%d" % i, [128, 16, 128], BF16) for i in range(2)]
                dXd = Dep()
                yaccs = [xT[:, 1, :].rearrange("p (a b) -> p a b", b=128), xT[:, 5, :].rearrange("p (a b) -> p a b", b=128)]
                dyaccs = [deps(16), deps(16)]
                arow = [xT[:, 0, i * 1024:(i + 1) * 1024].rearrange("p (a b) -> p a b", b=512) for i in range(2)]
                darow = deps(2)
                Hsr = [xT[:, 3, i * 128:(i + 1) * 128] for i in range(8)]
                dHsr = deps(8)
                lt = [sb(ph, "lt%d" % i, [128, 2, 512], BF16) for i in range(2)]
                dlt = deps(2)
                S_all = sb(ph, "S_all", [128, 2, 16, 128], BF16)
                dSall = [deps(4) for _ in range(2)]
                Hb_all = sb(ph, "Hb_all", [128, 2, 16, 128], BF16)
                dHball = [deps(16) for _ in range(2)]
                S.memset("dve", Hb_all[:, 0, 0, :], 0.0, [dHball[0][0]])
                S.memset("dve", Hb_all[:, 1, 15, :], 0.0, [dHball[1][15]])
                xdr = [sb(ph, "xdr%d" % i, [128, 128], BF16) for i in range(4)]
                dxdr = deps(4)
                szt2 = [sb(ph, "szt%d" % i, [128, 512], BF16) for i in range(2)]
                ygf2 = [xT[:, 3, 1024 + i * 512:1024 + (i + 1) * 512] for i in range(2)]
                sqg2 = [sb(ph, "sqg%d" % i, [128, 512], BF16) for i in range(2)]
                dgate2 = [deps(3) for _ in range(2)]
                ygb = [xT[:, 4, 1024 + i * 256:1024 + (i + 1) * 256].bitcast(BF16) for i in range(2)]
                dygb = deps(2)
                dygscr = Dep()
                kit = [0]
                kyg = [0]
                kxd = [0]
                khs = [0]

                def prep_conv(g, j, wx, dwx):
                    conv_chunk(wx, dwx, j * 128, g * 4 + j, xsT, [dxsT])

                def prep_evac(g, j, yacc, dyacc):
                    dt_tok = dtsets[g % 2][0]
                    ddt = ddts[g % 2]
                    fcx = g * 4 + j
                    h0 = 8 * g + 2 * j
                    for t8 in range(2):
                        for t1_ in range(8):
                            tt = t8 * 8 + t1_
                            S.tr(psb_t[:, t1_ * 128:(t1_ + 1) * 128], xsT[:, tt * 128:(tt + 1) * 128], identb[:], [dxsT, dcst["identb"]], [dpsb])
                        for t1_ in range(8):
                            tt = t8 * 8 + t1_
                            src = psb_t[:, t1_ * 128:(t1_ + 1) * 128].rearrange("p (a b) -> p a b", b=64)
                            for d in range(2):
                                hd0 = d * 8 + 2 * j
                                S.tt("dve", Xd[d][:, tt, :].rearrange("p (a b) -> p a b", b=64), src,
                                     dt_tok[:, tt, hd0:hd0 + 2].unsqueeze(2).to_broadcast([128, 2, 64]), ALU.mult, [dpsb, ddt], [dXd])
                            S.tt("dve", yacc[:, tt, :].rearrange("p (a b) -> p a b", b=64), src,
                                 dbc[:, h0:h0 + 2].unsqueeze(2).to_broadcast([128, 2, 64]), ALU.mult, [dpsb, dprm], [dyacc[tt]])
                    if fcx == 0:
                        dbg("xsT", xsT, [128, S_LEN], BF16, [dxsT])
                        dbg("Xf", Xd[0], [128, 16, 128], BF16, [dXd])
                        dbg("Xb", Xd[1], [128, 16, 128], BF16, [dXd])
                        dbg("Btok", Btok, [128, 16, 128], BF16, [dBtok])
                        dbg("cbt", cbt, [128, 16, 128], BF16, [dcbt])

                def phase_a(g, j, yacc, dyacc):
                    dt_tok, acum, ea, dec, cdb = dtsets[g % 2]
                    ddt = ddts[g % 2]
                    dacscr = dacscrs[g % 2]
                    r0 = (g % 2) * 16

                    def stage1(d, b4):
                        hd0 = d * 8 + 2 * j
                        negm = cst2["negf" if d == 0 else "negb"]
                        c0 = b4 * 4
                        k = kit[0]
                        kit[0] += 1
                        ar, dar = arow[k % 2], darow[k % 2]
                        lt_, dlt_ = lt[k % 2], dlt[k % 2]
                        for hh in range(2):
                            S.dma("sp", ar[:, hh, :], bass.AP(tensor=acscr.tensor, offset=(r0 + hd0 + hh) * S_LEN + c0 * 128, ap=[[0, 128], [1, 512]]),
                                  reads=[dacscr], writes=[dar])
                        for hh in range(2):
                            av = ar[:, hh, :].rearrange("p (a b) -> p a b", b=128)
                            S.tt("dve", av, av, acum[:, c0:c0 + 4, hd0 + hh:hd0 + hh + 1].to_broadcast([128, 4, 128]), ALU.subtract, [dar, ddt], [dar])
                        av8 = ar[:].rearrange("p h (a b) -> p (h a) b", b=128)
                        S.tt("dve", av8, av8, negm[:].unsqueeze(1).to_broadcast([128, 8, 128]), ALU.min, [dar, dcst2], [dar])
                        S.act(lt_[:], ar[:], AF.Exp, [dar], [dlt_])
                        return (d, b4, lt_, dlt_)

                    def stage2(st_):
                        d, b4, lt_, dlt_ = st_
                        hd0 = d * 8 + 2 * j
                        c0 = b4 * 4
                        S.tt("dve", lt_[:], lt_[:], cbt[:, c0:c0 + 4, :].rearrange("p a b -> p (a b)").unsqueeze(1).to_broadcast([128, 2, 512]), ALU.mult,
                             [dlt_, dcbt], [dlt_])
                        p1, dp1 = PSM.next()
                        for c in range(4):
                            for hh in range(2):
                                S.mm(p1[:, c * 128 + hh * 64:c * 128 + (hh + 1) * 64], lt_[:, hh, c * 128:(c + 1) * 128],
                                     Xd[d][:, c0 + c, hh * 64:(hh + 1) * 64], True, True, [dlt_, dXd], [dp1])
                        p3, dp3 = PSA.next()
                        for c in range(4):
                            kx = kxd[0] % 4
                            kxd[0] += 1
                            S.tt("pool", xdr[kx][:].rearrange("p (a b) -> p a b", b=64), Xd[d][:, c0 + c, :].rearrange("p (a b) -> p a b", b=64),
                                 dec[:, c0 + c, hd0:hd0 + 2].unsqueeze(2).to_broadcast([128, 2, 64]), ALU.mult, [dXd, ddt], [dxdr[kx]])
                            S.mm(p3[:, c * 128:(c + 1) * 128], Btok[:, c0 + c, :], xdr[kx][:], True, True, [dBtok, dxdr[kx]], [dp3])
                        S.tt("dve", yacc[:, c0:c0 + 4, :], p1[:].rearrange("p (a b) -> p a b", b=128), yacc[:, c0:c0 + 4, :], ALU.add,
                             [dp1] + dyacc[c0:c0 + 4], dyacc[c0:c0 + 4])
                        S.copy("act", S_all[:, d, c0:c0 + 4, :], p3[:].rearrange("p (a b) -> p a b", b=128), [dp3], [dSall[d][b4]])

                    prev_st = None
                    for (d, b4) in [(d, b4) for d in range(2) for b4 in range(4)]:
                        st_ = stage1(d, b4)
                        if prev_st is not None:
                            stage2(prev_st)
                        prev_st = st_
                    stage2(prev_st)

                def phase_b(g, j):
                    dt_tok, acum, ea, dec, cdb = dtsets[g % 2]
                    ddt = ddts[g % 2]
                    hprev = [None, None]
                    for step in range(16):
                        for d in range(2):
                            hd0 = d * 8 + 2 * j
                            c = step if d == 0 else 15 - step
                            kh = khs[0] % 8
                            khs[0] += 1
                            hn, dhn = Hsr[kh], dHsr[kh]
                            if step == 0:
                                S.copy("dve", hn, S_all[:, d, c, :], [dSall[d][c // 4]], [dhn])
                            else:
                                hp, dhp = hprev[d]
                                for hh in range(2):
                                    S.stt("dve", hn[:, hh * 64:(hh + 1) * 64], hp[:, hh * 64:(hh + 1) * 64], cdb[:, c, hd0 + hh:hd0 + hh + 1],
                                          S_all[:, d, c, hh * 64:(hh + 1) * 64], ALU.mult, ALU.add, [dhp, ddt, dSall[d][c // 4]], [dhn])
                            if step < 15:
                                cn = c + 1 if d == 0 else c - 1
                                S.copy("act", Hb_all[:, d, cn, :], hn, [dhn], [dHball[d][cn]])
                            hprev[d] = (hn, dhn)

                def phase_c(g, j, yacc, dyacc):
                    dt_tok, acum, ea, dec, cdb = dtsets[g % 2]
                    ddt = ddts[g % 2]
                    for d in range(2):
                        hd0 = d * 8 + 2 * j
                        for b4 in range(4):
                            c0 = b4 * 4
                            p2, dp2 = PSM.next()
                            for c in range(4):
                                csl = slice((c0 + c) * 128, (c0 + c + 1) * 128)
                                S.mm(p2[:, c * 128:(c + 1) * 128], CT[:, csl], Hb_all[:, d, c0 + c, :], True, True, [dCT, dHball[d][c0 + c]], [dp2])
                            for c in range(4):
                                for hh in range(2):
                                    ysl = yacc[:, c0 + c, hh * 64:(hh + 1) * 64]
                                    S.stt("dve", ysl, p2[:, c * 128 + hh * 64:c * 128 + (hh + 1) * 64], ea[:, c0 + c, hd0 + hh:hd0 + hh + 1], ysl,
                                          ALU.mult, ALU.add, [dp2, ddt, dyacc[c0 + c]], [dyacc[c0 + c]])

                def gating(g, j, wz, dwz, yacc, dyacc):
                    fcx = g * 4 + j
                    if fcx == 0:
                        dbg("yacc", xT[:, 1, :], [128, S_LEN], F32, dyacc)

                    def g1(tb):
                        tsl = slice(tb * 512, (tb + 1) * 512)
                        py, dpy = PSM.next()
                        for t4 in range(4):
                            tt = tb * 4 + t4
                            S.tr(py[:, t4 * 128:(t4 + 1) * 128], yacc[:, tt, :], identf[:], [dyacc[tt], dcst["identf"]], [dpy])
                        pz, dpz = PSM.next()
                        for kc in range(8):
                            S.mm(pz[:], wz[:, kc, j * 128:(j + 1) * 128], hT[:, kc, tsl], kc == 0, kc == 7, [dwz, dhT[kc][tb]], [dpz])
                        ig = kyg[0] % 2
                        kyg[0] += 1
                        szt, ygf, sqg = szt2[ig], ygf2[ig], sqg2[ig]
                        dsz_, dyg_, dsq_ = dgate2[ig]
                        S.act(szt[:], pz[:], AF.Silu, [dpz], [dsz_])
                        S.tt("dve", ygf, py[:], szt[:], ALU.mult, [dpy, dsz_], [dyg_])
                        S.act(sqg[:], ygf, AF.Square, [dyg_], [dsq_])
                        return (tb, ig)

                    def g2(st_):
                        tb, ig = st_
                        tsl = slice(tb * 512, (tb + 1) * 512)
                        szt, ygf, sqg = szt2[ig], ygf2[ig], sqg2[ig]
                        dsz_, dyg_, dsq_ = dgate2[ig]
                        pq, dpq = PSA.next()
                        S.mm(pq[:], onesb[:], sqg[:], True, True, [dsq_, dcst["onesb"]], [dpq])
                        if fcx == 0:
                            S.copy("dve", ssacc[:, tsl], pq[:], [dpq], [dssacc[tb]])
                        else:
                            S.tt("dve", ssacc[:, tsl], pq[:], ssacc[:, tsl], ALU.add, [dpq, dssacc[tb]], [dssacc[tb]])
                        S.ts("dve", ygb[ig][:], ygf, pcol[:, 32 + fcx:33 + fcx], None, ALU.mult, None, [dyg_, dpcol], [dygb[ig]])
                        S.dma("sp", ygscr[fcx * 128:(fcx + 1) * 128, tsl], ygb[ig][:], reads=[dygb[ig]], writes=[dygscr])

                    prev = None
                    for tb in range(4):
                        st_ = g1(tb)
                        if prev is not None:
                            g2(prev)
                        prev = st_
                    g2(prev)

                dt_path(0)
                for g in range(4):
                    if g == 0:
                        for nm_, t_ in zip(("dt", "acum", "ea", "dec", "cdb"), dtsets[0]):
                            dbg(nm_, t_, [128, 16, 16], F32, [ddts[0]])
                    wz, dwz = load_w(w_view("od_in_w", g * 512, (g + 1) * 512), 8, 512)
                    wx, dwx = load_w(w_view("od_in_w", 2048 + g * 512, 2048 + (g + 1) * 512), 8, 512)
                    wbc, dwbc = wring.next()
                    for kc in range(8):
                        S.dma("pool", wbc[:, kc, 0:128], w_view("od_in_w", 4096 + g * 128, 4096 + (g + 1) * 128)[:, kc, :], writes=[dwbc])
                        S.dma("pool", wbc[:, kc, 128:256], w_view("od_in_w", 4608 + g * 128, 4608 + (g + 1) * 128)[:, kc, :], writes=[dwbc])
                    conv_chunk(wbc, dwbc, 0, 16 + g, BT, [dBT])
                    conv_chunk(wbc, dwbc, 128, 20 + g, CT, [dCT])
                    if g == 0:
                        dbg("BT", BT, [128, S_LEN], BF16, [dBT])
                        dbg("CT", CT, [128, S_LEN], BF16, [dCT])
                    for t8 in range(2):
                        for t1_ in range(8):
                            tt = t8 * 8 + t1_
                            S.tr(psb_t[:, t1_ * 128:(t1_ + 1) * 128], BT[:, tt * 128:(tt + 1) * 128], identb[:], [dBT, dcst["identb"]], [dpsb])
                        S.copy("act", Btok[:, t8 * 8:(t8 + 1) * 8, :], psb_t[:, :].rearrange("p (a b) -> p a b", b=128), [dpsb], [dBtok])
                    for c in range(16):
                        ps, dps = PSM.next()
                        csl = slice(c * 128, (c + 1) * 128)
                        S.mm(ps[:, 0:128], BT[:, csl], CT[:, csl], True, True, [dBT, dCT], [dps])
                        S.copy("act", cbt[:, c, :], ps[:, 0:128], [dps], [dcbt])
                    prep_conv(g, 0, wx, dwx)
                    prep_evac(g, 0, yaccs[0], dyaccs[0])
                    for j in range(4):
                        ya, dya = yaccs[j % 2], dyaccs[j % 2]
                        phase_a(g, j, ya, dya)
                        if j < 3:
                            prep_conv(g, j + 1, wx, dwx)
                        phase_b(g, j)
                        if j < 3:
                            prep_evac(g, j + 1, yaccs[(j + 1) % 2], dyaccs[(j + 1) % 2])
                        elif g < 3:
                            dt_path(g + 1)
                        phase_c(g, j, ya, dya)
                        gating(g, j, wz, dwz, ya, dya)
                S.barrier()
                for fc in range(8):
                    S.dma("sp", xT[:, fc, :], xscr[:, fc, :], reads=[], writes=dxT[fc])

        def ssd_out(ssacc, dssacc):
            with ExitStack() as ph2:
                rs = sb(ph2, "rs_o", [128, S_LEN], F32)
                drs = Dep()
                S.act(rs[:], ssacc[:], AF.Sqrt, dssacc + [depsc], [drs], bias=epsc[:, 0:1], scale=1.0 / 2048)
                S.recip(rs[:], rs[:], [drs], [drs])
                dbg("rs", rs, [128, S_LEN], F32, [drs])
                ygt = [sb(ph2, "ygt%d" % i, [128, 16, 512], BF16) for i in range(2)]
                dygt = deps(2)
                ot1 = [sb(ph2, "ot1_%d" % i, [128, 512], F32) for i in range(2)]
                dot1 = deps(2)
                ko = 0
                for tb in range(4):
                    tsl = slice(tb * 512, (tb + 1) * 512)
                    yt, dyt = ygt[tb % 2], dygt[tb % 2]
                    for kc in range(16):
                        S.dma("sp", yt[:, kc, :], ygscr[kc * 128:(kc + 1) * 128, tsl], reads=[], writes=[dyt])
                    for half in range(2):
                        wa, dwa = load_w(P["od_out_w"][0:1024, half * 512:(half + 1) * 512].rearrange("(kc p) f -> p kc f", p=128), 8, 512)
                        wb_, dwb_ = load_w(P["od_out_w"][1024:2048, half * 512:(half + 1) * 512].rearrange("(kc p) f -> p kc f", p=128), 8, 512)
                        for d4 in range(4):
                            dc = half * 4 + d4
                            ps, dps = PSM.next()
                            for kc in range(16):
                                wt_, dwt_ = (wa, dwa) if kc < 8 else (wb_, dwb_)
                                S.mm(ps[:], wt_[:, kc % 8, d4 * 128:(d4 + 1) * 128], yt[:, kc, :], kc == 0, kc == 15, [dwt_, dyt], [dps])
                            o1, do1 = ot1[ko % 2], dot1[ko % 2]
                            ko += 1
                            S.tt("dve", o1[:], ps[:], rs[:, tsl], ALU.mult, [dps, drs], [do1])
                            S.stt("dve", xT[:, dc, tsl], o1[:], modcol[1][:, 16 + dc:17 + dc], xT[:, dc, tsl], ALU.mult, ALU.add,
                                  [do1, dmod[1], dxT[dc][tb]], [dxT[dc][tb]])
                S.barrier()


        def ssd_phase_inner():
            with ExitStack() as pho:
                ssacc = sb(pho, "ssacc", [128, S_LEN], F32)
                dssacc = deps(4)
                ssd_main(ssacc, dssacc)
                ssd_out(ssacc, dssacc)

        if p0 <= 0 < p1:
          if True:
            with ExitStack() as ph:
                with ExitStack() as ph2:
                    modulate(ph2, 0, 0, "a")
                    S.barrier()
                    stage(S, 1)
                qT = sb(ph, "qT", [128, 4, S_LEN], BF16)
                dqT = [deps(4) for _ in range(4)]
                kT = sb(ph, "kT", [128, S_LEN], BF16)
                dkT = deps(4)
                kTs = sb(ph, "kTs", [128, S_LEN], BF16)
                dkTs = Dep()
                vtok = sb(ph, "vtok", [128, 16, 2, 65], BF16)
                dvtok = deps(16)
                wqk = sb(ph, "wqk", [128, 2], F32)
                dwqk = Dep()
                ph_u = ExitStack()
                ph.enter_context(ph_u)
                uT = sb(ph_u, "uT", [128, 4, S_LEN], BF16)
                duT = [deps(4) for _ in range(4)]
                S.dma("sp", wqk[0:64, 0:1], P["ev_q_norm_w"][:, :], writes=[dwqk])
                S.dma("sp", wqk[64:128, 0:1], P["ev_q_norm_w"][:, :], writes=[dwqk])
                S.dma("sp", wqk[0:64, 1:2], P["ev_k_norm_w"][:, :], writes=[dwqk])
                S.dma("sp", wqk[64:128, 1:2], P["ev_k_norm_w"][:, :], writes=[dwqk])
                S.op("act", lambda e: e.mul(out=wqk[:, 0:1], in_=wqk[:, 0:1], mul=0.125), [dwqk], [dwqk])
                S.memset("dve", vtok[:], 1.0, dvtok)
                with ExitStack() as ph2:
                    sqb = [sb(ph2, "qsq%d" % i, [128, 512], BF16) for i in range(2)]
                    dsqb = [Dep(), Dep()]
                    qraw = [sb(ph2, "qraw%d" % i, [128, 512], F32) for i in range(2)]
                    dqraw = [Dep(), Dep()]
                    qrs = [sb(ph2, "qrs%d" % i, [128, 512], F32) for i in range(2)]
                    dqrs = [Dep(), Dep()]
                    wt, dwt = load_w(w_view("ev_in_w", 0, 512), 8, 512)
                    for oc in range(4):
                        for tb in range(4):
                            tsl = slice(tb * 512, (tb + 1) * 512)
                            ps, dps = PSM.next()
                            for kc in range(8):
                                S.mm(ps[:], wt[:, kc, oc * 128:(oc + 1) * 128], hT[:, kc, tsl], kc == 0, kc == 7, [dwt, dhT[kc][tb]], [dps])
                            S.copy(evq.next(), uT[:, oc, tsl], ps[:], [dps], [duT[oc][tb]])
                    stage(S, 21)
                    wt, dwt = load_w(w_view("ev_in_w", 512, 1024), 8, 512)
                    wt2, dwt2 = load_w(w_view("ev_in_w", 1024, 1280), 8, 256)
                    pend = []
                    kk = 0

                    def qk_stage2(ps, dps, ii, dst_ap, ddst, wcol):
                        if STAGE == 221:
                            return
                        pa, dpa = PSA.next()
                        S.mm(pa[:], bd64[:], sqb[ii][:], True, True, [dsqb[ii], dcst["bd64"]], [dpa])
                        S.act(qrs[ii][:], pa[:], AF.Sqrt, [dpa, depsc], [dqrs[ii]], bias=epsc[:, 0:1], scale=1.0 / 64)
                        S.recip(qrs[ii][:], qrs[ii][:], [dqrs[ii]], [dqrs[ii]])
                        S.stt("dve", dst_ap, qraw[ii][:], wqk[:, wcol:wcol + 1], qrs[ii][:], ALU.mult, ALU.mult, [dqraw[ii], dwqk, dqrs[ii]], [ddst])

                    for oc in range(5):
                        for tb in range(4):
                            tsl = slice(tb * 512, (tb + 1) * 512)
                            ps, dps = PSM.next()
                            for kc in range(8):
                                if oc < 4:
                                    S.mm(ps[:], wt[:, kc, oc * 128:(oc + 1) * 128], hT[:, kc, tsl], kc == 0, kc == 7, [dwt, dhT[kc][tb]], [dps])
                                else:
                                    S.mm(ps[:], wt2[:, kc, 0:128], hT[:, kc, tsl], kc == 0, kc == 7, [dwt2, dhT[kc][tb]], [dps])
                            ii = kk % 2
                            kk += 1
                            S.copy("act", qraw[ii][:], ps[:], [dps], [dqraw[ii]])
                            S.act(sqb[ii][:], qraw[ii][:], AF.Square, [dqraw[ii]], [dsqb[ii]])
                            if pend:
                                pend.pop(0)()
                            if oc < 4:
                                pend.append(lambda ps=ps, dps=dps, ii=ii, oc=oc, tb=tb, tsl=tsl: qk_stage2(ps, dps, ii, qT[:, oc, tsl], dqT[oc][tb], 0))
                            else:
                                pend.append(lambda ps=ps, dps=dps, ii=ii, tb=tb, tsl=tsl: qk_stage2(ps, dps, ii, kT[:, tsl], dkT[tb], 1))
                    while pend:
                        pend.pop(0)()
                    stage(S, 22)
                    S.dma("sp", kTs[64:128, :], kT[0:64, :], reads=dkT, writes=[dkTs])
                    S.dma("sp", kTs[0:64, :], kT[64:128, :], reads=dkT, writes=[dkTs])
                    for tt in range(16):
                        ps, dps = PSM.next()
                        for kc in range(8):
                            S.mm(ps[:, 0:128], hT[:, kc, tt * 128:(tt + 1) * 128], wt2[:, kc, 128:256], kc == 0, kc == 7, [dwt2, dhT[kc][tt // 4]], [dps])
                        S.copy(evq.next(), vtok[:, tt, :, 0:64], ps[:, 0:128].rearrange("p (a b) -> p a b", a=2), [dps], [dvtok[tt]])
                    S.barrier()
                    stage(S, 2)
                yT, dyT = hT, dhT
                with ExitStack() as ph2:
                    ccsc = sb(ph2, "ccsc", [128, 256], BF16)
                    dccsc = Dep()
                    S.dma("sp", ccsc[:], C["ccsc"][:, :], writes=[dccsc])
                    ab = sb(ph2, "ab", [128, 2, 16, 256], BF16)
                    dab = [deps(16) for _ in range(2)]
                    cst_t = [sb(ph2, "cs_t%d" % i, [128, 4, 512], BF16) for i in range(2)]
                    sst_t = [sb(ph2, "ss_t%d" % i, [128, 4, 512], BF16) for i in range(2)]
                    dcs_t = [Dep(), Dep()]
                    dss_t = [Dep(), Dep()]
                    csv = C["cs_mat"].rearrange("(t p) n -> p t n", p=128)
                    ssv = C["ss_mat"].rearrange("(t p) n -> p t n", p=128)
                    k = 0
                    for gp in range(2):
                        for g2 in range(2):
                            g = gp * 2 + g2
                            for tt in range(16):
                                ps, dps = PSA.next()
                                S.mm(ps[:, 0:256], uT[:, g, tt * 128:(tt + 1) * 128], ccsc[:], True, True, [duT[g][tt // 4], dccsc], [dps])
                                S.copy(evq.next(), ab[:, g2, tt, :], ps[:, 0:256], [dps], [dab[g2][tt]])
                        for nb in range(4):
                            accs = [PSM.next() for _ in range(2)]
                            for qq in range(4):
                                i = k % 2
                                k += 1
                                S.dma("sp", cst_t[i][:], csv[:, qq * 4:(qq + 1) * 4, nb * 512:(nb + 1) * 512], writes=[dcs_t[i]])
                                S.dma("act", sst_t[i][:], ssv[:, qq * 4:(qq + 1) * 4, nb * 512:(nb + 1) * 512], writes=[dss_t[i]])
                                for g2 in range(2):
                                    ps, dps = accs[g2]
                                    for t4 in range(4):
                                        tt = qq * 4 + t4
                                        S.mm(ps[:], ab[:, g2, tt, 0:128], cst_t[i][:, t4, :], tt == 0, False, [dab[g2][tt], dcs_t[i]], [dps])
                                        S.mm(ps[:], ab[:, g2, tt, 128:256], sst_t[i][:, t4, :], False, tt == 15, [dab[g2][tt], dss_t[i]], [dps])
                            for g2 in range(2):
                                ps, dps = accs[g2]
                                S.copy(evq.next(), yT[:, gp * 2 + g2, nb * 512:(nb + 1) * 512], ps[:], [dps], [dyT[gp * 2 + g2][nb]])
                    S.barrier()
                    stage(S, 3)
                ph_u.close()
                with ExitStack() as ph2:
                    rb = sb(ph2, "rb", [32, 8], F32)
                    drb = Dep()
                    rbb = sb(ph2, "rbb", [32, 8, 128], F32)
                    drbb = Dep()
                    ohr = sb(ph2, "ohr", [32, 512], F32)
                    dohr = Dep()
                    amask = sb(ph2, "amask", [128, 384], F32)
                    damask = Dep()
                    bm = sb(ph2, "bm", [128, 8, 384], F32)
                    dbm = Dep()
                    tv = sb(ph2, "tv", [128, 512], F32)
                    dtv = Dep()
                    esb = sb(ph2, "esb", [128, 8], F32)
                    desb = Dep()
                    dts = deps(8)
                    S.dma("sp", rb[:], P["rel_bias"][:, :], writes=[drb])
                    S.dma("sp", ohr[:], C["ohr"][:, :], writes=[dohr])
                    S.dma("sp", amask[:], C["amask"][:, :], writes=[damask])
                    S.dma("sp", esb[:], bass.AP(tensor=P["ev_sink"].tensor, offset=0, ap=[[0, 128], [1, 8]]), writes=[desb])
                    S.act(esb[:], esb[:], AF.Exp, [desb], [desb])
                    for h in range(8):
                        S.copy("dve", rbb[:, h, :], rb[:, h:h + 1].to_broadcast([32, 128]), [drb], [drbb])
                    for h in range(8):
                        ps, dps = PSA.next()
                        S.mm(ps[:], rbb[:, h, :], ohr[:], True, True, [drbb, dohr], [dps])
                        S.copy("dve", tv[:], ps[:], [dps], [dtv])
                        S.dma("sp", tscr[h], tv[:], reads=[dtv], writes=[dts[h]])
                        S.dma("sp", bm[:, h, :], bass.AP(tensor=tscr.tensor, offset=h * 128 * 512 + 127, ap=[[511, 128], [1, 384]]), reads=[dts[h]], writes=[dbm])
                    for h in range(8):
                        S.tt("dve", bm[:, h, :], bm[:, h, :], amask[:], ALU.add, [dbm, damask], [dbm])
                    et = [sb(ph2, "et%d" % i, [128, 8, 384], BF16) for i in range(5)]
                    det = [Dep() for _ in range(5)]
                    ltmp = [sb(ph2, "ltmp%d" % i, [128, 384], F32) for i in range(2)]
                    dltmp = [Dep(), Dep()]
                    otok = [sb(ph2, "otok%d" % i, [128, 512], BF16) for i in range(2)]
                    dotok = [Dep(), Dep()]
                    den = sb(ph2, "den", [128, 4], F32)
                    dden = Dep()
                    kq = 0

                    def pv_block(n):
                        ot, dot_ = otok[n % 2], dotok[n % 2]
                        for hq in range(2):
                            ps, dps = PSA.next()
                            for hh in range(4):
                                h = hq * 4 + hh
                                js = [j for j in (n - 1, n, n + 1) if 0 <= j < 16]
                                for ji, j in enumerate(js):
                                    c0 = (n - j + 1) * 128
                                    S.mm(ps[:, hh * 65:(hh + 1) * 65], et[j % 5][:, h, c0:c0 + 128], vtok[:, j, hq, :], ji == 0, ji == len(js) - 1,
                                         [det[j % 5], dvtok[j]], [dps])
                            pv = ps[:, 0:260].rearrange("p (a b) -> p a b", b=65)
                            S.tt("dve", den[:], pv[:, :, 64], esb[:, hq * 4:(hq + 1) * 4], ALU.add, [dps, desb], [dden])
                            S.recip(den[:], den[:], [dden], [dden])
                            S.tt("dve", ot[:, hq * 256:(hq + 1) * 256].rearrange("p (a b) -> p a b", b=64), pv[:, :, 0:64],
                                 den[:].unsqueeze(2).to_broadcast([128, 4, 64]), ALU.mult, [dps, dden], [dot_])
                        for fc in range(4):
                            S.tr(psb_t[:, fc * 128:(fc + 1) * 128], ot[:, fc * 128:(fc + 1) * 128], identb[:], [dot_, dcst["identb"]], [dpsb])
                        S.copy("act", yT[:, 4:8, n * 128:(n + 1) * 128], psb_t[:, 0:512].rearrange("p (a b) -> p a b", b=128), [dpsb],
                               [dyT[4 + i][n // 4] for i in range(4)])

                    for j in range(16):
                        e_t, de_t = et[j % 5], det[j % 5]
                        q0 = max(0, j - 1) * 128
                        q1 = min(16, j + 2) * 128
                        c0 = q0 - (j - 1) * 128
                        ncol = q1 - q0
                        for h in range(8):
                            kvh = h // 4
                            ch = h // 2
                            hf = h % 2
                            ksrc = kT if hf == kvh else kTs
                            ps, dps = PSM.next()
                            S.mm(ps[:, 0:ncol], ksrc[hf * 64:(hf + 1) * 64, j * 128:(j + 1) * 128], qT[hf * 64:(hf + 1) * 64, ch, q0:q1], True, True,
                                 [dkT[j // 4], dkTs] + [dqT[ch][b] for b in range(q0 // 512, (q1 - 1) // 512 + 1)], [dps])
                            lt, dlt = ltmp[kq % 2], dltmp[kq % 2]
                            kq += 1
                            S.tt("dve", lt[:, 0:ncol], ps[:, 0:ncol], bm[:, h, c0:c0 + ncol], ALU.add, [dps, dbm], [dlt])
                            S.act(e_t[:, h, c0:c0 + ncol], lt[:, 0:ncol], AF.Exp, [dlt], [de_t])
                        if j >= 2:
                            pv_block(j - 2)
                    pv_block(14)
                    pv_block(15)
                    S.barrier()
                    stage(S, 4)
                for half in range(2):
                    wt, dwt = load_w(w_view("ev_out_w", half * 512, (half + 1) * 512), 8, 512)
                    for d4 in range(4):
                        dc = half * 4 + d4
                        for tb in range(4):
                            tsl = slice(tb * 512, (tb + 1) * 512)
                            ps, dps = PSM.next()
                            for kc in range(8):
                                S.mm(ps[:], wt[:, kc, d4 * 128:(d4 + 1) * 128], yT[:, kc, tsl], kc == 0, kc == 7, [dwt, dyT[kc][tb]], [dps])
                            S.stt("dve", xT[:, dc, tsl], ps[:], modcol[0][:, 16 + dc:17 + dc], xT[:, dc, tsl], ALU.mult, ALU.add,
                                  [dps, dmod[0], dxT[dc][tb]], [dxT[dc][tb]])
                S.barrier()
          S.dead = False
          dump_x(0)

        if p0 <= 1 < p1:
            with ExitStack() as ph:
                modulate(ph, 0, 1, "b")

                def upd(po, dpo, dc, tb):
                    tsl = slice(tb * 512, (tb + 1) * 512)
                    S.stt("dve", xT[:, dc, tsl], po[:], modcol[0][:, 40 + dc:41 + dc], xT[:, dc, tsl], ALU.mult, ALU.add,
                          [dpo, dmod[0], dxT[dc][tb]], [dxT[dc][tb]])

                adab = make_adabufs(ph)
                nbq = list(range(12))

                def after_group():
                    for _ in range(2):
                        if nbq:
                            ada_block(1, nbq.pop(0), adab)

                swiglu(ph, lambda a, b: w_view("ev_ffn_w1", a, b), lambda a, b: w_view("ev_ffn_w3", a, b),
                       lambda f0, n: P["ev_ffn_w2"][f0 * 128:(f0 + n) * 128, :].rearrange("(j p) d -> p j d", p=128), 2816, upd, "f", after_group=after_group)
                while nbq:
                    ada_block(1, nbq.pop(0), adab)
                ada_finish(1)
                S.barrier()
            dump_x(1)

        if p0 <= 2 < p1:
            ssd_phase_inner()
            dump_x(2)

        if p0 <= 3 < p1:
            moe_phase_inner()
            dump_x(3)

        with ExitStack() as ph:
            ot = [sb(ph, "ot%d" % i, [128, 4, 1024], F32) for i in range(2)]
            dot = [Dep(), Dep()]
            dout = Dep()
            for tb in range(4):
                t, dt_ = ot[tb % 2], dot[tb % 2]
                for a in range(4):
                    for fq in range(2):
                        ps, dps = PSM.next()
                        for f4 in range(4):
                            fc = fq * 4 + f4
                            S.tr(ps[:, f4 * 128:(f4 + 1) * 128], xT[:, fc, tb * 512 + a * 128: tb * 512 + (a + 1) * 128], identf[:],
                                 [dxT[fc][tb], dcst["identf"]], [dps])
                        S.copy(evq.next(), t[:, a, fq * 512:(fq + 1) * 512], ps[:], [dps], [dt_])
                S.dma("sp", out_d[tb * 512:(tb + 1) * 512, :].rearrange("(a p) f -> p a f", p=128), t[:], reads=[dt_], writes=[dout])
            S.barrier()
        S.emit_all(block)
    nc._used_inputs = list(P.keys())
    return nc


def ssd_phase(nc, S, sb, P, C, L):
    raise NotImplementedError


def moe_phase(nc, S, sb, P, C, L):
    raise NotImplementedError


_CACHE = {}


def prep_inputs(inputs, b):
    m = {}
    f = lambda a: np.ascontiguousarray(np.asarray(a, dtype=np.float32))
    m["x"] = f(inputs["x"][b])
    m["c"] = f(inputs["c"][b]).reshape(8, 128)
    m["rel_bias"] = f(inputs["rel_bias"])
    for k, shp in PARAM_SHAPES.items():
        if k in m:
            continue
        m[k] = f(inputs[k][0]).reshape(shp)
    return m


def kernel(**inputs):
    if "nc" not in _CACHE:
        _CACHE["nc"] = build()
        _CACHE["consts"] = host_consts()
    nc = _CACHE["nc"]
    consts = _CACHE["consts"]
    in_maps = []
    for b in range(8):
        m = prep_inputs(inputs, b)
        m.update(consts)
        in_maps.append({k: m[k] for k in nc._used_inputs})
    res = run_bass_kernel_spmd(nc, in_maps, core_ids=list(range(8)))
    return np.stack([np.asarray(r["out"], dtype=np.float32) for r in res.results], 0)
```

```python
import numpy as np
import ml_dtypes
from contextlib import ExitStack
import concourse.bass as bass
import concourse.mybir as mybir
from concourse.bass_utils import run_bass_kernel_spmd

F32 = mybir.dt.float32
BF16 = mybir.dt.bfloat16
ALU = mybir.AluOpType
AF = mybir.ActivationFunctionType
ENGS = ("pe", "act", "dve", "pool", "sp")
S_LEN = 2048
D = 1024
EPS = 1e-6
NEG = -30000.0


class Dep:
    __slots__ = ("w", "r", "dsem", "dcnt", "wq")

    def __init__(self):
        self.wq = None
        self.w = None
        self.r = []
        self.dsem = None
        self.dcnt = 0


def deps(n):
    return [Dep() for _ in range(n)]


class Sched:
    def __init__(self, nc, stack):
        self.nc = nc
        self.stack = stack
        self.q = {e: [] for e in ENGS}
        self.cnt = {e: 0 for e in ENGS}
        self.sem = {e: stack.enter_context(nc.semaphore("s_" + e)) for e in ENGS}
        self.known = {e: {} for e in ENGS}
        self.same_wait = {"pe": False, "act": True, "dve": True, "pool": True, "sp": False}
        self.nsem = 0
        self.dma_deps = []
        self.dead = False

    def _waits(self, eng, reads, writes):
        evs = []
        for d in reads:
            if d.w is not None:
                evs.append(d.w)
        for d in writes:
            if d.w is not None:
                evs.append(d.w)
            evs.extend(d.r)
        waits = {}
        kn = self.known[eng]
        for (sem, val, e) in evs:
            if e == eng and not self.same_wait[eng]:
                continue
            if kn.get(id(sem), 0) >= val:
                continue
            cur = waits.get(id(sem))
            if cur is None or cur[1] < val:
                waits[id(sem)] = (sem, val)
        for k, (sem, val) in waits.items():
            kn[k] = val
        return list(waits.values())

    def op(self, eng, fn, reads=(), writes=()):
        if self.dead:
            return
        waits = self._waits(eng, reads, writes)
        self.cnt[eng] += 1
        sem = self.sem[eng]
        ev = (sem, self.cnt[eng], eng)

        def emit(e, waits=waits, fn=fn, sem=sem):
            for (s, v) in waits:
                e.wait_ge(s, v)
            fn(e).then_inc(sem, 1)

        self.q[eng].append(emit)
        for d in writes:
            d.w = ev
            d.r = []
        for d in reads:
            if d not in writes:
                d.r.append(ev)

    def dma(self, eng, out_ap, in_ap, reads=(), writes=(), **kw):
        if self.dead:
            return
        dst = writes[0]
        if dst.w is not None and dst.w[2] is None and dst.w[0] is dst.dsem and not dst.r and dst.wq == eng:
            waits = self._waits(eng, reads, ())
        else:
            waits = self._waits(eng, reads, writes)
        if dst.dsem is None:
            dst.dsem = self.stack.enter_context(self.nc.semaphore("d%d" % self.nsem))
            self.nsem += 1
            self.dma_deps.append(dst)
        dst.dcnt += 16
        dst.wq = eng
        ev = (dst.dsem, dst.dcnt, None)
        dsem = dst.dsem

        def emit(e, waits=waits, dsem=dsem, out_ap=out_ap, in_ap=in_ap, kw=kw):
            for (s, v) in waits:
                e.wait_ge(s, v)
            e.dma_start(out=out_ap, in_=in_ap, **kw).then_inc(dsem, 16)

        self.q[eng].append(emit)
        for d in writes:
            d.w = ev
            d.r = []
        for d in reads:
            d.r.append(ev)

    def barrier(self):
        if self.dead:
            return
        evs = [(self.sem[e], self.cnt[e]) for e in ENGS if self.cnt[e] > 0]
        evs += [(d.dsem, d.dcnt) for d in self.dma_deps]
        for eng in ENGS:
            kn = self.known[eng]
            waits = []
            for (sem, val) in evs:
                if sem is self.sem[eng]:
                    continue
                if kn.get(id(sem), 0) >= val:
                    continue
                kn[id(sem)] = val
                waits.append((sem, val))

            def emit(e, waits=waits):
                for (s, v) in waits:
                    e.wait_ge(s, v)

            self.q[eng].append(emit)

    def emit_all(self, block):
        m = {"pe": block.tensor, "act": block.scalar, "dve": block.vector, "pool": block.gpsimd, "sp": block.sync}
        for eng in ENGS:
            lst = self.q[eng]

            def body(e, lst=lst):
                for f in lst:
                    f(e)

            m[eng](body)

    def mm(self, out, lhsT, rhs, start, stop, r, w):
        self.op("pe", lambda e: e.matmul(out, lhsT=lhsT, rhs=rhs, start=start, stop=stop), r, w)

    def tr(self, out, in_, ident, r, w):
        self.op("pe", lambda e: e.transpose(out, in_, ident), r, w)

    def act(self, out, in_, func, r, w, bias=None, scale=None):
        kw = {}
        if bias is not None:
            kw["bias"] = bias
        if scale is not None:
            kw["scale"] = scale
        self.op("act", lambda e: e.activation(out=out, in_=in_, func=func, **kw), r, w)

    def copy(self, eng, out, in_, r, w):
        if eng == "act":
            self.op("act", lambda e: e.copy(out=out, in_=in_), r, w)
        else:
            self.op(eng, lambda e: e.tensor_copy(out=out, in_=in_), r, w)

    def tt(self, eng, out, in0, in1, op, r, w):
        self.op(eng, lambda e: e.tensor_tensor(out=out, in0=in0, in1=in1, op=op), r, w)

    def ts(self, eng, out, in0, s1, s2, op0, op1, r, w):
        if s2 is None:
            self.op(eng, lambda e: e.tensor_scalar(out=out, in0=in0, scalar1=s1, scalar2=None, op0=op0), r, w)
        else:
            self.op(eng, lambda e: e.tensor_scalar(out=out, in0=in0, scalar1=s1, scalar2=s2, op0=op0, op1=op1), r, w)

    def stt(self, eng, out, in0, scalar, in1, op0, op1, r, w):
        self.op(eng, lambda e: e.scalar_tensor_tensor(out=out, in0=in0, scalar=scalar, in1=in1, op0=op0, op1=op1), r, w)

    def memset(self, eng, ap, val, w):
        self.op(eng, lambda e: e.memset(ap, val), (), w)

    def recip(self, out, in_, r, w):
        self.op("dve", lambda e: e.reciprocal(out=out, in_=in_), r, w)


class _Stop(Exception):
    pass


STAGE = 99


def stage(S, k):
    if STAGE == k or (STAGE == 221 and k == 22):
        S.barrier()
        S.dead = True


class Ring:
    def __init__(self, items):
        self.items = items
        self.i = 0

    def next(self):
        it = self.items[self.i % len(self.items)]
        self.i += 1
        return it


def _bf(a):
    return np.ascontiguousarray(a.astype(np.float32)).astype(ml_dtypes.bfloat16)


def host_consts():
    c = {}
    c["identf"] = np.eye(128, dtype=np.float32)
    c["identb"] = _bf(np.eye(128))
    c["onesb"] = _bf(np.ones((128, 128)))
    c["onesf"] = np.ones((128, 128), np.float32)
    bd = np.zeros((128, 128), np.float32)
    bd[:64, :64] = 1
    bd[64:, 64:] = 1
    c["bd64"] = _bf(bd)
    k = np.arange(128)
    ang = 2 * np.pi * np.outer(k, k) / 128.0
    c["ccsc"] = _bf(np.concatenate([np.cos(ang), -np.sin(ang)], 1) / np.sqrt(128.0))
    n = np.arange(S_LEN)
    jk = np.outer(n, n) % S_LEN
    ang = 2 * np.pi * jk / float(S_LEN)
    c["cs_mat"] = _bf(np.cos(ang) / np.sqrt(float(S_LEN)))
    c["ss_mat"] = _bf(np.sin(ang) / np.sqrt(float(S_LEN)))
    i = np.arange(512)
    rel = 255 - i
    half, max_exact = 16, 8
    na = np.abs(rel)
    large = max_exact + (np.log(np.maximum(na, 1) / max_exact) / np.log(128 / max_exact) * (half - max_exact)).astype(np.int32)
    large = np.minimum(large, half - 1)
    bucket = (rel > 0).astype(np.int32) * half + np.where(na < max_exact, na, large)
    oh = np.zeros((32, 512), np.float32)
    oh[bucket, i] = 1.0
    oh[:, 511] = 0.0
    c["ohr"] = oh
    p = np.arange(128)[:, None]
    cc = np.arange(384)[None, :]
    relm = 128 + p - cc
    c["amask"] = np.where(np.abs(relm) <= 128, 0.0, NEG).astype(np.float32)
    s = np.arange(128)[:, None]
    l = np.arange(128)[None, :]
    c["trif"] = (s <= l).astype(np.float32)
    c["trib"] = (s >= l).astype(np.float32)
    c["maskf"] = _bf((l >= s).astype(np.float32))
    c["maskb"] = _bf((l <= s).astype(np.float32))
    c["negf"] = np.where(l >= s, 0.0, NEG).astype(np.float32)
    c["negb"] = np.where(l <= s, 0.0, NEG).astype(np.float32)
    sl = np.zeros((128, 128), np.float32)
    sl[127, :] = 1
    c["sellast"] = sl
    sf = np.zeros((128, 128), np.float32)
    sf[0, :] = 1
    c["selfirst"] = sf
    ee = np.zeros((8, 8, 128), np.float32)
    for e in range(8):
        ee[e, e, :] = 1
    c["esel"] = ee.reshape(8, 1024)
    return c


CONST_SHAPES = {
    "identf": ([128, 128], F32), "identb": ([128, 128], BF16), "onesb": ([128, 128], BF16), "onesf": ([128, 128], F32),
    "bd64": ([128, 128], BF16), "ccsc": ([128, 256], BF16), "cs_mat": ([2048, 2048], BF16), "ss_mat": ([2048, 2048], BF16),
    "ohr": ([32, 512], F32), "amask": ([128, 384], F32), "trif": ([128, 128], F32), "trib": ([128, 128], F32),
    "maskf": ([128, 128], BF16), "maskb": ([128, 128], BF16), "sellast": ([128, 128], F32), "selfirst": ([128, 128], F32),
    "esel": ([8, 1024], F32), "negf": ([128, 128], F32), "negb": ([128, 128], F32),
}

PARAM_SHAPES = {
    "x": [2048, 1024], "c": [8, 128], "rel_bias": [32, 8],
    "ev_ada_w": [1024, 6144], "ev_ada_b": [1, 6144], "ev_norm1_w": [8, 128], "ev_in_w": [1024, 1280],
    "ev_q_norm_w": [64, 1], "ev_k_norm_w": [64, 1], "ev_sink": [1, 8], "ev_out_w": [1024, 1024], "ev_norm2_w": [8, 128],
    "ev_ffn_w1": [1024, 2816], "ev_ffn_w3": [1024, 2816], "ev_ffn_w2": [2816, 1024],
    "od_ada_w": [1024, 6144], "od_ada_b": [1, 6144], "od_norm1_w": [8, 128], "od_in_w": [1024, 5184],
    "od_conv_w": [120, 128], "od_conv_b": [24, 128], "od_dt_bias_f": [1, 32], "od_dt_bias_b": [1, 32],
    "od_A_log_f": [1, 32], "od_A_log_b": [1, 32], "od_D": [1, 32], "od_gnorm_w": [16, 128], "od_out_w": [2048, 1024],
    "od_norm2_w": [8, 128], "od_router_w": [1024, 8], "od_router_b": [1, 8],
    "od_moe_w1": [8, 1024, 3584], "od_moe_w3": [8, 1024, 3584], "od_moe_w2": [8, 3584, 1024],
}


def build(p0=0, p1=4, dump=False):
    nc = bass.Bass("TRN2", target_bir_lowering=False)
    class _Lazy(dict):
        def __missing__(self, k):
            if k in PARAM_SHAPES:
                v = nc.dram_tensor(k, list(PARAM_SHAPES[k]), F32, kind="ExternalInput").ap()
            else:
                v = nc.dram_tensor(k, list(CONST_SHAPES[k][0]), CONST_SHAPES[k][1], kind="ExternalInput").ap()
            self[k] = v
            return v

    P = _Lazy()
    C = P
    out_d = nc.dram_tensor("out", [2048, 1024], F32, kind="ExternalOutput").ap()
    dump_d = [nc.dram_tensor("dump%d" % i, [2048, 1024], F32, kind="ExternalOutput").ap() for i in range(4)] if dump else None
    tscr = nc.dram_tensor("tscr", [8, 128, 512], F32, kind="Internal").ap()
    ygscr = nc.dram_tensor("ygscr", [2048, 2048], BF16, kind="ExternalOutput" if dump else "Internal").ap()
    acscr = nc.dram_tensor("acscr", [64, 2048], F32, kind="Internal").ap()
    xscr = nc.dram_tensor("xscr", [128, 8, 2048], F32, kind="Internal").ap()

    with ExitStack() as st:
        S = Sched(nc, st)

        def sb(stack, name, shape, dt):
            return stack.enter_context(nc.sbuf_tensor("sb_" + name, shape, dt))

        xT = sb(st, "xT", [128, 8, S_LEN], F32)
        dxT = [deps(4) for _ in range(8)]
        hT = sb(st, "hT", [128, 8, S_LEN], BF16)
        dhT = [deps(4) for _ in range(8)]
        cst = {}
        dcst = {}
        for k in ("identf", "identb", "onesb", "onesf", "bd64"):
            cst[k] = sb(st, "c_" + k, CONST_SHAPES[k][0], CONST_SHAPES[k][1])
            dcst[k] = Dep()
        modcol = [sb(st, "modcol%d" % i, [128, 48], F32) for i in range(2)]
        dmod = [Dep(), Dep()]
        pcol = sb(st, "pcol", [128, 72], F32)
        dpcol = Dep()
        acol = sb(st, "acol", [128, 4, 8], F32)
        dacol = Dep()
        cscol = sb(st, "cscol", [128, 8], F32)
        dcs = Dep()
        epsc = sb(st, "epsc", [128, 1], F32)
        depsc = Dep()
        wring_t = [sb(st, "wring%d" % i, [128, 8, 512], BF16) for i in range(3)]
        wring = Ring([(wring_t[i], Dep()) for i in range(3)])
        psf = [st.enter_context(nc.psum_tensor("psf%d" % i, [128, 512], F32)) for i in range(7)]
        psb_t = st.enter_context(nc.psum_tensor("psb", [128, 1024], BF16))
        dpsb = Dep()
        PSM = Ring([(psf[i], Dep()) for i in range(4)])
        PSA = Ring([(psf[i], Dep()) for i in range(4, 7)])
        block = st.enter_context(nc.Block())
        evq = Ring(["act", "dve"])
        WQ = Ring(["pool"])
        lastw = [None]

        identf, identb, onesb, onesf, bd64 = (cst[k] for k in ("identf", "identb", "onesb", "onesf", "bd64"))

        for k in cst:
            S.dma("sp", cst[k][:], C[k][:, :], writes=[dcst[k]])
        S.memset("dve", epsc[:], EPS, [depsc])

        akc = [0]

        def make_adabufs(stack):
            return dict(adat=[sb(stack, "adat%d_%d" % (akc[0], i), [128, 8, 512], F32) for i in range(2)], dadat=deps(2),
                        modrow=[sb(stack, "modrow%d_%d" % (akc[0], i), [1, 512], F32) for i in range(2)], dmr=deps(2),
                        brow=[sb(stack, "brow%d_%d" % (akc[0], i), [1, 512], F32) for i in range(2)], dbrow=deps(2), k=[0], tag=akc.__setitem__(0, akc[0] + 1))

        def ada_block(layer, nb, B_):
            nm = ("ev", "od")[layer]
            k = B_["k"][0]
            B_["k"][0] += 1
            t, dt_ = B_["adat"][k % 2], B_["dadat"][k % 2]
            mr, dmr_ = B_["modrow"][k % 2], B_["dmr"][k % 2]
            br, dbr_ = B_["brow"][k % 2], B_["dbrow"][k % 2]
            S.dma("act", br[:], P[nm + "_ada_b"][:, nb * 512:(nb + 1) * 512], writes=[dbr_])
            S.dma("sp", t[:], P[nm + "_ada_w"].rearrange("(kc p) f -> p kc f", p=128)[:, :, nb * 512:(nb + 1) * 512], writes=[dt_])
            ps, dps = PSA.next()
            for kc in range(8):
                S.mm(ps[0:1, :], cscol[:, kc:kc + 1], t[:, kc, :], kc == 0, kc == 7, [dcs, dt_], [dps])
            S.tt("dve", mr[:], ps[0:1, :], br[:], ALU.add, [dps, dbr_], [dmr_])
            pc, dpc = PSA.next()
            for j4 in range(4):
                S.mm(pc[:, j4:j4 + 1], mr[0:1, j4 * 128:(j4 + 1) * 128], onesf[0:1, 0:1], True, True, [dmr_, dcst["onesf"]], [dpc])
            S.copy("dve", modcol[layer][:, nb * 4:(nb + 1) * 4], pc[:, 0:4], [dpc], [dmod[layer]])

        def ada_finish(layer):
            for sub in range(2):
                S.stt("dve", acol[:, 2 * layer + sub, :], modcol[layer][:, 8 + 24 * sub:16 + 24 * sub], 1.0,
                      pcol[:, 16 * layer + 8 * sub:16 * layer + 8 * sub + 8], ALU.add, ALU.mult, [dmod[layer], dpcol], [dacol])

        with ExitStack() as ph:
            xst = [sb(ph, "xst%d" % i, [128, 4, 1024], F32) for i in range(2)]
            dxst = [Dep(), Dep()]
            for tb in range(4):
                t, dt_ = xst[tb % 2], dxst[tb % 2]
                S.dma("sp", t[:], P["x"][tb * 512:(tb + 1) * 512, :].rearrange("(a p) f -> p a f", p=128), writes=[dt_])
                for fc in range(8):
                    ps, dps = PSM.next()
                    for a in range(4):
                        S.tr(ps[:, a * 128:(a + 1) * 128], t[:, a, fc * 128:(fc + 1) * 128], identf[:], [dt_, dcst["identf"]], [dps])
                    S.copy(evq.next(), xT[:, fc, tb * 512:(tb + 1) * 512], ps[:], [dps], [dxT[fc][tb]])

            S.barrier()
        with ExitStack() as ph:
            c8 = sb(ph, "c8", [8, 128], F32)
            dc8 = Dep()
            S.dma("sp", c8[:], P["c"][:, :], writes=[dc8])
            ps, dps = PSA.next()
            S.tr(ps[:, 0:8], c8[:], identf[0:8, 0:8], [dc8, dcst["identf"]], [dps])
            S.act(cscol[:], ps[:, 0:8], AF.Silu, [dps], [dcs])
            defer_od = (p0 <= 1 < p1)
            adabufs = make_adabufs(ph)
            for layer in range(2):
                if layer == 1 and defer_od:
                    continue
                for nb in range(12):
                    ada_block(layer, nb, adabufs)
            prow = sb(ph, "prow", [72, 128], F32)
            dprow = Dep()
            for r0, nm in ((0, "ev_norm1_w"), (8, "ev_norm2_w"), (16, "od_norm1_w"), (24, "od_norm2_w"), (32, "od_gnorm_w"), (48, "od_conv_b")):
                n = PARAM_SHAPES[nm][0]
                S.dma("sp", prow[r0:r0 + n, :], P[nm][:, :], writes=[dprow])
            ps, dps = PSA.next()
            S.tr(ps[:, 0:72], prow[:], identf[0:72, 0:72], [dprow, dcst["identf"]], [dps])
            S.copy("dve", pcol[:], ps[:, 0:72], [dps], [dpcol])
            for layer in range(2):
                if layer == 1 and defer_od:
                    continue
                ada_finish(layer)
            S.barrier()

        def modulate(ph, layer, sub, tag, fp32_out=None):
            sq = [sb(ph, "sq%s%d" % (tag, i), [128, 512], BF16) for i in range(3)]
            dsq = deps(3)
            rstd = sb(ph, "rstd" + tag, [128, S_LEN], F32)
            drstd = deps(4)
            tmp = [sb(ph, "mtmp%s%d" % (tag, i), [128, 512], F32) for i in range(2)]
            dtmp = [Dep(), Dep()]
            bcol0 = 0 + 24 * sub
            k = 0
            for tb in range(4):
                tsl = slice(tb * 512, (tb + 1) * 512)
                ps, dps = PSA.next()
                for fc in range(8):
                    i = k % 3
                    k += 1
                    S.act(sq[i][:], xT[:, fc, tsl], AF.Square, [dxT[fc][tb]], [dsq[i]])
                    S.mm(ps[:], onesb[:], sq[i][:], fc == 0, fc == 7, [dsq[i], dcst["onesb"]], [dps])
                S.act(rstd[:, tsl], ps[:], AF.Sqrt, [dps, depsc], [drstd[tb]], bias=epsc[:, 0:1], scale=1.0 / D)
                S.recip(rstd[:, tsl], rstd[:, tsl], [drstd[tb]], [drstd[tb]])
            k = 0
            for tb in range(4):
                tsl = slice(tb * 512, (tb + 1) * 512)
                for fc in range(8):
                    t, dt_ = tmp[k % 2], dtmp[k % 2]
                    k += 1
                    S.stt("dve", t[:], xT[:, fc, tsl], acol[:, 2 * layer + sub, fc:fc + 1], rstd[:, tsl], ALU.mult, ALU.mult,
                          [dxT[fc][tb], dacol, drstd[tb]], [dt_])
                    S.act(hT[:, fc, tsl], t[:], AF.Identity, [dt_, dmod[layer]], [dhT[fc][tb]],
                          bias=modcol[layer][:, bcol0 + fc:bcol0 + fc + 1], scale=1.0)
                    if fp32_out is not None:
                        fp32_out(fc, tb, t, dt_, modcol[layer][:, bcol0 + fc:bcol0 + fc + 1])

        def dbg(name, tile, shape, dt_, reads):
            if not dump:
                return
            d_ = nc.dram_tensor("dbg_" + name, list(shape), dt_, kind="ExternalOutput").ap()
            S.dma("sp", d_, tile[:], reads=reads, writes=[Dep()])

        def load_w(dram_ap_3d, kc_n, ncols):
            t, dt_ = wring.next()
            rd = [lastw[0]] if lastw[0] is not None and lastw[0] is not dt_ else []
            for kc in range(kc_n):
                S.dma(WQ.next(), t[:, kc, 0:ncols], dram_ap_3d[:, kc, :], reads=rd, writes=[dt_])
            lastw[0] = dt_
            return t, dt_

        def w_view(name, c0, c1):
            return P[name].rearrange("(kc p) f -> p kc f", p=128)[:, :, c0:c1]

        def swiglu(ph, w1v, w3v, w2v, F, upd, tag, gate=None, after_group=None):
            if getattr(ph, "_sw", None) is None:
                gT = sb(ph, "gT" + tag, [128, 4, S_LEN], BF16)
                dgT = [deps(4) for _ in range(4)]
                sl_ = [sb(ph, "sil%s%d" % (tag, i), [128, 512], BF16) for i in range(2)]
                dsl = [Dep(), Dep()]
                ph._sw = (gT, dgT, sl_, dsl)
            gT, dgT, sl_, dsl = ph._sw
            nfc_tot = F // 128
            fc0 = 0
            k = 0
            while fc0 < nfc_tot:
                nfc = min(4, nfc_tot - fc0)
                w1t, dw1 = load_w(w1v(fc0 * 128, (fc0 + nfc) * 128), 8, nfc * 128)
                w3t, dw3 = load_w(w3v(fc0 * 128, (fc0 + nfc) * 128), 8, nfc * 128)
                for j in range(nfc):
                    for tb in range(4):
                        tsl = slice(tb * 512, (tb + 1) * 512)
                        p1, dp1 = PSM.next()
                        p3, dp3 = PSM.next()
                        for kc in range(8):
                            S.mm(p1[:], w1t[:, kc, j * 128:(j + 1) * 128], hT[:, kc, tsl], kc == 0, kc == 7, [dw1, dhT[kc][tb]], [dp1])
                        for kc in range(8):
                            S.mm(p3[:], w3t[:, kc, j * 128:(j + 1) * 128], hT[:, kc, tsl], kc == 0, kc == 7, [dw3, dhT[kc][tb]], [dp3])
                        s_, ds_ = sl_[k % 2], dsl[k % 2]
                        k += 1
                        S.act(s_[:], p1[:], AF.Silu, [dp1], [ds_])
                        if gate is None:
                            S.tt("dve", gT[:, j, tsl], p3[:], s_[:], ALU.mult, [dp3, ds_], [dgT[j][tb]])
                        else:
                            gb_, dgb_ = gate(tb)
                            S.tt("dve", gT[:, j, tsl], p3[:], gb_, ALU.mult, [dp3, dgb_], [dgT[j][tb]])
                            S.tt("dve", gT[:, j, tsl], gT[:, j, tsl], s_[:], ALU.mult, [ds_, dgT[j][tb]], [dgT[j][tb]])
                w2t = []
                for half in range(2):
                    w2t.append(load_w(w2v(fc0, nfc)[:, :, half * 512:(half + 1) * 512], nfc, 512))
                for dc in range(8):
                    wt, dwt = w2t[dc // 4]
                    for tb in range(4):
                        tsl = slice(tb * 512, (tb + 1) * 512)
                        po, dpo = PSA.next()
                        for j in range(nfc):
                            S.mm(po[:], wt[:, j, (dc % 4) * 128:(dc % 4 + 1) * 128], gT[:, j, tsl], j == 0, j == nfc - 1, [dwt, dgT[j][tb]], [dpo])
                        upd(po, dpo, dc, tb)
                if after_group is not None:
                    after_group()
                fc0 += nfc

        def dump_x(idx):
            if not dump:
                return
            with ExitStack() as dph:
                ot = [sb(dph, "dot%d_%d" % (idx, i), [128, 4, 1024], F32) for i in range(2)]
                dot = [Dep(), Dep()]
                dd = Dep()
                for tb in range(4):
                    t, dt_ = ot[tb % 2], dot[tb % 2]
                    for a in range(4):
                        for fq in range(2):
                            ps, dps = PSM.next()
                            for f4 in range(4):
                                fc = fq * 4 + f4
                                S.tr(ps[:, f4 * 128:(f4 + 1) * 128], xT[:, fc, tb * 512 + a * 128: tb * 512 + (a + 1) * 128], identf[:],
                                     [dxT[fc][tb], dcst["identf"]], [dps])
                            S.copy(evq.next(), t[:, a, fq * 512:(fq + 1) * 512], ps[:], [dps], [dt_])
                    S.dma("sp", dump_d[idx][tb * 512:(tb + 1) * 512, :].rearrange("(a p) f -> p a f", p=128), t[:], reads=[dt_], writes=[dd])
                S.barrier()


        def moe_phase_inner():
            with ExitStack() as ph:
                rw = sb(ph, "rw", [128, 8, 8], F32)
                drw = Dep()
                S.dma("sp", rw[:], P["od_router_w"].rearrange("(kc p) e -> p kc e", p=128), writes=[drw])
                rbb = sb(ph, "m_rbb", [128, 8], F32)
                drbb = Dep()
                S.dma("sp", rbb[:], bass.AP(tensor=P["od_router_b"].tensor, offset=0, ap=[[0, 128], [1, 8]]), writes=[drbb])
                esel = sb(ph, "esel", [8, 8, 128], F32)
                desel = Dep()
                S.dma("sp", esel[:], P["esel"].rearrange("k (e m) -> k e m", e=8), writes=[desel])
                logit = sb(ph, "logit", [128, 16, 8], F32)
                dlogit = deps(4)
                gtok = sb(ph, "gtok", [128, 16, 8], F32)
                dgtok = deps(4)
                gT8 = sb(ph, "gT8", [8, S_LEN], F32)
                dgT8 = deps(4)
                with ExitStack() as ph2:
                    hfa = sb(ph2, "hfa", [128, 8, 512], F32)
                    dhfa = deps(8)

                    def fp32_out(fc, tb, t, dt_, bcol):
                        S.ts("dve", hfa[:, fc, :], t[:], bcol, None, ALU.add, None, [dt_, dmod[1]], [dhfa[fc]])
                        if fc == 7:
                            pl, dpl = PSM.next()
                            for tt in range(4):
                                for f2 in range(8):
                                    S.mm(pl[:, tt * 8:(tt + 1) * 8], hfa[:, f2, tt * 128:(tt + 1) * 128], rw[:, f2, :], f2 == 0, f2 == 7, [dhfa[f2], drw], [dpl])
                        if fc == 7:
                            S.tt("dve", logit[:, tb * 4:(tb + 1) * 4, :], pl[:, 0:32].rearrange("p (a b) -> p a b", b=8),
                                 rbb[:].unsqueeze(1).to_broadcast([128, 4, 8]), ALU.add, [dpl, drbb], [dlogit[tb]])

                    modulate(ph2, 1, 1, "m", fp32_out=fp32_out)
                    top8 = sb(ph2, "top8", [128, 8], F32)
                    nm1 = sb(ph2, "nm1", [128, 1], F32)
                    ex = sb(ph2, "ex", [128, 8], F32)
                    e2 = sb(ph2, "e2", [128, 1], F32)
                    msk = sb(ph2, "msk", [128, 8], F32)
                    dtp = Dep()
                    for tt in range(16):
                        lg = logit[:, tt, :]
                        dl = dlogit[tt // 4]
                        S.op("dve", lambda e, lg=lg: e.max(out=top8[:], in_=lg), [dl], [dtp])
                        S.ts("dve", nm1[:], top8[:, 0:1], -1.0, None, ALU.mult, None, [dtp], [dtp])
                        S.act(ex[:], lg, AF.Exp, [dl, dtp], [dtp], bias=nm1[:, 0:1], scale=1.0)
                        S.act(e2[:], top8[:, 1:2], AF.Exp, [dtp], [dtp], bias=nm1[:, 0:1], scale=1.0)
                        S.ts("dve", e2[:], e2[:], 1.0, None, ALU.add, None, [dtp], [dtp])
                        S.recip(e2[:], e2[:], [dtp], [dtp])
                        S.ts("dve", msk[:], lg, top8[:, 1:2], None, ALU.is_ge, None, [dl, dtp], [dtp])
                        S.stt("dve", gtok[:, tt, :], ex[:], e2[:, 0:1], msk[:], ALU.mult, ALU.mult, [dtp], [dgtok[tt // 4]])
                    for tb in range(4):
                        ps, dps = PSA.next()
                        for t4 in range(4):
                            S.tr(ps[0:8, t4 * 128:(t4 + 1) * 128], gtok[:, tb * 4 + t4, :], identf[:], [dgtok[tb], dcst["identf"]], [dps])
                        S.copy("dve", gT8[:, tb * 512:(tb + 1) * 512], ps[0:8, :], [dps], [dgT8[tb]])
                    S.barrier()
                gbc = [sb(ph, "gbc%d" % i, [128, 512], F32) for i in range(8)]
                dgbc = deps(8)
                utmp = [sb(ph, "utmp%d" % i, [128, 512], F32) for i in range(2)]
                dutmp = [Dep(), Dep()]
                ucnt = [0]
                for e in range(8):
                    base = (e % 2) * 4
                    for tb in range(4):
                        ps, dps = PSA.next()
                        S.mm(ps[:], esel[:, e, :], gT8[:, tb * 512:(tb + 1) * 512], True, True, [desel, dgT8[tb]], [dps])
                        S.copy("act", gbc[base + tb][:], ps[:], [dps], [dgbc[base + tb]])

                    def upd(po, dpo, dc, tb, base=base):
                        tsl = slice(tb * 512, (tb + 1) * 512)
                        S.stt("dve", xT[:, dc, tsl], po[:], modcol[1][:, 40 + dc:41 + dc], xT[:, dc, tsl], ALU.mult, ALU.add,
                              [dpo, dmod[1], dxT[dc][tb]], [dxT[dc][tb]])

                    def gate(tb, base=base):
                        return gbc[base + tb][:], dgbc[base + tb]

                    w1e = P["od_moe_w1"][e].rearrange("(kc p) f -> p kc f", p=128)
                    w3e = P["od_moe_w3"][e].rearrange("(kc p) f -> p kc f", p=128)
                    w2e = P["od_moe_w2"][e]
                    swiglu(ph, lambda a, b, w=w1e: w[:, :, a:b], lambda a, b, w=w3e: w[:, :, a:b],
                           lambda f0, n, w=w2e: w[f0 * 128:(f0 + n) * 128, :].rearrange("(j p) d -> p j d", p=128), 3584, upd, "m%d" % e, gate=gate)
                S.barrier()


        def ssd_main(ssacc, dssacc):
            with ExitStack() as ph:
                with ExitStack() as ph2:
                    modulate(ph2, 1, 0, "s")
                    dxscr = Dep()
                    for fc in range(8):
                        S.dma("sp", xscr[:, fc, :], xT[:, fc, :], reads=dxT[fc], writes=[dxscr])
                    S.barrier()
                cwcol = sb(ph, "cwcol", [128, 120], F32)
                dcw = Dep()
                dtb = sb(ph, "dtb", [128, 64], F32)
                abc = sb(ph, "abc", [128, 64], F32)
                dbc = sb(ph, "dbc", [128, 32], F32)
                dprm = Dep()
                cst2 = {}
                for k in ("trif", "trib", "sellast", "selfirst", "negf", "negb"):
                    cst2[k] = sb(ph, "c_" + k, [128, 128], F32)
                wdtt = sb(ph, "wdtt", [128, 8, 64], BF16)
                dwdt = Dep()
                for kc in range(8):
                    S.dma("pool", wdtt[:, kc, :], w_view("od_in_w", 5120, 5184)[:, kc, :], writes=[dwdt])
                dcst2 = Dep()
                for k in cst2:
                    S.dma("sp", cst2[k][:], P[k][:, :], writes=[dcst2])
                with ExitStack() as ph2:
                    cwrow = sb(ph2, "cwrow", [120, 128], F32)
                    dcwr = Dep()
                    S.dma("sp", cwrow[:], P["od_conv_w"][:, :], writes=[dcwr])
                    ps, dps = PSA.next()
                    S.tr(ps[:, 0:120], cwrow[:], identf[0:120, 0:120], [dcwr, dcst["identf"]], [dps])
                    S.copy("dve", cwcol[:], ps[:, 0:120], [dps], [dcw])
                    bc = lambda nm, n: bass.AP(tensor=P[nm].tensor, offset=0, ap=[[0, 128], [1, n]])
                    S.dma("sp", dtb[:, 0:32], bc("od_dt_bias_f", 32), writes=[dprm])
                    S.dma("sp", dtb[:, 32:64], bc("od_dt_bias_b", 32), writes=[dprm])
                    S.dma("sp", abc[:, 0:32], bc("od_A_log_f", 32), writes=[dprm])
                    S.dma("sp", abc[:, 32:64], bc("od_A_log_b", 32), writes=[dprm])
                    S.dma("sp", dbc[:], bc("od_D", 32), writes=[dprm])
                    S.act(abc[:], abc[:], AF.Exp, [dprm], [dprm])
                    S.ts("dve", abc[:], abc[:], -1.0, None, ALU.mult, None, [dprm], [dprm])
                    S.barrier()
                def _dtset(i):
                    if i == 0:
                        return [sb(ph, "dtset0_%d" % k, [128, 16, 16], F32) for k in range(5)]
                    return [xT[:, 6, k * 256:(k + 1) * 256].rearrange("p (a b) -> p a b", b=16) for k in range(5)]
                dtsets = [_dtset(0), _dtset(1)]
                ddts = deps(2)
                dacscrs = deps(2)
                ddtmp = Dep()
                a_tok = sb(ph, "a_tok", [128, 16, 16], F32)
                acT = [sb(ph, "acT%d" % i, [16, 512], F32) for i in range(2)]
                dacT = deps(2)
                tmpd = sb(ph, "tmpd", [128, 16], F32)
                dtmpd = Dep()
                dtbg = sb(ph, "dtbg", [128, 16], F32)
                abcg = sb(ph, "abcg", [128, 16], F32)

                def dt_path(g):
                    dt_tok, acum, ea, dec, cdb = dtsets[g % 2]
                    ddt = ddts[g % 2]
                    dacscr = dacscrs[g % 2]
                    r0 = (g % 2) * 16
                    for d in range(2):
                        S.copy("dve", dtbg[:, d * 8:(d + 1) * 8], dtb[:, d * 32 + g * 8:d * 32 + g * 8 + 8], [dprm], [ddtmp])
                        S.copy("dve", abcg[:, d * 8:(d + 1) * 8], abc[:, d * 32 + g * 8:d * 32 + g * 8 + 8], [dprm], [ddtmp])
                    for tt in range(16):
                        ps, dps = PSM.next()
                        for d in range(2):
                            for kc in range(8):
                                S.mm(ps[:, d * 8:(d + 1) * 8], hT[:, kc, tt * 128:(tt + 1) * 128], wdtt[:, kc, d * 32 + g * 8:d * 32 + g * 8 + 8], kc == 0, kc == 7,
                                     [dhT[kc][tt // 4], dwdt], [dps])
                        S.tt("dve", tmpd[:], ps[:, 0:16], dtbg[:], ALU.add, [dps, ddtmp], [dtmpd])
                        S.act(tmpd[:], tmpd[:], AF.Exp, [dtmpd], [dtmpd])
                        S.act(dt_tok[:, tt, :], tmpd[:], AF.Ln, [dtmpd, dcst["onesf"]], [ddt], bias=onesf[:, 0:1], scale=1.0)
                        S.tt("dve", a_tok[:, tt, :], dt_tok[:, tt, :], abcg[:], ALU.mult, [ddt, ddtmp], [ddtmp])
                    for c in range(16):
                        ps, dps = PSM.next()
                        S.mm(ps[:, 0:8], cst2["trif"][:], a_tok[:, c, 0:8], True, True, [dcst2, ddtmp], [dps])
                        S.mm(ps[:, 8:16], cst2["trib"][:], a_tok[:, c, 8:16], True, True, [dcst2, ddtmp], [dps])
                        S.copy("dve", acum[:, c, :], ps[:, 0:16], [dps], [ddt])
                        S.act(ea[:, c, :], ps[:, 0:16], AF.Exp, [dps], [ddt])
                        p2, dp2 = PSM.next()
                        S.mm(p2[:, 0:8], cst2["sellast"][:], acum[:, c, 0:8], True, True, [dcst2, ddt], [dp2])
                        S.mm(p2[:, 8:16], cst2["selfirst"][:], acum[:, c, 8:16], True, True, [dcst2, ddt], [dp2])
                        S.act(cdb[:, c, :], p2[:, 0:16], AF.Exp, [dp2], [ddt])
                        S.tt("dve", tmpd[:], p2[:, 0:16], acum[:, c, :], ALU.subtract, [dp2, ddt], [dtmpd])
                        S.act(dec[:, c, :], tmpd[:], AF.Exp, [dtmpd], [ddt])
                    for c4 in range(4):
                        ps, dps = PSA.next()
                        for c1 in range(4):
                            c = c4 * 4 + c1
                            S.tr(ps[0:16, c1 * 128:(c1 + 1) * 128], acum[:, c, :], identf[:], [ddt, dcst["identf"]], [dps])
                        S.copy("dve", acT[c4 % 2][:], ps[0:16, :], [dps], [dacT[c4 % 2]])
                        S.dma("sp", acscr[r0:r0 + 16, c4 * 512:(c4 + 1) * 512], acT[c4 % 2][:], reads=[dacT[c4 % 2]], writes=[dacscr])

                raw = sb(ph, "craw", [128, S_LEN + 4], BF16)
                draw = Dep()
                S.memset("pool", raw[:, 0:2], 0.0, [draw])
                S.memset("pool", raw[:, S_LEN + 2:S_LEN + 4], 0.0, [draw])

                dgs = [xT[:, 4, i * 320:(i + 1) * 320].bitcast(BF16).rearrange("p (a b) -> p a b", b=128) for i in range(2)]
                ddgs = deps(2)
                kdg = [0]

                def conv_chunk(wt, dwt, wcol0, cchunk, dst_ap, ddst):
                    i = kdg[0] % 2
                    kdg[0] += 1
                    dg, ddg = dgs[i], ddgs[i]
                    for w in range(5):
                        S.ts("dve", dg[:, w, :], identb[:], cwcol[:, w * 24 + cchunk:w * 24 + cchunk + 1], None, ALU.mult, None, [dcst["identb"], dcw], [ddg])
                    for tb in range(4):
                        tsl = slice(tb * 512, (tb + 1) * 512)
                        ps, dps = PSM.next()
                        for kc in range(8):
                            S.mm(ps[:], wt[:, kc, wcol0:wcol0 + 128], hT[:, kc, tsl], kc == 0, kc == 7, [dwt, dhT[kc][tb]], [dps])
                        S.copy("act", raw[:, 2 + tb * 512:2 + (tb + 1) * 512], ps[:], [dps], [draw])
                    for tb in range(4):
                        tsl = slice(tb * 512, (tb + 1) * 512)
                        pc, dpc = PSM.next()
                        for w in range(5):
                            S.mm(pc[:], dg[:, w, :], raw[:, w + tb * 512:w + tb * 512 + 512], w == 0, w == 4, [ddg, draw], [dpc])
                        S.act(dst_ap[:, tsl], pc[:], AF.Silu, [dpc, dpcol], ddst, bias=pcol[:, 48 + cchunk:49 + cchunk], scale=1.0)

                BT = sb(ph, "BT", [128, S_LEN], BF16)
                CT = sb(ph, "CT", [128, S_LEN], BF16)
                dBT, dCT = Dep(), Dep()
                Btok = sb(ph, "Btok", [128, 16, 128], BF16)
                dBtok = Dep()
                cbt = sb(ph, "cbt", [128, 16, 128], BF16)
                dcbt = Dep()
                xsT = sb(ph, "xsT", [128, S_LEN], BF16)
                dxsT = Dep()
                Xd = [sb(ph, "Xd%d" % i, [128, 16, 128], BF16) for i in range(2)]
                dXd = Dep()
                yaccs = [xT[:, 1, :].rearrange("p (a b) -> p a b", b=128), xT[:, 5, :].rearrange("p (a b) -> p a b", b=128)]
                dyaccs = [deps(16), deps(16)]
                arow = [xT[:, 0, i * 1024:(i + 1) * 1024].rearrange("p (a b) -> p a b", b=512) for i in range(2)]
                darow = deps(2)
                Hsr = [xT[:, 3, i * 128:(i + 1) * 128] for i in range(8)]
                dHsr = deps(8)
                lt = [sb(ph, "lt%d" % i, [128, 2, 512], BF16) for i in range(2)]
                dlt = deps(2)
                S_all = sb(ph, "S_all", [128, 2, 16, 128], BF16)
                dSall = [deps(4) for _ in range(2)]
                Hb_all = sb(ph, "Hb_all", [128, 2, 16, 128], BF16)
                dHball = [deps(16) for _ in range(2)]
                S.memset("dve", Hb_all[:, 0, 0, :], 0.0, [dHball[0][0]])
                S.memset("dve", Hb_all[:, 1, 15, :], 0.0, [dHball[1][15]])
                xdr = [sb(ph, "xdr%d" % i, [128, 128], BF16) for i in range(4)]
                dxdr = deps(4)
                szt2 = [sb(ph, "szt%d" % i, [128, 512], BF16) for i in range(2)]
                ygf2 = [xT[:, 3, 1024 + i * 512:1024 + (i + 1) * 512] for i in range(2)]
                sqg2 = [sb(ph, "sqg%d" % i, [128, 512], BF16) for i in range(2)]
                dgate2 = [deps(3) for _ in range(2)]
                ygb = [xT[:, 4, 1024 + i * 256:1024 + (i + 1) * 256].bitcast(BF16) for i in range(2)]
                dygb = deps(2)
                dygscr = Dep()
                kit = [0]
                kyg = [0]
                kxd = [0]
                khs = [0]

                def prep_conv(g, j, wx, dwx):
                    conv_chunk(wx, dwx, j * 128, g * 4 + j, xsT, [dxsT])

                def prep_evac(g, j, yacc, dyacc):
                    dt_tok = dtsets[g % 2][0]
                    ddt = ddts[g % 2]
                    fcx = g * 4 + j
                    h0 = 8 * g + 2 * j
                    for t8 in range(2):
                        for t1_ in range(8):
                            tt = t8 * 8 + t1_
                            S.tr(psb_t[:, t1_ * 128:(t1_ + 1) * 128], xsT[:, tt * 128:(tt + 1) * 128], identb[:], [dxsT, dcst["identb"]], [dpsb])
                        for t1_ in range(8):
                            tt = t8 * 8 + t1_
                            src = psb_t[:, t1_ * 128:(t1_ + 1) * 128].rearrange("p (a b) -> p a b", b=64)
                            for d in range(2):
                                hd0 = d * 8 + 2 * j
                                S.tt("dve", Xd[d][:, tt, :].rearrange("p (a b) -> p a b", b=64), src,
                                     dt_tok[:, tt, hd0:hd0 + 2].unsqueeze(2).to_broadcast([128, 2, 64]), ALU.mult, [dpsb, ddt], [dXd])
                            S.tt("dve", yacc[:, tt, :].rearrange("p (a b) -> p a b", b=64), src,
                                 dbc[:, h0:h0 + 2].unsqueeze(2).to_broadcast([128, 2, 64]), ALU.mult, [dpsb, dprm], [dyacc[tt]])
                    if fcx == 0:
                        dbg("xsT", xsT, [128, S_LEN], BF16, [dxsT])
                        dbg("Xf", Xd[0], [128, 16, 128], BF16, [dXd])
                        dbg("Xb", Xd[1], [128, 16, 128], BF16, [dXd])
                        dbg("Btok", Btok, [128, 16, 128], BF16, [dBtok])
                        dbg("cbt", cbt, [128, 16, 128], BF16, [dcbt])

                def phase_a(g, j, yacc, dyacc):
                    dt_tok, acum, ea, dec, cdb = dtsets[g % 2]
                    ddt = ddts[g % 2]
                    dacscr = dacscrs[g % 2]
                    r0 = (g % 2) * 16

                    def stage1(d, b4):
                        hd0 = d * 8 + 2 * j
                        negm = cst2["negf" if d == 0 else "negb"]
                        c0 = b4 * 4
                        k = kit[0]
                        kit[0] += 1
                        ar, dar = arow[k % 2], darow[k % 2]
                        lt_, dlt_ = lt[k % 2], dlt[k % 2]
                        for hh in range(2):
                            S.dma("sp", ar[:, hh, :], bass.AP(tensor=acscr.tensor, offset=(r0 + hd0 + hh) * S_LEN + c0 * 128, ap=[[0, 128], [1, 512]]),
                                  reads=[dacscr], writes=[dar])
                        for hh in range(2):
                            av = ar[:, hh, :].rearrange("p (a b) -> p a b", b=128)
                            S.tt("dve", av, av, acum[:, c0:c0 + 4, hd0 + hh:hd0 + hh + 1].to_broadcast([128, 4, 128]), ALU.subtract, [dar, ddt], [dar])
                        av8 = ar[:].rearrange("p h (a b) -> p (h a) b", b=128)
                        S.tt("dve", av8, av8, negm[:].unsqueeze(1).to_broadcast([128, 8, 128]), ALU.min, [dar, dcst2], [dar])
                        S.act(lt_[:], ar[:], AF.Exp, [dar], [dlt_])
                        return (d, b4, lt_, dlt_)

                    def stage2(st_):
                        d, b4, lt_, dlt_ = st_
                        hd0 = d * 8 + 2 * j
                        c0 = b4 * 4
                        S.tt("dve", lt_[:], lt_[:], cbt[:, c0:c0 + 4, :].rearrange("p a b -> p (a b)").unsqueeze(1).to_broadcast([128, 2, 512]), ALU.mult,
                             [dlt_, dcbt], [dlt_])
                        p1, dp1 = PSM.next()
                        for c in range(4):
                            for hh in range(2):
                                S.mm(p1[:, c * 128 + hh * 64:c * 128 + (hh + 1) * 64], lt_[:, hh, c * 128:(c + 1) * 128],
                                     Xd[d][:, c0 + c, hh * 64:(hh + 1) * 64], True, True, [dlt_, dXd], [dp1])
                        p3, dp3 = PSA.next()
                        for c in range(4):
                            kx = kxd[0] % 4
                            kxd[0] += 1
                            S.tt("pool", xdr[kx][:].rearrange("p (a b) -> p a b", b=64), Xd[d][:, c0 + c, :].rearrange("p (a b) -> p a b", b=64),
                                 dec[:, c0 + c, hd0:hd0 + 2].unsqueeze(2).to_broadcast([128, 2, 64]), ALU.mult, [dXd, ddt], [dxdr[kx]])
                            S.mm(p3[:, c * 128:(c + 1) * 128], Btok[:, c0 + c, :], xdr[kx][:], True, True, [dBtok, dxdr[kx]], [dp3])
                        S.tt("dve", yacc[:, c0:c0 + 4, :], p1[:].rearrange("p (a b) -> p a b", b=128), yacc[:, c0:c0 + 4, :], ALU.add,
                             [dp1] + dyacc[c0:c0 + 4], dyacc[c0:c0 + 4])
                        S.copy("act", S_all[:, d, c0:c0 + 4, :], p3[:].rearrange("p (a b) -> p a b", b=128), [dp3], [dSall[d][b4]])

                    prev_st = None
                    for (d, b4) in [(d, b4) for d in range(2) for b4 in range(4)]:
                        st_ = stage1(d, b4)
                        if prev_st is not None:
                            stage2(prev_st)
                        prev_st = st_
                    stage2(prev_st)

                def phase_b(g, j):
                    dt_tok, acum, ea, dec, cdb = dtsets[g % 2]
                    ddt = ddts[g % 2]
                    hprev = [None, None]
                    for step in range(16):
                        for d in range(2):
                            hd0 = d * 8 + 2 * j
                            c = step if d == 0 else 15 - step
                            kh = khs[0] % 8
                            khs[0] += 1
                            hn, dhn = Hsr[kh], dHsr[kh]
                            if step == 0:
                                S.copy("dve", hn, S_all[:, d, c, :], [dSall[d][c // 4]], [dhn])
                            else:
                                hp, dhp = hprev[d]
                                for hh in range(2):
                                    S.stt("dve", hn[:, hh * 64:(hh + 1) * 64], hp[:, hh * 64:(hh + 1) * 64], cdb[:, c, hd0 + hh:hd0 + hh + 1],
                                          S_all[:, d, c, hh * 64:(hh + 1) * 64], ALU.mult, ALU.add, [dhp, ddt, dSall[d][c // 4]], [dhn])
                            if step < 15:
                                cn = c + 1 if d == 0 else c - 1
                                S.copy("act", Hb_all[:, d, cn, :], hn, [dhn], [dHball[d][cn]])
                            hprev[d] = (hn, dhn)

                def phase_c(g, j, yacc, dyacc):
                    dt_tok, acum, ea, dec, cdb = dtsets[g % 2]
                    ddt = ddts[g % 2]
                    for d in range(2):
                        hd0 = d * 8 + 2 * j
                        for b4 in range(4):
                            c0 = b4 * 4
                            p2, dp2 = PSM.next()
                            for c in range(4):
                                csl = slice((c0 + c) * 128, (c0 + c + 1) * 128)
                                S.mm(p2[:, c * 128:(c + 1) * 128], CT[:, csl], Hb_all[:, d, c0 + c, :], True, True, [dCT, dHball[d][c0 + c]], [dp2])
                            for c in range(4):
                                for hh in range(2):
                                    ysl = yacc[:, c0 + c, hh * 64:(hh + 1) * 64]
                                    S.stt("dve", ysl, p2[:, c * 128 + hh * 64:c * 128 + (hh + 1) * 64], ea[:, c0 + c, hd0 + hh:hd0 + hh + 1], ysl,
                                          ALU.mult, ALU.add, [dp2, ddt, dyacc[c0 + c]], [dyacc[c0 + c]])

                def gating(g, j, wz, dwz, yacc, dyacc):
                    fcx = g * 4 + j
                    if fcx == 0:
                        dbg("yacc", xT[:, 1, :], [128, S_LEN], F32, dyacc)

                    def g1(tb):
                        tsl = slice(tb * 512, (tb + 1) * 512)
                        py, dpy = PSM.next()
                        for t4 in range(4):
                            tt = tb * 4 + t4
                            S.tr(py[:, t4 * 128:(t4 + 1) * 128], yacc[:, tt, :], identf[:], [dyacc[tt], dcst["identf"]], [dpy])
                        pz, dpz = PSM.next()
                        for kc in range(8):
                            S.mm(pz[:], wz[:, kc, j * 128:(j + 1) * 128], hT[:, kc, tsl], kc == 0, kc == 7, [dwz, dhT[kc][tb]], [dpz])
                        ig = kyg[0] % 2
                        kyg[0] += 1
                        szt, ygf, sqg = szt2[ig], ygf2[ig], sqg2[ig]
                        dsz_, dyg_, dsq_ = dgate2[ig]
                        S.act(szt[:], pz[:], AF.Silu, [dpz], [dsz_])
                        S.tt("dve", ygf, py[:], szt[:], ALU.mult, [dpy, dsz_], [dyg_])
                        S.act(sqg[:], ygf, AF.Square, [dyg_], [dsq_])
                        return (tb, ig)

                    def g2(st_):
                        tb, ig = st_
                        tsl = slice(tb * 512, (tb + 1) * 512)
                        szt, ygf, sqg = szt2[ig], ygf2[ig], sqg2[ig]
                        dsz_, dyg_, dsq_ = dgate2[ig]
                        pq, dpq = PSA.next()
                        S.mm(pq[:], onesb[:], sqg[:], True, True, [dsq_, dcst["onesb"]], [dpq])
                        if fcx == 0:
                            S.copy("dve", ssacc[:, tsl], pq[:], [dpq], [dssacc[tb]])
                        else:
                            S.tt("dve", ssacc[:, tsl], pq[:], ssacc[:, tsl], ALU.add, [dpq, dssacc[tb]], [dssacc[tb]])
                        S.ts("dve", ygb[ig][:], ygf, pcol[:, 32 + fcx:33 + fcx], None, ALU.mult, None, [dyg_, dpcol], [dygb[ig]])
                        S.dma("sp", ygscr[fcx * 128:(fcx + 1) * 128, tsl], ygb[ig][:], reads=[dygb[ig]], writes=[dygscr])

                    prev = None
                    for tb in range(4):
                        st_ = g1(tb)
                        if prev is not None:
                            g2(prev)
                        prev = st_
                    g2(prev)

                BTs = [BT, xT[:, 2, 0:1024].bitcast(BF16)]
                CTs = [CT, xT[:, 2, 1024:2048].bitcast(BF16)]
                Btoks = [Btok, xT[:, 7, 0:1024].bitcast(BF16).rearrange("p (a b) -> p a b", b=128)]
                cbts = [cbt, xT[:, 7, 1024:2048].bitcast(BF16).rearrange("p (a b) -> p a b", b=128)]
                dBTs, dCTs, dBtoks, dcbts = deps(2), deps(2), deps(2), deps(2)
                (wzt, dwz), (wxt, dwx), (wbct, dwbc) = wring.items

                def load_into(t, dt_, view, ncols, col0=0):
                    for kc in range(8):
                        S.dma("pool", t[:, kc, col0:col0 + ncols], view[:, kc, :], writes=[dt_])

                def group_prologue(g):
                    s_ = g % 2
                    load_into(wxt, dwx, w_view("od_in_w", 2048 + g * 512, 2048 + (g + 1) * 512), 512)
                    load_into(wbct, dwbc, w_view("od_in_w", 4096 + g * 128, 4096 + (g + 1) * 128), 128, 0)
                    load_into(wbct, dwbc, w_view("od_in_w", 4608 + g * 128, 4608 + (g + 1) * 128), 128, 128)
                    conv_chunk(wbct, dwbc, 0, 16 + g, BTs[s_], [dBTs[s_]])
                    conv_chunk(wbct, dwbc, 128, 20 + g, CTs[s_], [dCTs[s_]])
                    if g == 0:
                        dbg("BT", BTs[0], [128, S_LEN], BF16, [dBTs[0]])
                        dbg("CT", CTs[0], [128, S_LEN], BF16, [dCTs[0]])
                    for t8 in range(2):
                        for t1_ in range(8):
                            tt = t8 * 8 + t1_
                            S.tr(psb_t[:, t1_ * 128:(t1_ + 1) * 128], BTs[s_][:, tt * 128:(tt + 1) * 128], identb[:], [dBTs[s_], dcst["identb"]], [dpsb])
                        S.copy("act", Btoks[s_][:, t8 * 8:(t8 + 1) * 8, :], psb_t[:, :].rearrange("p (a b) -> p a b", b=128), [dpsb], [dBtoks[s_]])
                    for c in range(16):
                        ps, dps = PSM.next()
                        csl = slice(c * 128, (c + 1) * 128)
                        S.mm(ps[:, 0:128], BTs[s_][:, csl], CTs[s_][:, csl], True, True, [dBTs[s_], dCTs[s_]], [dps])
                        S.copy("act", cbts[s_][:, c, :], ps[:, 0:128], [dps], [dcbts[s_]])

                dt_path(0)
                group_prologue(0)
                for g in range(4):
                    s_ = g % 2
                    BT, CT, Btok, cbt = BTs[s_], CTs[s_], Btoks[s_], cbts[s_]
                    dBT, dCT, dBtok, dcbt = dBTs[s_], dCTs[s_], dBtoks[s_], dcbts[s_]
                    if g == 0:
                        for nm_, t_ in zip(("dt", "acum", "ea", "dec", "cdb"), dtsets[0]):
                            dbg(nm_, t_, [128, 16, 16], F32, [ddts[0]])
                    load_into(wzt, dwz, w_view("od_in_w", g * 512, (g + 1) * 512), 512)
                    prep_conv(g, 0, wxt, dwx)
                    prep_evac(g, 0, yaccs[0], dyaccs[0])
                    for j in range(4):
                        ya, dya = yaccs[j % 2], dyaccs[j % 2]
                        phase_a(g, j, ya, dya)
                        if j < 3:
                            prep_conv(g, j + 1, wxt, dwx)
                        if j == 2 and g < 3:
                            group_prologue(g + 1)
                        phase_b(g, j)
                        if j < 3:
                            prep_evac(g, j + 1, yaccs[(j + 1) % 2], dyaccs[(j + 1) % 2])
                        elif g < 3:
                            dt_path(g + 1)
                        phase_c(g, j, ya, dya)
                        gating(g, j, wzt, dwz, ya, dya)
                S.barrier()
                for fc in range(8):
                    S.dma("sp", xT[:, fc, :], xscr[:, fc, :], reads=[], writes=dxT[fc])

        def ssd_out(ssacc, dssacc):
            with ExitStack() as ph2:
                rs = sb(ph2, "rs_o", [128, S_LEN], F32)
                drs = Dep()
                S.act(rs[:], ssacc[:], AF.Sqrt, dssacc + [depsc], [drs], bias=epsc[:, 0:1], scale=1.0 / 2048)
                S.recip(rs[:], rs[:], [drs], [drs])
                dbg("rs", rs, [128, S_LEN], F32, [drs])
                ygt = [sb(ph2, "ygt%d" % i, [128, 16, 512], BF16) for i in range(2)]
                dygt = deps(2)
                ot1 = [sb(ph2, "ot1_%d" % i, [128, 512], F32) for i in range(2)]
                dot1 = deps(2)
                ko = 0
                for tb in range(4):
                    tsl = slice(tb * 512, (tb + 1) * 512)
                    yt, dyt = ygt[tb % 2], dygt[tb % 2]
                    for kc in range(16):
                        S.dma("sp", yt[:, kc, :], ygscr[kc * 128:(kc + 1) * 128, tsl], reads=[], writes=[dyt])
                    for half in range(2):
                        wa, dwa = load_w(P["od_out_w"][0:1024, half * 512:(half + 1) * 512].rearrange("(kc p) f -> p kc f", p=128), 8, 512)
                        wb_, dwb_ = load_w(P["od_out_w"][1024:2048, half * 512:(half + 1) * 512].rearrange("(kc p) f -> p kc f", p=128), 8, 512)
                        for d4 in range(4):
                            dc = half * 4 + d4
                            ps, dps = PSM.next()
                            for kc in range(16):
                                wt_, dwt_ = (wa, dwa) if kc < 8 else (wb_, dwb_)
                                S.mm(ps[:], wt_[:, kc % 8, d4 * 128:(d4 + 1) * 128], yt[:, kc, :], kc == 0, kc == 15, [dwt_, dyt], [dps])
                            o1, do1 = ot1[ko % 2], dot1[ko % 2]
                            ko += 1
                            S.tt("dve", o1[:], ps[:], rs[:, tsl], ALU.mult, [dps, drs], [do1])
                            S.stt("dve", xT[:, dc, tsl], o1[:], modcol[1][:, 16 + dc:17 + dc], xT[:, dc, tsl], ALU.mult, ALU.add,
                                  [do1, dmod[1], dxT[dc][tb]], [dxT[dc][tb]])
                S.barrier()


        def ssd_phase_inner():
            with ExitStack() as pho:
                ssacc = sb(pho, "ssacc", [128, S_LEN], F32)
                dssacc = deps(4)
                ssd_main(ssacc, dssacc)
                ssd_out(ssacc, dssacc)

        if p0 <= 0 < p1:
          if True:
            with ExitStack() as ph:
                with ExitStack() as ph2:
                    modulate(ph2, 0, 0, "a")
                    S.barrier()
                    stage(S, 1)
                qT = sb(ph, "qT", [128, 4, S_LEN], BF16)
                dqT = [deps(4) for _ in range(4)]
                kT = sb(ph, "kT", [128, S_LEN], BF16)
                dkT = deps(4)
                kTs = sb(ph, "kTs", [128, S_LEN], BF16)
                dkTs = Dep()
                vtok = sb(ph, "vtok", [128, 16, 2, 65], BF16)
                dvtok = deps(16)
                wqk = sb(ph, "wqk", [128, 2], F32)
                dwqk = Dep()
                ph_u = ExitStack()
                ph.enter_context(ph_u)
                uT = sb(ph_u, "uT", [128, 4, S_LEN], BF16)
                duT = [deps(4) for _ in range(4)]
                S.dma("sp", wqk[0:64, 0:1], P["ev_q_norm_w"][:, :], writes=[dwqk])
                S.dma("sp", wqk[64:128, 0:1], P["ev_q_norm_w"][:, :], writes=[dwqk])
                S.dma("sp", wqk[0:64, 1:2], P["ev_k_norm_w"][:, :], writes=[dwqk])
                S.dma("sp", wqk[64:128, 1:2], P["ev_k_norm_w"][:, :], writes=[dwqk])
                S.op("act", lambda e: e.mul(out=wqk[:, 0:1], in_=wqk[:, 0:1], mul=0.125), [dwqk], [dwqk])
                S.memset("dve", vtok[:], 1.0, dvtok)
                with ExitStack() as ph2:
                    sqb = [sb(ph2, "qsq%d" % i, [128, 512], BF16) for i in range(2)]
                    dsqb = [Dep(), Dep()]
                    qraw = [sb(ph2, "qraw%d" % i, [128, 512], F32) for i in range(2)]
                    dqraw = [Dep(), Dep()]
                    qrs = [sb(ph2, "qrs%d" % i, [128, 512], F32) for i in range(2)]
                    dqrs = [Dep(), Dep()]
                    wt, dwt = load_w(w_view("ev_in_w", 0, 512), 8, 512)
                    for oc in range(4):
                        for tb in range(4):
                            tsl = slice(tb * 512, (tb + 1) * 512)
                            ps, dps = PSM.next()
                            for kc in range(8):
                                S.mm(ps[:], wt[:, kc, oc * 128:(oc + 1) * 128], hT[:, kc, tsl], kc == 0, kc == 7, [dwt, dhT[kc][tb]], [dps])
                            S.copy(evq.next(), uT[:, oc, tsl], ps[:], [dps], [duT[oc][tb]])
                    stage(S, 21)
                    wt, dwt = load_w(w_view("ev_in_w", 512, 1024), 8, 512)
                    wt2, dwt2 = load_w(w_view("ev_in_w", 1024, 1280), 8, 256)
                    pend = []
                    kk = 0

                    def qk_stage2(ps, dps, ii, dst_ap, ddst, wcol):
                        if STAGE == 221:
                            return
                        pa, dpa = PSA.next()
                        S.mm(pa[:], bd64[:], sqb[ii][:], True, True, [dsqb[ii], dcst["bd64"]], [dpa])
                        S.act(qrs[ii][:], pa[:], AF.Sqrt, [dpa, depsc], [dqrs[ii]], bias=epsc[:, 0:1], scale=1.0 / 64)
                        S.recip(qrs[ii][:], qrs[ii][:], [dqrs[ii]], [dqrs[ii]])
                        S.stt("dve", dst_ap, qraw[ii][:], wqk[:, wcol:wcol + 1], qrs[ii][:], ALU.mult, ALU.mult, [dqraw[ii], dwqk, dqrs[ii]], [ddst])

                    for oc in range(5):
                        for tb in range(4):
                            tsl = slice(tb * 512, (tb + 1) * 512)
                            ps, dps = PSM.next()
                            for kc in range(8):
                                if oc < 4:
                                    S.mm(ps[:], wt[:, kc, oc * 128:(oc + 1) * 128], hT[:, kc, tsl], kc == 0, kc == 7, [dwt, dhT[kc][tb]], [dps])
                                else:
                                    S.mm(ps[:], wt2[:, kc, 0:128], hT[:, kc, tsl], kc == 0, kc == 7, [dwt2, dhT[kc][tb]], [dps])
                            ii = kk % 2
                            kk += 1
                            S.copy("act", qraw[ii][:], ps[:], [dps], [dqraw[ii]])
                            S.act(sqb[ii][:], qraw[ii][:], AF.Square, [dqraw[ii]], [dsqb[ii]])
                            if pend:
                                pend.pop(0)()
                            if oc < 4:
                                pend.append(lambda ps=ps, dps=dps, ii=ii, oc=oc, tb=tb, tsl=tsl: qk_stage2(ps, dps, ii, qT[:, oc, tsl], dqT[oc][tb], 0))
                            else:
                                pend.append(lambda ps=ps, dps=dps, ii=ii, tb=tb, tsl=tsl: qk_stage2(ps, dps, ii, kT[:, tsl], dkT[tb], 1))
                    while pend:
                        pend.pop(0)()
                    stage(S, 22)
                    S.dma("sp", kTs[64:128, :], kT[0:64, :], reads=dkT, writes=[dkTs])
                    S.dma("sp", kTs[0:64, :], kT[64:128, :], reads=dkT, writes=[dkTs])
                    for tt in range(16):
                        ps, dps = PSM.next()
                        for kc in range(8):
                            S.mm(ps[:, 0:128], hT[:, kc, tt * 128:(tt + 1) * 128], wt2[:, kc, 128:256], kc == 0, kc == 7, [dwt2, dhT[kc][tt // 4]], [dps])
                        S.copy(evq.next(), vtok[:, tt, :, 0:64], ps[:, 0:128].rearrange("p (a b) -> p a b", a=2), [dps], [dvtok[tt]])
                    S.barrier()
                    stage(S, 2)
                yT, dyT = hT, dhT
                with ExitStack() as ph2:
                    ccsc = sb(ph2, "ccsc", [128, 256], BF16)
                    dccsc = Dep()
                    S.dma("sp", ccsc[:], C["ccsc"][:, :], writes=[dccsc])
                    ab = sb(ph2, "ab", [128, 2, 16, 256], BF16)
                    dab = [deps(16) for _ in range(2)]
                    cst_t = [sb(ph2, "cs_t%d" % i, [128, 4, 512], BF16) for i in range(2)]
                    sst_t = [sb(ph2, "ss_t%d" % i, [128, 4, 512], BF16) for i in range(2)]
                    dcs_t = [Dep(), Dep()]
                    dss_t = [Dep(), Dep()]
                    csv = C["cs_mat"].rearrange("(t p) n -> p t n", p=128)
                    ssv = C["ss_mat"].rearrange("(t p) n -> p t n", p=128)
                    k = 0
                    for gp in range(2):
                        for g2 in range(2):
                            g = gp * 2 + g2
                            for tt in range(16):
                                ps, dps = PSA.next()
                                S.mm(ps[:, 0:256], uT[:, g, tt * 128:(tt + 1) * 128], ccsc[:], True, True, [duT[g][tt // 4], dccsc], [dps])
                                S.copy(evq.next(), ab[:, g2, tt, :], ps[:, 0:256], [dps], [dab[g2][tt]])
                        for nb in range(4):
                            accs = [PSM.next() for _ in range(2)]
                            for qq in range(4):
                                i = k % 2
                                k += 1
                                S.dma("sp", cst_t[i][:], csv[:, qq * 4:(qq + 1) * 4, nb * 512:(nb + 1) * 512], writes=[dcs_t[i]])
                                S.dma("act", sst_t[i][:], ssv[:, qq * 4:(qq + 1) * 4, nb * 512:(nb + 1) * 512], writes=[dss_t[i]])
                                for g2 in range(2):
                                    ps, dps = accs[g2]
                                    for t4 in range(4):
                                        tt = qq * 4 + t4
                                        S.mm(ps[:], ab[:, g2, tt, 0:128], cst_t[i][:, t4, :], tt == 0, False, [dab[g2][tt], dcs_t[i]], [dps])
                                        S.mm(ps[:], ab[:, g2, tt, 128:256], sst_t[i][:, t4, :], False, tt == 15, [dab[g2][tt], dss_t[i]], [dps])
                            for g2 in range(2):
                                ps, dps = accs[g2]
                                S.copy(evq.next(), yT[:, gp * 2 + g2, nb * 512:(nb + 1) * 512], ps[:], [dps], [dyT[gp * 2 + g2][nb]])
                    S.barrier()
                    stage(S, 3)
                ph_u.close()
                with ExitStack() as ph2:
                    rb = sb(ph2, "rb", [32, 8], F32)
                    drb = Dep()
                    rbb = sb(ph2, "rbb", [32, 8, 128], F32)
                    drbb = Dep()
                    ohr = sb(ph2, "ohr", [32, 512], F32)
                    dohr = Dep()
                    amask = sb(ph2, "amask", [128, 384], F32)
                    damask = Dep()
                    bm = sb(ph2, "bm", [128, 8, 384], F32)
                    dbm = Dep()
                    tv = sb(ph2, "tv", [128, 512], F32)
                    dtv = Dep()
                    esb = sb(ph2, "esb", [128, 8], F32)
                    desb = Dep()
                    dts = deps(8)
                    S.dma("sp", rb[:], P["rel_bias"][:, :], writes=[drb])
                    S.dma("sp", ohr[:], C["ohr"][:, :], writes=[dohr])
                    S.dma("sp", amask[:], C["amask"][:, :], writes=[damask])
                    S.dma("sp", esb[:], bass.AP(tensor=P["ev_sink"].tensor, offset=0, ap=[[0, 128], [1, 8]]), writes=[desb])
                    S.act(esb[:], esb[:], AF.Exp, [desb], [desb])
                    for h in range(8):
                        S.copy("dve", rbb[:, h, :], rb[:, h:h + 1].to_broadcast([32, 128]), [drb], [drbb])
                    for h in range(8):
                        ps, dps = PSA.next()
                        S.mm(ps[:], rbb[:, h, :], ohr[:], True, True, [drbb, dohr], [dps])
                        S.copy("dve", tv[:], ps[:], [dps], [dtv])
                        S.dma("sp", tscr[h], tv[:], reads=[dtv], writes=[dts[h]])
                        S.dma("sp", bm[:, h, :], bass.AP(tensor=tscr.tensor, offset=h * 128 * 512 + 127, ap=[[511, 128], [1, 384]]), reads=[dts[h]], writes=[dbm])
                    for h in range(8):
                        S.tt("dve", bm[:, h, :], bm[:, h, :], amask[:], ALU.add, [dbm, damask], [dbm])
                    et = [sb(ph2, "et%d" % i, [128, 8, 384], BF16) for i in range(5)]
                    det = [Dep() for _ in range(5)]
                    ltmp = [sb(ph2, "ltmp%d" % i, [128, 384], F32) for i in range(2)]
                    dltmp = [Dep(), Dep()]
                    otok = [sb(ph2, "otok%d" % i, [128, 512], BF16) for i in range(2)]
                    dotok = [Dep(), Dep()]
                    den = sb(ph2, "den", [128, 4], F32)
                    dden = Dep()
                    kq = 0

                    def pv_block(n):
                        ot, dot_ = otok[n % 2], dotok[n % 2]
                        for hq in range(2):
                            ps, dps = PSA.next()
                            for hh in range(4):
                                h = hq * 4 + hh
                                js = [j for j in (n - 1, n, n + 1) if 0 <= j < 16]
                                for ji, j in enumerate(js):
                                    c0 = (n - j + 1) * 128
                                    S.mm(ps[:, hh * 65:(hh + 1) * 65], et[j % 5][:, h, c0:c0 + 128], vtok[:, j, hq, :], ji == 0, ji == len(js) - 1,
                                         [det[j % 5], dvtok[j]], [dps])
                            pv = ps[:, 0:260].rearrange("p (a b) -> p a b", b=65)
                            S.tt("dve", den[:], pv[:, :, 64], esb[:, hq * 4:(hq + 1) * 4], ALU.add, [dps, desb], [dden])
                            S.recip(den[:], den[:], [dden], [dden])
                            S.tt("dve", ot[:, hq * 256:(hq + 1) * 256].rearrange("p (a b) -> p a b", b=64), pv[:, :, 0:64],
                                 den[:].unsqueeze(2).to_broadcast([128, 4, 64]), ALU.mult, [dps, dden], [dot_])
                        for fc in range(4):
                            S.tr(psb_t[:, fc * 128:(fc + 1) * 128], ot[:, fc * 128:(fc + 1) * 128], identb[:], [dot_, dcst["identb"]], [dpsb])
                        S.copy("act", yT[:, 4:8, n * 128:(n + 1) * 128], psb_t[:, 0:512].rearrange("p (a b) -> p a b", b=128), [dpsb],
                               [dyT[4 + i][n // 4] for i in range(4)])

                    for j in range(16):
                        e_t, de_t = et[j % 5], det[j % 5]
                        q0 = max(0, j - 1) * 128
                        q1 = min(16, j + 2) * 128
                        c0 = q0 - (j - 1) * 128
                        ncol = q1 - q0
                        for h in range(8):
                            kvh = h // 4
                            ch = h // 2
                            hf = h % 2
                            ksrc = kT if hf == kvh else kTs
                            ps, dps = PSM.next()
                            S.mm(ps[:, 0:ncol], ksrc[hf * 64:(hf + 1) * 64, j * 128:(j + 1) * 128], qT[hf * 64:(hf + 1) * 64, ch, q0:q1], True, True,
                                 [dkT[j // 4], dkTs] + [dqT[ch][b] for b in range(q0 // 512, (q1 - 1) // 512 + 1)], [dps])
                            lt, dlt = ltmp[kq % 2], dltmp[kq % 2]
                            kq += 1
                            S.tt("dve", lt[:, 0:ncol], ps[:, 0:ncol], bm[:, h, c0:c0 + ncol], ALU.add, [dps, dbm], [dlt])
                            S.act(e_t[:, h, c0:c0 + ncol], lt[:, 0:ncol], AF.Exp, [dlt], [de_t])
                        if j >= 2:
                            pv_block(j - 2)
                    pv_block(14)
                    pv_block(15)
                    S.barrier()
                    stage(S, 4)
                for half in range(2):
                    wt, dwt = load_w(w_view("ev_out_w", half * 512, (half + 1) * 512), 8, 512)
                    for d4 in range(4):
                        dc = half * 4 + d4
                        for tb in range(4):
                            tsl = slice(tb * 512, (tb + 1) * 512)
                            ps, dps = PSM.next()
                            for kc in range(8):
                                S.mm(ps[:], wt[:, kc, d4 * 128:(d4 + 1) * 128], yT[:, kc, tsl], kc == 0, kc == 7, [dwt, dyT[kc][tb]], [dps])
                            S.stt("dve", xT[:, dc, tsl], ps[:], modcol[0][:, 16 + dc:17 + dc], xT[:, dc, tsl], ALU.mult, ALU.add,
                                  [dps, dmod[0], dxT[dc][tb]], [dxT[dc][tb]])
                S.barrier()
          S.dead = False
          dump_x(0)

        if p0 <= 1 < p1:
            with ExitStack() as ph:
                modulate(ph, 0, 1, "b")

                def upd(po, dpo, dc, tb):
                    tsl = slice(tb * 512, (tb + 1) * 512)
                    S.stt("dve", xT[:, dc, tsl], po[:], modcol[0][:, 40 + dc:41 + dc], xT[:, dc, tsl], ALU.mult, ALU.add,
                          [dpo, dmod[0], dxT[dc][tb]], [dxT[dc][tb]])

                adab = make_adabufs(ph)
                nbq = list(range(12))

                def after_group():
                    for _ in range(2):
                        if nbq:
                            ada_block(1, nbq.pop(0), adab)

                swiglu(ph, lambda a, b: w_view("ev_ffn_w1", a, b), lambda a, b: w_view("ev_ffn_w3", a, b),
                       lambda f0, n: P["ev_ffn_w2"][f0 * 128:(f0 + n) * 128, :].rearrange("(j p) d -> p j d", p=128), 2816, upd, "f", after_group=after_group)
                while nbq:
                    ada_block(1, nbq.pop(0), adab)
                ada_finish(1)
                S.barrier()
            dump_x(1)

        if p0 <= 2 < p1:
            ssd_phase_inner()
            dump_x(2)

        if p0 <= 3 < p1:
            moe_phase_inner()
            dump_x(3)

        with ExitStack() as ph:
            ot = [sb(ph, "ot%d" % i, [128, 4, 1024], F32) for i in range(2)]
            dot = [Dep(), Dep()]
            dout = Dep()
            for tb in range(4):
                t, dt_ = ot[tb % 2], dot[tb % 2]
                for a in range(4):
                    for fq in range(2):
                        ps, dps = PSM.next()
                        for f4 in range(4):
                            fc = fq * 4 + f4
                            S.tr(ps[:, f4 * 128:(f4 + 1) * 128], xT[:, fc, tb * 512 + a * 128: tb * 512 + (a + 1) * 128], identf[:],
                                 [dxT[fc][tb], dcst["identf"]], [dps])
                        S.copy(evq.next(), t[:, a, fq * 512:(fq + 1) * 512], ps[:], [dps], [dt_])
                S.dma("sp", out_d[tb * 512:(tb + 1) * 512, :].rearrange("(a p) f -> p a f", p=128), t[:], reads=[dt_], writes=[dout])
            S.barrier()
        S.emit_all(block)
    nc._used_inputs = list(P.keys())
    return nc


def ssd_phase(nc, S, sb, P, C, L):
    raise NotImplementedError


def moe_phase(nc, S, sb, P, C, L):
    raise NotImplementedError


_CACHE = {}


def prep_inputs(inputs, b):
    m = {}
    f = lambda a: np.ascontiguousarray(np.asarray(a, dtype=np.float32))
    m["x"] = f(inputs["x"][b])
    m["c"] = f(inputs["c"][b]).reshape(8, 128)
    m["rel_bias"] = f(inputs["rel_bias"])
    for k, shp in PARAM_SHAPES.items():
        if k in m:
            continue
        m[k] = f(inputs[k][0]).reshape(shp)
    return m


def kernel(**inputs):
    if "nc" not in _CACHE:
        _CACHE["nc"] = build()
        _CACHE["consts"] = host_consts()
    nc = _CACHE["nc"]
    consts = _CACHE["consts"]
    in_maps = []
    for b in range(8):
        m = prep_inputs(inputs, b)
        m.update(consts)
        in_maps.append({k: m[k] for k in nc._used_inputs})
    res = run_bass_kernel_spmd(nc, in_maps, core_ids=list(range(8)))
    return np.stack([np.asarray(r["out"], dtype=np.float32) for r in res.results], 0)
```

```python
import numpy as np
import ml_dtypes
from contextlib import ExitStack
import concourse.bass as bass
import concourse.mybir as mybir
from concourse.bass_utils import run_bass_kernel_spmd

F32 = mybir.dt.float32
BF16 = mybir.dt.bfloat16
ALU = mybir.AluOpType
AF = mybir.ActivationFunctionType
ENGS = ("pe", "act", "dve", "pool", "sp")
S_LEN = 2048
D = 1024
EPS = 1e-6
NEG = -30000.0


class Dep:
    __slots__ = ("w", "r", "dsem", "dcnt", "wq")

    def __init__(self):
        self.wq = None
        self.w = None
        self.r = []
        self.dsem = None
        self.dcnt = 0


def deps(n):
    return [Dep() for _ in range(n)]


class Sched:
    def __init__(self, nc, stack):
        self.nc = nc
        self.stack = stack
        self.q = {e: [] for e in ENGS}
        self.cnt = {e: 0 for e in ENGS}
        self.sem = {e: stack.enter_context(nc.semaphore("s_" + e)) for e in ENGS}
        self.known = {e: {} for e in ENGS}
        self.same_wait = {"pe": False, "act": True, "dve": True, "pool": True, "sp": False}
        self.nsem = 0
        self.dma_deps = []
        self.dead = False

    def _waits(self, eng, reads, writes):
        evs = []
        for d in reads:
            if d.w is not None:
                evs.append(d.w)
        for d in writes:
            if d.w is not None:
                evs.append(d.w)
            evs.extend(d.r)
        waits = {}
        kn = self.known[eng]
        for (sem, val, e) in evs:
            if e == eng and not self.same_wait[eng]:
                continue
            if kn.get(id(sem), 0) >= val:
                continue
            cur = waits.get(id(sem))
            if cur is None or cur[1] < val:
                waits[id(sem)] = (sem, val)
        for k, (sem, val) in waits.items():
            kn[k] = val
        return list(waits.values())

    def op(self, eng, fn, reads=(), writes=()):
        if self.dead:
            return
        waits = self._waits(eng, reads, writes)
        self.cnt[eng] += 1
        sem = self.sem[eng]
        ev = (sem, self.cnt[eng], eng)

        def emit(e, waits=waits, fn=fn, sem=sem):
            for (s, v) in waits:
                e.wait_ge(s, v)
            fn(e).then_inc(sem, 1)

        self.q[eng].append(emit)
        for d in writes:
            d.w = ev
            d.r = []
        for d in reads:
            if d not in writes:
                d.r.append(ev)

    def dma(self, eng, out_ap, in_ap, reads=(), writes=(), **kw):
        if self.dead:
            return
        dst = writes[0]
        if dst.w is not None and dst.w[2] is None and dst.w[0] is dst.dsem and not dst.r and dst.wq == eng:
            waits = self._waits(eng, reads, ())
        else:
            waits = self._waits(eng, reads, writes)
        if dst.dsem is None:
            dst.dsem = self.stack.enter_context(self.nc.semaphore("d%d" % self.nsem))
            self.nsem += 1
            self.dma_deps.append(dst)
        dst.dcnt += 16
        dst.wq = eng
        ev = (dst.dsem, dst.dcnt, None)
        dsem = dst.dsem

        def emit(e, waits=waits, dsem=dsem, out_ap=out_ap, in_ap=in_ap, kw=kw):
            for (s, v) in waits:
                e.wait_ge(s, v)
            e.dma_start(out=out_ap, in_=in_ap, **kw).then_inc(dsem, 16)

        self.q[eng].append(emit)
        for d in writes:
            d.w = ev
            d.r = []
        for d in reads:
            d.r.append(ev)

    def barrier(self):
        if self.dead:
            return
        evs = [(self.sem[e], self.cnt[e]) for e in ENGS if self.cnt[e] > 0]
        evs += [(d.dsem, d.dcnt) for d in self.dma_deps]
        for eng in ENGS:
            kn = self.known[eng]
            waits = []
            for (sem, val) in evs:
                if sem is self.sem[eng]:
                    continue
                if kn.get(id(sem), 0) >= val:
                    continue
                kn[id(sem)] = val
                waits.append((sem, val))

            def emit(e, waits=waits):
                for (s, v) in waits:
                    e.wait_ge(s, v)

            self.q[eng].append(emit)

    def emit_all(self, block):
        m = {"pe": block.tensor, "act": block.scalar, "dve": block.vector, "pool": block.gpsimd, "sp": block.sync}
        for eng in ENGS:
            lst = self.q[eng]

            def body(e, lst=lst):
                for f in lst:
                    f(e)

            m[eng](body)

    def mm(self, out, lhsT, rhs, start, stop, r, w):
        self.op("pe", lambda e: e.matmul(out, lhsT=lhsT, rhs=rhs, start=start, stop=stop), r, w)

    def tr(self, out, in_, ident, r, w):
        self.op("pe", lambda e: e.transpose(out, in_, ident), r, w)

    def act(self, out, in_, func, r, w, bias=None, scale=None):
        kw = {}
        if bias is not None:
            kw["bias"] = bias
        if scale is not None:
            kw["scale"] = scale
        self.op("act", lambda e: e.activation(out=out, in_=in_, func=func, **kw), r, w)

    def copy(self, eng, out, in_, r, w):
        if eng == "act":
            self.op("act", lambda e: e.copy(out=out, in_=in_), r, w)
        else:
            self.op(eng, lambda e: e.tensor_copy(out=out, in_=in_), r, w)

    def tt(self, eng, out, in0, in1, op, r, w):
        self.op(eng, lambda e: e.tensor_tensor(out=out, in0=in0, in1=in1, op=op), r, w)

    def ts(self, eng, out, in0, s1, s2, op0, op1, r, w):
        if s2 is None:
            self.op(eng, lambda e: e.tensor_scalar(out=out, in0=in0, scalar1=s1, scalar2=None, op0=op0), r, w)
        else:
            self.op(eng, lambda e: e.tensor_scalar(out=out, in0=in0, scalar1=s1, scalar2=s2, op0=op0, op1=op1), r, w)

    def stt(self, eng, out, in0, scalar, in1, op0, op1, r, w):
        self.op(eng, lambda e: e.scalar_tensor_tensor(out=out, in0=in0, scalar=scalar, in1=in1, op0=op0, op1=op1), r, w)

    def memset(self, eng, ap, val, w):
        self.op(eng, lambda e: e.memset(ap, val), (), w)

    def recip(self, out, in_, r, w):
        self.op("dve", lambda e: e.reciprocal(out=out, in_=in_), r, w)


class _Stop(Exception):
    pass


STAGE = 99


def stage(S, k):
    if STAGE == k or (STAGE == 221 and k == 22):
        S.barrier()
        S.dead = True


class Ring:
    def __init__(self, items):
        self.items = items
        self.i = 0

    def next(self):
        it = self.items[self.i % len(self.items)]
        self.i += 1
        return it


def _bf(a):
    return np.ascontiguousarray(a.astype(np.float32)).astype(ml_dtypes.bfloat16)


def host_consts():
    c = {}
    c["identf"] = np.eye(128, dtype=np.float32)
    c["identb"] = _bf(np.eye(128))
    c["onesb"] = _bf(np.ones((128, 128)))
    c["onesf"] = np.ones((128, 128), np.float32)
    bd = np.zeros((128, 128), np.float32)
    bd[:64, :64] = 1
    bd[64:, 64:] = 1
    c["bd64"] = _bf(bd)
    k = np.arange(128)
    ang = 2 * np.pi * np.outer(k, k) / 128.0
    c["ccsc"] = _bf(np.concatenate([np.cos(ang), -np.sin(ang)], 1) / np.sqrt(128.0))
    n = np.arange(S_LEN)
    jk = np.outer(n, n) % S_LEN
    ang = 2 * np.pi * jk / float(S_LEN)
    c["cs_mat"] = _bf(np.cos(ang) / np.sqrt(float(S_LEN)))
    c["ss_mat"] = _bf(np.sin(ang) / np.sqrt(float(S_LEN)))
    i = np.arange(512)
    rel = 255 - i
    half, max_exact = 16, 8
    na = np.abs(rel)
    large = max_exact + (np.log(np.maximum(na, 1) / max_exact) / np.log(128 / max_exact) * (half - max_exact)).astype(np.int32)
    large = np.minimum(large, half - 1)
    bucket = (rel > 0).astype(np.int32) * half + np.where(na < max_exact, na, large)
    oh = np.zeros((32, 512), np.float32)
    oh[bucket, i] = 1.0
    oh[:, 511] = 0.0
    c["ohr"] = oh
    p = np.arange(128)[:, None]
    cc = np.arange(384)[None, :]
    relm = 128 + p - cc
    c["amask"] = np.where(np.abs(relm) <= 128, 0.0, NEG).astype(np.float32)
    s = np.arange(128)[:, None]
    l = np.arange(128)[None, :]
    c["trif"] = (s <= l).astype(np.float32)
    c["trib"] = (s >= l).astype(np.float32)
    c["maskf"] = _bf((l >= s).astype(np.float32))
    c["maskb"] = _bf((l <= s).astype(np.float32))
    c["negf"] = np.where(l >= s, 0.0, NEG).astype(np.float32)
    c["negb"] = np.where(l <= s, 0.0, NEG).astype(np.float32)
    sl = np.zeros((128, 128), np.float32)
    sl[127, :] = 1
    c["sellast"] = sl
    sf = np.zeros((128, 128), np.float32)
    sf[0, :] = 1
    c["selfirst"] = sf
    ee = np.zeros((8, 8, 128), np.float32)
    for e in range(8):
        ee[e, e, :] = 1
    c["esel"] = ee.reshape(8, 1024)
    return c


CONST_SHAPES = {
    "identf": ([128, 128], F32), "identb": ([128, 128], BF16), "onesb": ([128, 128], BF16), "onesf": ([128, 128], F32),
    "bd64": ([128, 128], BF16), "ccsc": ([128, 256], BF16), "cs_mat": ([2048, 2048], BF16), "ss_mat": ([2048, 2048], BF16),
    "ohr": ([32, 512], F32), "amask": ([128, 384], F32), "trif": ([128, 128], F32), "trib": ([128, 128], F32),
    "maskf": ([128, 128], BF16), "maskb": ([128, 128], BF16), "sellast": ([128, 128], F32), "selfirst": ([128, 128], F32),
    "esel": ([8, 1024], F32), "negf": ([128, 128], F32), "negb": ([128, 128], F32),
}

PARAM_SHAPES = {
    "x": [2048, 1024], "c": [8, 128], "rel_bias": [32, 8],
    "ev_ada_w": [1024, 6144], "ev_ada_b": [1, 6144], "ev_norm1_w": [8, 128], "ev_in_w": [1024, 1280],
    "ev_q_norm_w": [64, 1], "ev_k_norm_w": [64, 1], "ev_sink": [1, 8], "ev_out_w": [1024, 1024], "ev_norm2_w": [8, 128],
    "ev_ffn_w1": [1024, 2816], "ev_ffn_w3": [1024, 2816], "ev_ffn_w2": [2816, 1024],
    "od_ada_w": [1024, 6144], "od_ada_b": [1, 6144], "od_norm1_w": [8, 128], "od_in_w": [1024, 5184],
    "od_conv_w": [120, 128], "od_conv_b": [24, 128], "od_dt_bias_f": [1, 32], "od_dt_bias_b": [1, 32],
    "od_A_log_f": [1, 32], "od_A_log_b": [1, 32], "od_D": [1, 32], "od_gnorm_w": [16, 128], "od_out_w": [2048, 1024],
    "od_norm2_w": [8, 128], "od_router_w": [1024, 8], "od_router_b": [1, 8],
    "od_moe_w1": [8, 1024, 3584], "od_moe_w3": [8, 1024, 3584], "od_moe_w2": [8, 3584, 1024],
}


def build(p0=0, p1=4, dump=False):
    nc = bass.Bass("TRN2", target_bir_lowering=False)
    class _Lazy(dict):
        def __missing__(self, k):
            if k in PARAM_SHAPES:
                v = nc.dram_tensor(k, list(PARAM_SHAPES[k]), F32, kind="ExternalInput").ap()
            else:
                v = nc.dram_tensor(k, list(CONST_SHAPES[k][0]), CONST_SHAPES[k][1], kind="ExternalInput").ap()
            self[k] = v
            return v

    P = _Lazy()
    C = P
    out_d = nc.dram_tensor("out", [2048, 1024], F32, kind="ExternalOutput").ap()
    dump_d = [nc.dram_tensor("dump%d" % i, [2048, 1024], F32, kind="ExternalOutput").ap() for i in range(4)] if dump else None
    tscr = nc.dram_tensor("tscr", [8, 128, 512], F32, kind="Internal").ap()
    ygscr = nc.dram_tensor("ygscr", [2048, 2048], BF16, kind="ExternalOutput" if dump else "Internal").ap()
    acscr = nc.dram_tensor("acscr", [64, 2048], F32, kind="Internal").ap()
    xscr = nc.dram_tensor("xscr", [128, 8, 2048], F32, kind="Internal").ap()

    with ExitStack() as st:
        S = Sched(nc, st)

        def sb(stack, name, shape, dt):
            return stack.enter_context(nc.sbuf_tensor("sb_" + name, shape, dt))

        xT = sb(st, "xT", [128, 8, S_LEN], F32)
        dxT = [deps(4) for _ in range(8)]
        hT = sb(st, "hT", [128, 8, S_LEN], BF16)
        dhT = [deps(4) for _ in range(8)]
        cst = {}
        dcst = {}
        for k in ("identf", "identb", "onesb", "onesf", "bd64"):
            cst[k] = sb(st, "c_" + k, CONST_SHAPES[k][0], CONST_SHAPES[k][1])
            dcst[k] = Dep()
        modcol = [sb(st, "modcol%d" % i, [128, 48], F32) for i in range(2)]
        dmod = [Dep(), Dep()]
        pcol = sb(st, "pcol", [128, 72], F32)
        dpcol = Dep()
        acol = sb(st, "acol", [128, 4, 8], F32)
        dacol = Dep()
        cscol = sb(st, "cscol", [128, 8], F32)
        dcs = Dep()
        epsc = sb(st, "epsc", [128, 1], F32)
        depsc = Dep()
        wring_t = [sb(st, "wring%d" % i, [128, 8, 512], BF16) for i in range(3)]
        wring = Ring([(wring_t[i], Dep()) for i in range(3)])
        psf = [st.enter_context(nc.psum_tensor("psf%d" % i, [128, 512], F32)) for i in range(7)]
        psb_t = st.enter_context(nc.psum_tensor("psb", [128, 1024], BF16))
        dpsb = Dep()
        PSM = Ring([(psf[i], Dep()) for i in range(4)])
        PSA = Ring([(psf[i], Dep()) for i in range(4, 7)])
        block = st.enter_context(nc.Block())
        evq = Ring(["act", "dve"])
        WQ = Ring(["pool"])
        lastw = [None]

        identf, identb, onesb, onesf, bd64 = (cst[k] for k in ("identf", "identb", "onesb", "onesf", "bd64"))

        for k in cst:
            S.dma("sp", cst[k][:], C[k][:, :], writes=[dcst[k]])
        S.memset("dve", epsc[:], EPS, [depsc])

        akc = [0]

        def make_adabufs(stack):
            return dict(adat=[sb(stack, "adat%d_%d" % (akc[0], i), [128, 8, 512], F32) for i in range(2)], dadat=deps(2),
                        modrow=[sb(stack, "modrow%d_%d" % (akc[0], i), [1, 512], F32) for i in range(2)], dmr=deps(2),
                        brow=[sb(stack, "brow%d_%d" % (akc[0], i), [1, 512], F32) for i in range(2)], dbrow=deps(2), k=[0], tag=akc.__setitem__(0, akc[0] + 1))

        def ada_block(layer, nb, B_):
            nm = ("ev", "od")[layer]
            k = B_["k"][0]
            B_["k"][0] += 1
            t, dt_ = B_["adat"][k % 2], B_["dadat"][k % 2]
            mr, dmr_ = B_["modrow"][k % 2], B_["dmr"][k % 2]
            br, dbr_ = B_["brow"][k % 2], B_["dbrow"][k % 2]
            S.dma("act", br[:], P[nm + "_ada_b"][:, nb * 512:(nb + 1) * 512], writes=[dbr_])
            S.dma("sp", t[:], P[nm + "_ada_w"].rearrange("(kc p) f -> p kc f", p=128)[:, :, nb * 512:(nb + 1) * 512], writes=[dt_])
            ps, dps = PSA.next()
            for kc in range(8):
                S.mm(ps[0:1, :], cscol[:, kc:kc + 1], t[:, kc, :], kc == 0, kc == 7, [dcs, dt_], [dps])
            S.tt("dve", mr[:], ps[0:1, :], br[:], ALU.add, [dps, dbr_], [dmr_])
            pc, dpc = PSA.next()
            for j4 in range(4):
                S.mm(pc[:, j4:j4 + 1], mr[0:1, j4 * 128:(j4 + 1) * 128], onesf[0:1, 0:1], True, True, [dmr_, dcst["onesf"]], [dpc])
            S.copy("dve", modcol[layer][:, nb * 4:(nb + 1) * 4], pc[:, 0:4], [dpc], [dmod[layer]])

        def ada_finish(layer):
            for sub in range(2):
                S.stt("dve", acol[:, 2 * layer + sub, :], modcol[layer][:, 8 + 24 * sub:16 + 24 * sub], 1.0,
                      pcol[:, 16 * layer + 8 * sub:16 * layer + 8 * sub + 8], ALU.add, ALU.mult, [dmod[layer], dpcol], [dacol])

        with ExitStack() as ph:
            xst = [sb(ph, "xst%d" % i, [128, 4, 1024], F32) for i in range(2)]
            dxst = [Dep(), Dep()]
            for tb in range(4):
                t, dt_ = xst[tb % 2], dxst[tb % 2]
                S.dma("sp", t[:], P["x"][tb * 512:(tb + 1) * 512, :].rearrange("(a p) f -> p a f", p=128), writes=[dt_])
                for fc in range(8):
                    ps, dps = PSM.next()
                    for a in range(4):
                        S.tr(ps[:, a * 128:(a + 1) * 128], t[:, a, fc * 128:(fc + 1) * 128], identf[:], [dt_, dcst["identf"]], [dps])
                    S.copy(evq.next(), xT[:, fc, tb * 512:(tb + 1) * 512], ps[:], [dps], [dxT[fc][tb]])

            S.barrier()
        with ExitStack() as ph:
            c8 = sb(ph, "c8", [8, 128], F32)
            dc8 = Dep()
            S.dma("sp", c8[:], P["c"][:, :], writes=[dc8])
            ps, dps = PSA.next()
            S.tr(ps[:, 0:8], c8[:], identf[0:8, 0:8], [dc8, dcst["identf"]], [dps])
            S.act(cscol[:], ps[:, 0:8], AF.Silu, [dps], [dcs])
            defer_od = (p0 <= 1 < p1)
            adabufs = make_adabufs(ph)
            for layer in range(2):
                if layer == 1 and defer_od:
                    continue
                for nb in range(12):
                    ada_block(layer, nb, adabufs)
            prow = sb(ph, "prow", [72, 128], F32)
            dprow = Dep()
            for r0, nm in ((0, "ev_norm1_w"), (8, "ev_norm2_w"), (16, "od_norm1_w"), (24, "od_norm2_w"), (32, "od_gnorm_w"), (48, "od_conv_b")):
                n = PARAM_SHAPES[nm][0]
                S.dma("sp", prow[r0:r0 + n, :], P[nm][:, :], writes=[dprow])
            ps, dps = PSA.next()
            S.tr(ps[:, 0:72], prow[:], identf[0:72, 0:72], [dprow, dcst["identf"]], [dps])
            S.copy("dve", pcol[:], ps[:, 0:72], [dps], [dpcol])
            for layer in range(2):
                if layer == 1 and defer_od:
                    continue
                ada_finish(layer)
            S.barrier()

        def modulate(ph, layer, sub, tag, fp32_out=None):
            sq = [sb(ph, "sq%s%d" % (tag, i), [128, 512], BF16) for i in range(3)]
            dsq = deps(3)
            rstd = sb(ph, "rstd" + tag, [128, S_LEN], F32)
            drstd = deps(4)
            tmp = [sb(ph, "mtmp%s%d" % (tag, i), [128, 512], F32) for i in range(2)]
            dtmp = [Dep(), Dep()]
            bcol0 = 0 + 24 * sub
            k = 0
            for tb in range(4):
                tsl = slice(tb * 512, (tb + 1) * 512)
                ps, dps = PSA.next()
                for fc in range(8):
                    i = k % 3
                    k += 1
                    S.act(sq[i][:], xT[:, fc, tsl], AF.Square, [dxT[fc][tb]], [dsq[i]])
                    S.mm(ps[:], onesb[:], sq[i][:], fc == 0, fc == 7, [dsq[i], dcst["onesb"]], [dps])
                S.act(rstd[:, tsl], ps[:], AF.Sqrt, [dps, depsc], [drstd[tb]], bias=epsc[:, 0:1], scale=1.0 / D)
                S.recip(rstd[:, tsl], rstd[:, tsl], [drstd[tb]], [drstd[tb]])
            k = 0
            for tb in range(4):
                tsl = slice(tb * 512, (tb + 1) * 512)
                for fc in range(8):
                    t, dt_ = tmp[k % 2], dtmp[k % 2]
                    k += 1
                    S.stt("dve", t[:], xT[:, fc, tsl], acol[:, 2 * layer + sub, fc:fc + 1], rstd[:, tsl], ALU.mult, ALU.mult,
                          [dxT[fc][tb], dacol, drstd[tb]], [dt_])
                    S.act(hT[:, fc, tsl], t[:], AF.Identity, [dt_, dmod[layer]], [dhT[fc][tb]],
                          bias=modcol[layer][:, bcol0 + fc:bcol0 + fc + 1], scale=1.0)
                    if fp32_out is not None:
                        fp32_out(fc, tb, t, dt_, modcol[layer][:, bcol0 + fc:bcol0 + fc + 1])

        def dbg(name, tile, shape, dt_, reads):
            if not dump:
                return
            d_ = nc.dram_tensor("dbg_" + name, list(shape), dt_, kind="ExternalOutput").ap()
            S.dma("sp", d_, tile[:], reads=reads, writes=[Dep()])

        def load_w(dram_ap_3d, kc_n, ncols):
            t, dt_ = wring.next()
            rd = [lastw[0]] if lastw[0] is not None and lastw[0] is not dt_ else []
            for kc in range(kc_n):
                S.dma(WQ.next(), t[:, kc, 0:ncols], dram_ap_3d[:, kc, :], reads=rd, writes=[dt_])
            lastw[0] = dt_
            return t, dt_

        def w_view(name, c0, c1):
            return P[name].rearrange("(kc p) f -> p kc f", p=128)[:, :, c0:c1]

        def swiglu(ph, w1v, w3v, w2v, F, upd, tag, gate=None, after_group=None):
            if getattr(ph, "_sw", None) is None:
                gT = sb(ph, "gT" + tag, [128, 4, S_LEN], BF16)
                dgT = [deps(4) for _ in range(4)]
                sl_ = [sb(ph, "sil%s%d" % (tag, i), [128, 512], BF16) for i in range(2)]
                dsl = [Dep(), Dep()]
                ph._sw = (gT, dgT, sl_, dsl)
            gT, dgT, sl_, dsl = ph._sw
            nfc_tot = F // 128
            fc0 = 0
            k = 0
            while fc0 < nfc_tot:
                nfc = min(4, nfc_tot - fc0)
                w1t, dw1 = load_w(w1v(fc0 * 128, (fc0 + nfc) * 128), 8, nfc * 128)
                w3t, dw3 = load_w(w3v(fc0 * 128, (fc0 + nfc) * 128), 8, nfc * 128)
                for j in range(nfc):
                    for tb in range(4):
                        tsl = slice(tb * 512, (tb + 1) * 512)
                        p1, dp1 = PSM.next()
                        p3, dp3 = PSM.next()
                        for kc in range(8):
                            S.mm(p1[:], w1t[:, kc, j * 128:(j + 1) * 128], hT[:, kc, tsl], kc == 0, kc == 7, [dw1, dhT[kc][tb]], [dp1])
                        for kc in range(8):
                            S.mm(p3[:], w3t[:, kc, j * 128:(j + 1) * 128], hT[:, kc, tsl], kc == 0, kc == 7, [dw3, dhT[kc][tb]], [dp3])
                        s_, ds_ = sl_[k % 2], dsl[k % 2]
                        k += 1
                        S.act(s_[:], p1[:], AF.Silu, [dp1], [ds_])
                        if gate is None:
                            S.tt("dve", gT[:, j, tsl], p3[:], s_[:], ALU.mult, [dp3, ds_], [dgT[j][tb]])
                        else:
                            gb_, dgb_ = gate(tb)
                            S.tt("dve", gT[:, j, tsl], p3[:], gb_, ALU.mult, [dp3, dgb_], [dgT[j][tb]])
                            S.tt("dve", gT[:, j, tsl], gT[:, j, tsl], s_[:], ALU.mult, [ds_, dgT[j][tb]], [dgT[j][tb]])
                w2t = []
                for half in range(2):
                    w2t.append(load_w(w2v(fc0, nfc)[:, :, half * 512:(half + 1) * 512], nfc, 512))
                for dc in range(8):
                    wt, dwt = w2t[dc // 4]
                    for tb in range(4):
                        tsl = slice(tb * 512, (tb + 1) * 512)
                        po, dpo = PSA.next()
                        for j in range(nfc):
                            S.mm(po[:], wt[:, j, (dc % 4) * 128:(dc % 4 + 1) * 128], gT[:, j, tsl], j == 0, j == nfc - 1, [dwt, dgT[j][tb]], [dpo])
                        upd(po, dpo, dc, tb)
                if after_group is not None:
                    after_group()
                fc0 += nfc

        def dump_x(idx):
            if not dump:
                return
            with ExitStack() as dph:
                ot = [sb(dph, "dot%d_%d" % (idx, i), [128, 4, 1024], F32) for i in range(2)]
                dot = [Dep(), Dep()]
                dd = Dep()
                for tb in range(4):
                    t, dt_ = ot[tb % 2], dot[tb % 2]
                    for a in range(4):
                        for fq in range(2):
                            ps, dps = PSM.next()
                            for f4 in range(4):
                                fc = fq * 4 + f4
                                S.tr(ps[:, f4 * 128:(f4 + 1) * 128], xT[:, fc, tb * 512 + a * 128: tb * 512 + (a + 1) * 128], identf[:],
                                     [dxT[fc][tb], dcst["identf"]], [dps])
                            S.copy(evq.next(), t[:, a, fq * 512:(fq + 1) * 512], ps[:], [dps], [dt_])
                    S.dma("sp", dump_d[idx][tb * 512:(tb + 1) * 512, :].rearrange("(a p) f -> p a f", p=128), t[:], reads=[dt_], writes=[dd])
                S.barrier()


        def moe_phase_inner():
            with ExitStack() as ph:
                rw = sb(ph, "rw", [128, 8, 8], F32)
                drw = Dep()
                S.dma("sp", rw[:], P["od_router_w"].rearrange("(kc p) e -> p kc e", p=128), writes=[drw])
                rbb = sb(ph, "m_rbb", [128, 8], F32)
                drbb = Dep()
                S.dma("sp", rbb[:], bass.AP(tensor=P["od_router_b"].tensor, offset=0, ap=[[0, 128], [1, 8]]), writes=[drbb])
                esel = sb(ph, "esel", [8, 8, 128], F32)
                desel = Dep()
                S.dma("sp", esel[:], P["esel"].rearrange("k (e m) -> k e m", e=8), writes=[desel])
                logit = sb(ph, "logit", [128, 16, 8], F32)
                dlogit = deps(4)
                gtok = sb(ph, "gtok", [128, 16, 8], F32)
                dgtok = deps(4)
                gT8 = sb(ph, "gT8", [8, S_LEN], F32)
                dgT8 = deps(4)
                with ExitStack() as ph2:
                    hfa = sb(ph2, "hfa", [128, 8, 512], F32)
                    dhfa = deps(8)

                    def fp32_out(fc, tb, t, dt_, bcol):
                        S.ts("dve", hfa[:, fc, :], t[:], bcol, None, ALU.add, None, [dt_, dmod[1]], [dhfa[fc]])
                        if fc == 7:
                            pl, dpl = PSM.next()
                            for tt in range(4):
                                for f2 in range(8):
                                    S.mm(pl[:, tt * 8:(tt + 1) * 8], hfa[:, f2, tt * 128:(tt + 1) * 128], rw[:, f2, :], f2 == 0, f2 == 7, [dhfa[f2], drw], [dpl])
                        if fc == 7:
                            S.tt("dve", logit[:, tb * 4:(tb + 1) * 4, :], pl[:, 0:32].rearrange("p (a b) -> p a b", b=8),
                                 rbb[:].unsqueeze(1).to_broadcast([128, 4, 8]), ALU.add, [dpl, drbb], [dlogit[tb]])

                    modulate(ph2, 1, 1, "m", fp32_out=fp32_out)
                    top8 = sb(ph2, "top8", [128, 8], F32)
                    nm1 = sb(ph2, "nm1", [128, 1], F32)
                    ex = sb(ph2, "ex", [128, 8], F32)
                    e2 = sb(ph2, "e2", [128, 1], F32)
                    msk = sb(ph2, "msk", [128, 8], F32)
                    dtp = Dep()
                    for tt in range(16):
                        lg = logit[:, tt, :]
                        dl = dlogit[tt // 4]
                        S.op("dve", lambda e, lg=lg: e.max(out=top8[:], in_=lg), [dl], [dtp])
                        S.ts("dve", nm1[:], top8[:, 0:1], -1.0, None, ALU.mult, None, [dtp], [dtp])
                        S.act(ex[:], lg, AF.Exp, [dl, dtp], [dtp], bias=nm1[:, 0:1], scale=1.0)
                        S.act(e2[:], top8[:, 1:2], AF.Exp, [dtp], [dtp], bias=nm1[:, 0:1], scale=1.0)
                        S.ts("dve", e2[:], e2[:], 1.0, None, ALU.add, None, [dtp], [dtp])
                        S.recip(e2[:], e2[:], [dtp], [dtp])
                        S.ts("dve", msk[:], lg, top8[:, 1:2], None, ALU.is_ge, None, [dl, dtp], [dtp])
                        S.stt("dve", gtok[:, tt, :], ex[:], e2[:, 0:1], msk[:], ALU.mult, ALU.mult, [dtp], [dgtok[tt // 4]])
                    for tb in range(4):
                        ps, dps = PSA.next()
                        for t4 in range(4):
                            S.tr(ps[0:8, t4 * 128:(t4 + 1) * 128], gtok[:, tb * 4 + t4, :], identf[:], [dgtok[tb], dcst["identf"]], [dps])
                        S.copy("dve", gT8[:, tb * 512:(tb + 1) * 512], ps[0:8, :], [dps], [dgT8[tb]])
                    S.barrier()
                gbc = [sb(ph, "gbc%d" % i, [128, 512], F32) for i in range(8)]
                dgbc = deps(8)
                utmp = [sb(ph, "utmp%d" % i, [128, 512], F32) for i in range(2)]
                dutmp = [Dep(), Dep()]
                ucnt = [0]
                for e in range(8):
                    base = (e % 2) * 4
                    for tb in range(4):
                        ps, dps = PSA.next()
                        S.mm(ps[:], esel[:, e, :], gT8[:, tb * 512:(tb + 1) * 512], True, True, [desel, dgT8[tb]], [dps])
                        S.copy("act", gbc[base + tb][:], ps[:], [dps], [dgbc[base + tb]])

                    def upd(po, dpo, dc, tb, base=base):
                        tsl = slice(tb * 512, (tb + 1) * 512)
                        S.stt("dve", xT[:, dc, tsl], po[:], modcol[1][:, 40 + dc:41 + dc], xT[:, dc, tsl], ALU.mult, ALU.add,
                              [dpo, dmod[1], dxT[dc][tb]], [dxT[dc][tb]])

                    def gate(tb, base=base):
                        return gbc[base + tb][:], dgbc[base + tb]

                    w1e = P["od_moe_w1"][e].rearrange("(kc p) f -> p kc f", p=128)
                    w3e = P["od_moe_w3"][e].rearrange("(kc p) f -> p kc f", p=128)
                    w2e = P["od_moe_w2"][e]
                    swiglu(ph, lambda a, b, w=w1e: w[:, :, a:b], lambda a, b, w=w3e: w[:, :, a:b],
                           lambda f0, n, w=w2e: w[f0 * 128:(f0 + n) * 128, :].rearrange("(j p) d -> p j d", p=128), 3584, upd, "m%d" % e, gate=gate)
                S.barrier()


        def ssd_main(ssacc, dssacc):
            with ExitStack() as ph:
                with ExitStack() as ph2:
                    modulate(ph2, 1, 0, "s")
                    dxscr = Dep()
                    for fc in range(8):
                        S.dma("sp", xscr[:, fc, :], xT[:, fc, :], reads=dxT[fc], writes=[dxscr])
                    S.barrier()
                cwcol = sb(ph, "cwcol", [128, 120], F32)
                dcw = Dep()
                dtb = sb(ph, "dtb", [128, 64], F32)
                abc = sb(ph, "abc", [128, 64], F32)
                dbc = sb(ph, "dbc", [128, 32], F32)
                dprm = Dep()
                cst2 = {}
                for k in ("trif", "trib", "sellast", "selfirst", "negf", "negb"):
                    cst2[k] = sb(ph, "c_" + k, [128, 128], F32)
                wdtt = sb(ph, "wdtt", [128, 8, 64], BF16)
                dwdt = Dep()
                for kc in range(8):
                    S.dma("pool", wdtt[:, kc, :], w_view("od_in_w", 5120, 5184)[:, kc, :], writes=[dwdt])
                dcst2 = Dep()
                for k in cst2:
                    S.dma("sp", cst2[k][:], P[k][:, :], writes=[dcst2])
                with ExitStack() as ph2:
                    cwrow = sb(ph2, "cwrow", [120, 128], F32)
                    dcwr = Dep()
                    S.dma("sp", cwrow[:], P["od_conv_w"][:, :], writes=[dcwr])
                    ps, dps = PSA.next()
                    S.tr(ps[:, 0:120], cwrow[:], identf[0:120, 0:120], [dcwr, dcst["identf"]], [dps])
                    S.copy("dve", cwcol[:], ps[:, 0:120], [dps], [dcw])
                    bc = lambda nm, n: bass.AP(tensor=P[nm].tensor, offset=0, ap=[[0, 128], [1, n]])
                    S.dma("sp", dtb[:, 0:32], bc("od_dt_bias_f", 32), writes=[dprm])
                    S.dma("sp", dtb[:, 32:64], bc("od_dt_bias_b", 32), writes=[dprm])
                    S.dma("sp", abc[:, 0:32], bc("od_A_log_f", 32), writes=[dprm])
                    S.dma("sp", abc[:, 32:64], bc("od_A_log_b", 32), writes=[dprm])
                    S.dma("sp", dbc[:], bc("od_D", 32), writes=[dprm])
                    S.act(abc[:], abc[:], AF.Exp, [dprm], [dprm])
                    S.ts("dve", abc[:], abc[:], -1.0, None, ALU.mult, None, [dprm], [dprm])
                    S.barrier()
                def _dtset(i):
                    if i == 0:
                        return [sb(ph, "dtset0_%d" % k, [128, 16, 16], F32) for k in range(5)]
                    return [xT[:, 6, k * 256:(k + 1) * 256].rearrange("p (a b) -> p a b", b=16) for k in range(5)]
                dtsets = [_dtset(0), _dtset(1)]
                ddts = deps(2)
                dacscrs = deps(2)
                ddtmp = Dep()
                a_tok = sb(ph, "a_tok", [128, 16, 16], F32)
                acT = [sb(ph, "acT%d" % i, [16, 512], F32) for i in range(2)]
                dacT = deps(2)
                tmpd = sb(ph, "tmpd", [128, 16], F32)
                dtmpd = Dep()
                dtbg = sb(ph, "dtbg", [128, 16], F32)
                abcg = sb(ph, "abcg", [128, 16], F32)

                def dt_path(g):
                    dt_tok, acum, ea, dec, cdb = dtsets[g % 2]
                    ddt = ddts[g % 2]
                    dacscr = dacscrs[g % 2]
                    r0 = (g % 2) * 16
                    for d in range(2):
                        S.copy("dve", dtbg[:, d * 8:(d + 1) * 8], dtb[:, d * 32 + g * 8:d * 32 + g * 8 + 8], [dprm], [ddtmp])
                        S.copy("dve", abcg[:, d * 8:(d + 1) * 8], abc[:, d * 32 + g * 8:d * 32 + g * 8 + 8], [dprm], [ddtmp])
                    for tt in range(16):
                        ps, dps = PSM.next()
                        for d in range(2):
                            for kc in range(8):
                                S.mm(ps[:, d * 8:(d + 1) * 8], hT[:, kc, tt * 128:(tt + 1) * 128], wdtt[:, kc, d * 32 + g * 8:d * 32 + g * 8 + 8], kc == 0, kc == 7,
                                     [dhT[kc][tt // 4], dwdt], [dps])
                        S.tt("dve", tmpd[:], ps[:, 0:16], dtbg[:], ALU.add, [dps, ddtmp], [dtmpd])
                        S.act(tmpd[:], tmpd[:], AF.Exp, [dtmpd], [dtmpd])
                        S.act(dt_tok[:, tt, :], tmpd[:], AF.Ln, [dtmpd, dcst["onesf"]], [ddt], bias=onesf[:, 0:1], scale=1.0)
                        S.tt("dve", a_tok[:, tt, :], dt_tok[:, tt, :], abcg[:], ALU.mult, [ddt, ddtmp], [ddtmp])
                    for c in range(16):
                        ps, dps = PSM.next()
                        S.mm(ps[:, 0:8], cst2["trif"][:], a_tok[:, c, 0:8], True, True, [dcst2, ddtmp], [dps])
                        S.mm(ps[:, 8:16], cst2["trib"][:], a_tok[:, c, 8:16], True, True, [dcst2, ddtmp], [dps])
                        S.copy("dve", acum[:, c, :], ps[:, 0:16], [dps], [ddt])
                        S.act(ea[:, c, :], ps[:, 0:16], AF.Exp, [dps], [ddt])
                        p2, dp2 = PSM.next()
                        S.mm(p2[:, 0:8], cst2["sellast"][:], acum[:, c, 0:8], True, True, [dcst2, ddt], [dp2])
                        S.mm(p2[:, 8:16], cst2["selfirst"][:], acum[:, c, 8:16], True, True, [dcst2, ddt], [dp2])
                        S.act(cdb[:, c, :], p2[:, 0:16], AF.Exp, [dp2], [ddt])
                        S.tt("dve", tmpd[:], p2[:, 0:16], acum[:, c, :], ALU.subtract, [dp2, ddt], [dtmpd])
                        S.act(dec[:, c, :], tmpd[:], AF.Exp, [dtmpd], [ddt])
                    for c4 in range(4):
                        ps, dps = PSA.next()
                        for c1 in range(4):
                            c = c4 * 4 + c1
                            S.tr(ps[0:16, c1 * 128:(c1 + 1) * 128], acum[:, c, :], identf[:], [ddt, dcst["identf"]], [dps])
                        S.copy("dve", acT[c4 % 2][:], ps[0:16, :], [dps], [dacT[c4 % 2]])
                        S.dma("sp", acscr[r0:r0 + 16, c4 * 512:(c4 + 1) * 512], acT[c4 % 2][:], reads=[dacT[c4 % 2]], writes=[dacscr])

                raw = sb(ph, "craw", [128, S_LEN + 4], BF16)
                draw = Dep()
                S.memset("pool", raw[:, 0:2], 0.0, [draw])
                S.memset("pool", raw[:, S_LEN + 2:S_LEN + 4], 0.0, [draw])

                dgs = [xT[:, 4, i * 320:(i + 1) * 320].bitcast(BF16).rearrange("p (a b) -> p a b", b=128) for i in range(2)]
                ddgs = deps(2)
                kdg = [0]

                def conv_chunk(wt, dwt, wcol0, cchunk, dst_ap, ddst):
                    i = kdg[0] % 2
                    kdg[0] += 1
                    dg, ddg = dgs[i], ddgs[i]
                    for w in range(5):
                        S.ts("dve", dg[:, w, :], identb[:], cwcol[:, w * 24 + cchunk:w * 24 + cchunk + 1], None, ALU.mult, None, [dcst["identb"], dcw], [ddg])
                    for tb in range(4):
                        tsl = slice(tb * 512, (tb + 1) * 512)
                        ps, dps = PSM.next()
                        for kc in range(8):
                            S.mm(ps[:], wt[:, kc, wcol0:wcol0 + 128], hT[:, kc, tsl], kc == 0, kc == 7, [dwt, dhT[kc][tb]], [dps])
                        S.copy("act", raw[:, 2 + tb * 512:2 + (tb + 1) * 512], ps[:], [dps], [draw])
                    for tb in range(4):
                        tsl = slice(tb * 512, (tb + 1) * 512)
                        pc, dpc = PSM.next()
                        for w in range(5):
                            S.mm(pc[:], dg[:, w, :], raw[:, w + tb * 512:w + tb * 512 + 512], w == 0, w == 4, [ddg, draw], [dpc])
                        S.act(dst_ap[:, tsl], pc[:], AF.Silu, [dpc, dpcol], ddst, bias=pcol[:, 48 + cchunk:49 + cchunk], scale=1.0)

                BT = sb(ph, "BT", [128, S_LEN], BF16)
                CT = sb(ph, "CT", [128, S_LEN], BF16)
                dBT, dCT = Dep(), Dep()
                Btok = sb(ph, "Btok", [128, 16, 128], BF16)
                dBtok = Dep()
                cbt = sb(ph, "cbt", [128, 16, 128], BF16)
                dcbt = Dep()
                xsT = sb(ph, "xsT", [128, S_LEN], BF16)
                dxsT = Dep()
                Xd = [sb(ph, "Xd%d" % i, [128, 16, 128], BF16) for i in range(2)]
                dXd = Dep()
                yaccs = [xT[:, 1, :].rearrange("p (a b) -> p a b", b=128), xT[:, 5, :].rearrange("p (a b) -> p a b", b=128)]
                dyaccs = [deps(16), deps(16)]
                arow = [xT[:, 0, i * 1024:(i + 1) * 1024].rearrange("p (a b) -> p a b", b=512) for i in range(2)]
                darow = deps(2)
                Hsr = [xT[:, 3, i * 128:(i + 1) * 128] for i in range(8)]
                dHsr = deps(8)
                lt = [sb(ph, "lt%d" % i, [128, 2, 512], BF16) for i in range(2)]
                dlt = deps(2)
                S_all = sb(ph, "S_all", [128, 2, 16, 128], BF16)
                dSall = [deps(4) for _ in range(2)]
                Hb_all = sb(ph, "Hb_all", [128, 2, 16, 128], BF16)
                dHball = [deps(16) for _ in range(2)]
                S.memset("dve", Hb_all[:, 0, 0, :], 0.0, [dHball[0][0]])
                S.memset("dve", Hb_all[:, 1, 15, :], 0.0, [dHball[1][15]])
                xdr = [sb(ph, "xdr%d" % i, [128, 128], BF16) for i in range(4)]
                dxdr = deps(4)
                szt2 = [sb(ph, "szt%d" % i, [128, 512], BF16) for i in range(2)]
                ygf2 = [xT[:, 3, 1024 + i * 512:1024 + (i + 1) * 512] for i in range(2)]
                sqg2 = [sb(ph, "sqg%d" % i, [128, 512], BF16) for i in range(2)]
                dgate2 = [deps(3) for _ in range(2)]
                ygb = [xT[:, 4, 1024 + i * 256:1024 + (i + 1) * 256].bitcast(BF16) for i in range(2)]
                dygb = deps(2)
                dygscr = Dep()
                kit = [0]
                kyg = [0]
                kxd = [0]
                khs = [0]

                def prep_conv(g, j, wx, dwx):
                    conv_chunk(wx, dwx, j * 128, g * 4 + j, xsT, [dxsT])

                def prep_evac(g, j, yacc, dyacc):
                    dt_tok = dtsets[g % 2][0]
                    ddt = ddts[g % 2]
                    fcx = g * 4 + j
                    h0 = 8 * g + 2 * j
                    for t8 in range(2):
                        for t1_ in range(8):
                            tt = t8 * 8 + t1_
                            S.tr(psb_t[:, t1_ * 128:(t1_ + 1) * 128], xsT[:, tt * 128:(tt + 1) * 128], identb[:], [dxsT, dcst["identb"]], [dpsb])
                        for t1_ in range(8):
                            tt = t8 * 8 + t1_
                            src = psb_t[:, t1_ * 128:(t1_ + 1) * 128].rearrange("p (a b) -> p a b", b=64)
                            for d in range(2):
                                hd0 = d * 8 + 2 * j
                                S.tt("dve", Xd[d][:, tt, :].rearrange("p (a b) -> p a b", b=64), src,
                                     dt_tok[:, tt, hd0:hd0 + 2].unsqueeze(2).to_broadcast([128, 2, 64]), ALU.mult, [dpsb, ddt], [dXd])
                            S.tt("dve", yacc[:, tt, :].rearrange("p (a b) -> p a b", b=64), src,
                                 dbc[:, h0:h0 + 2].unsqueeze(2).to_broadcast([128, 2, 64]), ALU.mult, [dpsb, dprm], [dyacc[tt]])
                    if fcx == 0:
                        dbg("xsT", xsT, [128, S_LEN], BF16, [dxsT])
                        dbg("Xf", Xd[0], [128, 16, 128], BF16, [dXd])
                        dbg("Xb", Xd[1], [128, 16, 128], BF16, [dXd])
                        dbg("Btok", Btok, [128, 16, 128], BF16, [dBtok])
                        dbg("cbt", cbt, [128, 16, 128], BF16, [dcbt])

                def phase_a(g, j, yacc, dyacc):
                    dt_tok, acum, ea, dec, cdb = dtsets[g % 2]
                    ddt = ddts[g % 2]
                    dacscr = dacscrs[g % 2]
                    r0 = (g % 2) * 16

                    def stage1(d, b4):
                        hd0 = d * 8 + 2 * j
                        negm = cst2["negf" if d == 0 else "negb"]
                        c0 = b4 * 4
                        k = kit[0]
                        kit[0] += 1
                        ar, dar = arow[k % 2], darow[k % 2]
                        lt_, dlt_ = lt[k % 2], dlt[k % 2]
                        for hh in range(2):
                            S.dma("sp", ar[:, hh, :], bass.AP(tensor=acscr.tensor, offset=(r0 + hd0 + hh) * S_LEN + c0 * 128, ap=[[0, 128], [1, 512]]),
                                  reads=[dacscr], writes=[dar])
                        for hh in range(2):
                            av = ar[:, hh, :].rearrange("p (a b) -> p a b", b=128)
                            S.tt("dve", av, av, acum[:, c0:c0 + 4, hd0 + hh:hd0 + hh + 1].to_broadcast([128, 4, 128]), ALU.subtract, [dar, ddt], [dar])
                        av8 = ar[:].rearrange("p h (a b) -> p (h a) b", b=128)
                        S.tt("dve", av8, av8, negm[:].unsqueeze(1).to_broadcast([128, 8, 128]), ALU.min, [dar, dcst2], [dar])
                        S.act(lt_[:], ar[:], AF.Exp, [dar], [dlt_])
                        return (d, b4, lt_, dlt_)

                    def stage2(st_):
                        d, b4, lt_, dlt_ = st_
                        hd0 = d * 8 + 2 * j
                        c0 = b4 * 4
                        S.tt("dve", lt_[:], lt_[:], cbt[:, c0:c0 + 4, :].rearrange("p a b -> p (a b)").unsqueeze(1).to_broadcast([128, 2, 512]), ALU.mult,
                             [dlt_, dcbt], [dlt_])
                        p1, dp1 = PSM.next()
                        for c in range(4):
                            for hh in range(2):
                                S.mm(p1[:, c * 128 + hh * 64:c * 128 + (hh + 1) * 64], lt_[:, hh, c * 128:(c + 1) * 128],
                                     Xd[d][:, c0 + c, hh * 64:(hh + 1) * 64], True, True, [dlt_, dXd], [dp1])
                        p3, dp3 = PSA.next()
                        for c in range(4):
                            kx = kxd[0] % 4
                            kxd[0] += 1
                            S.tt("pool", xdr[kx][:].rearrange("p (a b) -> p a b", b=64), Xd[d][:, c0 + c, :].rearrange("p (a b) -> p a b", b=64),
                                 dec[:, c0 + c, hd0:hd0 + 2].unsqueeze(2).to_broadcast([128, 2, 64]), ALU.mult, [dXd, ddt], [dxdr[kx]])
                            S.mm(p3[:, c * 128:(c + 1) * 128], Btok[:, c0 + c, :], xdr[kx][:], True, True, [dBtok, dxdr[kx]], [dp3])
                        S.tt("dve", yacc[:, c0:c0 + 4, :], p1[:].rearrange("p (a b) -> p a b", b=128), yacc[:, c0:c0 + 4, :], ALU.add,
                             [dp1] + dyacc[c0:c0 + 4], dyacc[c0:c0 + 4])
                        S.copy("act", S_all[:, d, c0:c0 + 4, :], p3[:].rearrange("p (a b) -> p a b", b=128), [dp3], [dSall[d][b4]])

                    prev_st = None
                    for (d, b4) in [(d, b4) for d in range(2) for b4 in range(4)]:
                        st_ = stage1(d, b4)
                        if prev_st is not None:
                            stage2(prev_st)
                        prev_st = st_
                    stage2(prev_st)

                def phase_b(g, j):
                    dt_tok, acum, ea, dec, cdb = dtsets[g % 2]
                    ddt = ddts[g % 2]
                    hprev = [None, None]
                    for step in range(16):
                        for d in range(2):
                            hd0 = d * 8 + 2 * j
                            c = step if d == 0 else 15 - step
                            kh = khs[0] % 8
                            khs[0] += 1
                            hn, dhn = Hsr[kh], dHsr[kh]
                            if step == 0:
                                S.copy("dve", hn, S_all[:, d, c, :], [dSall[d][c // 4]], [dhn])
                            else:
                                hp, dhp = hprev[d]
                                for hh in range(2):
                                    S.stt("dve", hn[:, hh * 64:(hh + 1) * 64], hp[:, hh * 64:(hh + 1) * 64], cdb[:, c, hd0 + hh:hd0 + hh + 1],
                                          S_all[:, d, c, hh * 64:(hh + 1) * 64], ALU.mult, ALU.add, [dhp, ddt, dSall[d][c // 4]], [dhn])
                            if step < 15:
                                cn = c + 1 if d == 0 else c - 1
                                S.copy("act", Hb_all[:, d, cn, :], hn, [dhn], [dHball[d][cn]])
                            hprev[d] = (hn, dhn)

                def phase_c(g, j, yacc, dyacc):
                    dt_tok, acum, ea, dec, cdb = dtsets[g % 2]
                    ddt = ddts[g % 2]
                    for d in range(2):
                        hd0 = d * 8 + 2 * j
                        for b4 in range(4):
                            c0 = b4 * 4
                            p2, dp2 = PSM.next()
                            for c in range(4):
                                csl = slice((c0 + c) * 128, (c0 + c + 1) * 128)
                                S.mm(p2[:, c * 128:(c + 1) * 128], CT[:, csl], Hb_all[:, d, c0 + c, :], True, True, [dCT, dHball[d][c0 + c]], [dp2])
                            for c in range(4):
                                for hh in range(2):
                                    ysl = yacc[:, c0 + c, hh * 64:(hh + 1) * 64]
                                    S.stt("dve", ysl, p2[:, c * 128 + hh * 64:c * 128 + (hh + 1) * 64], ea[:, c0 + c, hd0 + hh:hd0 + hh + 1], ysl,
                                          ALU.mult, ALU.add, [dp2, ddt, dyacc[c0 + c]], [dyacc[c0 + c]])

                def gating(g, j, wz, dwz, yacc, dyacc):
                    fcx = g * 4 + j
                    if fcx == 0:
                        dbg("yacc", xT[:, 1, :], [128, S_LEN], F32, dyacc)

                    def g1(tb):
                        tsl = slice(tb * 512, (tb + 1) * 512)
                        py, dpy = PSM.next()
                        for t4 in range(4):
                            tt = tb * 4 + t4
                            S.tr(py[:, t4 * 128:(t4 + 1) * 128], yacc[:, tt, :], identf[:], [dyacc[tt], dcst["identf"]], [dpy])
                        pz, dpz = PSM.next()
                        for kc in range(8):
                            S.mm(pz[:], wz[:, kc, j * 128:(j + 1) * 128], hT[:, kc, tsl], kc == 0, kc == 7, [dwz, dhT[kc][tb]], [dpz])
                        ig = kyg[0] % 2
                        kyg[0] += 1
                        szt, ygf, sqg = szt2[ig], ygf2[ig], sqg2[ig]
                        dsz_, dyg_, dsq_ = dgate2[ig]
                        S.act(szt[:], pz[:], AF.Silu, [dpz], [dsz_])
                        S.tt("dve", ygf, py[:], szt[:], ALU.mult, [dpy, dsz_], [dyg_])
                        S.act(sqg[:], ygf, AF.Square, [dyg_], [dsq_])
                        return (tb, ig)

                    def g2(st_):
                        tb, ig = st_
                        tsl = slice(tb * 512, (tb + 1) * 512)
                        szt, ygf, sqg = szt2[ig], ygf2[ig], sqg2[ig]
                        dsz_, dyg_, dsq_ = dgate2[ig]
                        pq, dpq = PSA.next()
                        S.mm(pq[:], onesb[:], sqg[:], True, True, [dsq_, dcst["onesb"]], [dpq])
                        if fcx == 0:
                            S.copy("dve", ssacc[:, tsl], pq[:], [dpq], [dssacc[tb]])
                        else:
                            S.tt("dve", ssacc[:, tsl], pq[:], ssacc[:, tsl], ALU.add, [dpq, dssacc[tb]], [dssacc[tb]])
                        S.ts("dve", ygb[ig][:], ygf, pcol[:, 32 + fcx:33 + fcx], None, ALU.mult, None, [dyg_, dpcol], [dygb[ig]])
                        S.dma("sp", ygscr[fcx * 128:(fcx + 1) * 128, tsl], ygb[ig][:], reads=[dygb[ig]], writes=[dygscr])

                    prev = None
                    for tb in range(4):
                        st_ = g1(tb)
                        if prev is not None:
                            g2(prev)
                        prev = st_
                    g2(prev)

                BTs = [BT, xT[:, 2, 0:1024].bitcast(BF16)]
                CTs = [CT, xT[:, 2, 1024:2048].bitcast(BF16)]
                Btoks = [Btok, xT[:, 7, 0:1024].bitcast(BF16).rearrange("p (a b) -> p a b", b=128)]
                cbts = [cbt, xT[:, 7, 1024:2048].bitcast(BF16).rearrange("p (a b) -> p a b", b=128)]
                dBTs, dCTs, dBtoks, dcbts = deps(2), deps(2), deps(2), deps(2)
                (wzt, dwz), (wxt, dwx), (wbct, dwbc) = wring.items

                def load_into(t, dt_, view, ncols, col0=0):
                    for kc in range(8):
                        S.dma("pool", t[:, kc, col0:col0 + ncols], view[:, kc, :], writes=[dt_])

                def group_prologue(g):
                    s_ = g % 2
                    load_into(wxt, dwx, w_view("od_in_w", 2048 + g * 512, 2048 + (g + 1) * 512), 512)
                    load_into(wbct, dwbc, w_view("od_in_w", 4096 + g * 128, 4096 + (g + 1) * 128), 128, 0)
                    load_into(wbct, dwbc, w_view("od_in_w", 4608 + g * 128, 4608 + (g + 1) * 128), 128, 128)
                    conv_chunk(wbct, dwbc, 0, 16 + g, BTs[s_], [dBTs[s_]])
                    conv_chunk(wbct, dwbc, 128, 20 + g, CTs[s_], [dCTs[s_]])
                    if g == 0:
                        dbg("BT", BTs[0], [128, S_LEN], BF16, [dBTs[0]])
                        dbg("CT", CTs[0], [128, S_LEN], BF16, [dCTs[0]])
                    for t8 in range(2):
                        for t1_ in range(8):
                            tt = t8 * 8 + t1_
                            S.tr(psb_t[:, t1_ * 128:(t1_ + 1) * 128], BTs[s_][:, tt * 128:(tt + 1) * 128], identb[:], [dBTs[s_], dcst["identb"]], [dpsb])
                        S.copy("act", Btoks[s_][:, t8 * 8:(t8 + 1) * 8, :], psb_t[:, :].rearrange("p (a b) -> p a b", b=128), [dpsb], [dBtoks[s_]])
                    for c in range(16):
                        ps, dps = PSM.next()
                        csl = slice(c * 128, (c + 1) * 128)
                        S.mm(ps[:, 0:128], BTs[s_][:, csl], CTs[s_][:, csl], True, True, [dBTs[s_], dCTs[s_]], [dps])
                        S.copy("act", cbts[s_][:, c, :], ps[:, 0:128], [dps], [dcbts[s_]])

                dt_path(0)
                group_prologue(0)
                for g in range(4):
                    s_ = g % 2
                    BT, CT, Btok, cbt = BTs[s_], CTs[s_], Btoks[s_], cbts[s_]
                    dBT, dCT, dBtok, dcbt = dBTs[s_], dCTs[s_], dBtoks[s_], dcbts[s_]
                    if g == 0:
                        for nm_, t_ in zip(("dt", "acum", "ea", "dec", "cdb"), dtsets[0]):
                            dbg(nm_, t_, [128, 16, 16], F32, [ddts[0]])
                    load_into(wzt, dwz, w_view("od_in_w", g * 512, (g + 1) * 512), 512)
                    prep_conv(g, 0, wxt, dwx)
                    prep_evac(g, 0, yaccs[0], dyaccs[0])
                    for j in range(4):
                        ya, dya = yaccs[j % 2], dyaccs[j % 2]
                        phase_a(g, j, ya, dya)
                        if j < 3:
                            prep_conv(g, j + 1, wxt, dwx)
                        if j == 2 and g < 3:
                            group_prologue(g + 1)
                        phase_b(g, j)
                        if j < 3:
                            prep_evac(g, j + 1, yaccs[(j + 1) % 2], dyaccs[(j + 1) % 2])
                        elif g < 3:
                            dt_path(g + 1)
                        phase_c(g, j, ya, dya)
                        gating(g, j, wzt, dwz, ya, dya)
                S.barrier()
                for fc in range(8):
                    S.dma("sp", xT[:, fc, :], xscr[:, fc, :], reads=[], writes=dxT[fc])

        def ssd_out(ssacc, dssacc):
            with ExitStack() as ph2:
                rs = sb(ph2, "rs_o", [128, S_LEN], F32)
                drs = Dep()
                S.act(rs[:], ssacc[:], AF.Sqrt, dssacc + [depsc], [drs], bias=epsc[:, 0:1], scale=1.0 / 2048)
                S.recip(rs[:], rs[:], [drs], [drs])
                dbg("rs", rs, [128, S_LEN], F32, [drs])
                ygt = [sb(ph2, "ygt%d" % i, [128, 16, 512], BF16) for i in range(2)]
                dygt = deps(2)
                ot1 = [sb(ph2, "ot1_%d" % i, [128, 512], F32) for i in range(2)]
                dot1 = deps(2)
                ko = 0
                for tb in range(4):
                    tsl = slice(tb * 512, (tb + 1) * 512)
                    yt, dyt = ygt[tb % 2], dygt[tb % 2]
                    for kc in range(16):
                        S.dma("sp", yt[:, kc, :], ygscr[kc * 128:(kc + 1) * 128, tsl], reads=[], writes=[dyt])
                    for half in range(2):
                        wa, dwa = load_w(P["od_out_w"][0:1024, half * 512:(half + 1) * 512].rearrange("(kc p) f -> p kc f", p=128), 8, 512)
                        wb_, dwb_ = load_w(P["od_out_w"][1024:2048, half * 512:(half + 1) * 512].rearrange("(kc p) f -> p kc f", p=128), 8, 512)
                        for d4 in range(4):
                            dc = half * 4 + d4
                            ps, dps = PSM.next()
                            for kc in range(16):
                                wt_, dwt_ = (wa, dwa) if kc < 8 else (wb_, dwb_)
                                S.mm(ps[:], wt_[:, kc % 8, d4 * 128:(d4 + 1) * 128], yt[:, kc, :], kc == 0, kc == 15, [dwt_, dyt], [dps])
                            o1, do1 = ot1[ko % 2], dot1[ko % 2]
                            ko += 1
                            S.tt("dve", o1[:], ps[:], rs[:, tsl], ALU.mult, [dps, drs], [do1])
                            S.stt("dve", xT[:, dc, tsl], o1[:], modcol[1][:, 16 + dc:17 + dc], xT[:, dc, tsl], ALU.mult, ALU.add,
                                  [do1, dmod[1], dxT[dc][tb]], [dxT[dc][tb]])
                S.barrier()


        def ssd_phase_inner():
            with ExitStack() as pho:
                ssacc = sb(pho, "ssacc", [128, S_LEN], F32)
                dssacc = deps(4)
                ssd_main(ssacc, dssacc)
                ssd_out(ssacc, dssacc)

        if p0 <= 0 < p1:
          if True:
            with ExitStack() as ph:
                with ExitStack() as ph2:
                    modulate(ph2, 0, 0, "a")
                    S.barrier()
                    stage(S, 1)
                qT = sb(ph, "qT", [128, 4, S_LEN], BF16)
                dqT = [deps(4) for _ in range(4)]
                kT = sb(ph, "kT", [128, S_LEN], BF16)
                dkT = deps(4)
                kTs = sb(ph, "kTs", [128, S_LEN], BF16)
                dkTs = Dep()
                vtok = sb(ph, "vtok", [128, 16, 2, 65], BF16)
                dvtok = deps(16)
                wqk = sb(ph, "wqk", [128, 2], F32)
                dwqk = Dep()
                ph_u = ExitStack()
                ph.enter_context(ph_u)
                uT = sb(ph_u, "uT", [128, 4, S_LEN], BF16)
                duT = [deps(4) for _ in range(4)]
                S.dma("sp", wqk[0:64, 0:1], P["ev_q_norm_w"][:, :], writes=[dwqk])
                S.dma("sp", wqk[64:128, 0:1], P["ev_q_norm_w"][:, :], writes=[dwqk])
                S.dma("sp", wqk[0:64, 1:2], P["ev_k_norm_w"][:, :], writes=[dwqk])
                S.dma("sp", wqk[64:128, 1:2], P["ev_k_norm_w"][:, :], writes=[dwqk])
                S.op("act", lambda e: e.mul(out=wqk[:, 0:1], in_=wqk[:, 0:1], mul=0.125), [dwqk], [dwqk])
                S.memset("dve", vtok[:], 1.0, dvtok)
                with ExitStack() as ph2:
                    sqb = [sb(ph2, "qsq%d" % i, [128, 512], BF16) for i in range(2)]
                    dsqb = [Dep(), Dep()]
                    qraw = [sb(ph2, "qraw%d" % i, [128, 512], F32) for i in range(2)]
                    dqraw = [Dep(), Dep()]
                    qrs = [sb(ph2, "qrs%d" % i, [128, 512], F32) for i in range(2)]
                    dqrs = [Dep(), Dep()]
                    wt, dwt = load_w(w_view("ev_in_w", 0, 512), 8, 512)
                    for oc in range(4):
                        for tb in range(4):
                            tsl = slice(tb * 512, (tb + 1) * 512)
                            ps, dps = PSM.next()
                            for kc in range(8):
                                S.mm(ps[:], wt[:, kc, oc * 128:(oc + 1) * 128], hT[:, kc, tsl], kc == 0, kc == 7, [dwt, dhT[kc][tb]], [dps])
                            S.copy(evq.next(), uT[:, oc, tsl], ps[:], [dps], [duT[oc][tb]])
                    stage(S, 21)
                    wt, dwt = load_w(w_view("ev_in_w", 512, 1024), 8, 512)
                    wt2, dwt2 = load_w(w_view("ev_in_w", 1024, 1280), 8, 256)
                    pend = []
                    kk = 0

                    def qk_stage2(ps, dps, ii, dst_ap, ddst, wcol):
                        if STAGE == 221:
                            return
                        pa, dpa = PSA.next()
                        S.mm(pa[:], bd64[:], sqb[ii][:], True, True, [dsqb[ii], dcst["bd64"]], [dpa])
                        S.act(qrs[ii][:], pa[:], AF.Sqrt, [dpa, depsc], [dqrs[ii]], bias=epsc[:, 0:1], scale=1.0 / 64)
                        S.recip(qrs[ii][:], qrs[ii][:], [dqrs[ii]], [dqrs[ii]])
                        S.stt("dve", dst_ap, qraw[ii][:], wqk[:, wcol:wcol + 1], qrs[ii][:], ALU.mult, ALU.mult, [dqraw[ii], dwqk, dqrs[ii]], [ddst])

                    for oc in range(5):
                        for tb in range(4):
                            tsl = slice(tb * 512, (tb + 1) * 512)
                            ps, dps = PSM.next()
                            for kc in range(8):
                                if oc < 4:
                                    S.mm(ps[:], wt[:, kc, oc * 128:(oc + 1) * 128], hT[:, kc, tsl], kc == 0, kc == 7, [dwt, dhT[kc][tb]], [dps])
                                else:
                                    S.mm(ps[:], wt2[:, kc, 0:128], hT[:, kc, tsl], kc == 0, kc == 7, [dwt2, dhT[kc][tb]], [dps])
                            ii = kk % 2
                            kk += 1
                            S.copy("act", qraw[ii][:], ps[:], [dps], [dqraw[ii]])
                            S.act(sqb[ii][:], qraw[ii][:], AF.Square, [dqraw[ii]], [dsqb[ii]])
                            if pend:
                                pend.pop(0)()
                            if oc < 4:
                                pend.append(lambda ps=ps, dps=dps, ii=ii, oc=oc, tb=tb, tsl=tsl: qk_stage2(ps, dps, ii, qT[:, oc, tsl], dqT[oc][tb], 0))
                            else:
                                pend.append(lambda ps=ps, dps=dps, ii=ii, tb=tb, tsl=tsl: qk_stage2(ps, dps, ii, kT[:, tsl], dkT[tb], 1))
                    while pend:
                        pend.pop(0)()
                    stage(S, 22)
                    S.dma("sp", kTs[64:128, :], kT[0:64, :], reads=dkT, writes=[dkTs])
                    S.dma("sp", kTs[0:64, :], kT[64:128, :], reads=dkT, writes=[dkTs])
                    for tt in range(16):
                        ps, dps = PSM.next()
                        for kc in range(8):
                            S.mm(ps[:, 0:128], hT[:, kc, tt * 128:(tt + 1) * 128], wt2[:, kc, 128:256], kc == 0, kc == 7, [dwt2, dhT[kc][tt // 4]], [dps])
                        S.copy(evq.next(), vtok[:, tt, :, 0:64], ps[:, 0:128].rearrange("p (a b) -> p a b", a=2), [dps], [dvtok[tt]])
                    S.barrier()
                    stage(S, 2)
                yT, dyT = hT, dhT
                with ExitStack() as ph2:
                    ccsc = sb(ph2, "ccsc", [128, 256], BF16)
                    dccsc = Dep()
                    S.dma("sp", ccsc[:], C["ccsc"][:, :], writes=[dccsc])
                    NGP = 4
                    ab = sb(ph2, "ab", [128, NGP, 16, 256], BF16)
                    dab = [deps(16) for _ in range(NGP)]
                    cst_t = [sb(ph2, "cs_t%d" % i, [128, 2, 512], BF16) for i in range(2)]
                    sst_t = [sb(ph2, "ss_t%d" % i, [128, 2, 512], BF16) for i in range(2)]
                    dcs_t = [Dep(), Dep()]
                    dss_t = [Dep(), Dep()]
                    csv = C["cs_mat"].rearrange("(t p) n -> p t n", p=128)
                    ssv = C["ss_mat"].rearrange("(t p) n -> p t n", p=128)
                    k = 0
                    for gp in range(4 // NGP):
                        for g2 in range(NGP):
                            g = gp * NGP + g2
                            for tt in range(16):
                                ps, dps = PSA.next()
                                S.mm(ps[:, 0:256], uT[:, g, tt * 128:(tt + 1) * 128], ccsc[:], True, True, [duT[g][tt // 4], dccsc], [dps])
                                S.copy(evq.next(), ab[:, g2, tt, :], ps[:, 0:256], [dps], [dab[g2][tt]])
                        for nb in range(4):
                            accs = [PSM.next() for _ in range(NGP)]
                            for qq in range(8):
                                i = k % 2
                                k += 1
                                S.dma("sp", cst_t[i][:], csv[:, qq * 2:(qq + 1) * 2, nb * 512:(nb + 1) * 512], writes=[dcs_t[i]])
                                S.dma("act", sst_t[i][:], ssv[:, qq * 2:(qq + 1) * 2, nb * 512:(nb + 1) * 512], writes=[dss_t[i]])
                                for g2 in range(NGP):
                                    ps, dps = accs[g2]
                                    for t4 in range(2):
                                        tt = qq * 2 + t4
                                        S.mm(ps[:], ab[:, g2, tt, 0:128], cst_t[i][:, t4, :], tt == 0, False, [dab[g2][tt], dcs_t[i]], [dps])
                                        S.mm(ps[:], ab[:, g2, tt, 128:256], sst_t[i][:, t4, :], False, tt == 15, [dab[g2][tt], dss_t[i]], [dps])
                            for g2 in range(NGP):
                                ps, dps = accs[g2]
                                S.copy(evq.next(), yT[:, gp * NGP + g2, nb * 512:(nb + 1) * 512], ps[:], [dps], [dyT[gp * NGP + g2][nb]])
                    S.barrier()
                    stage(S, 3)
                ph_u.close()
                with ExitStack() as ph2:
                    rb = sb(ph2, "rb", [32, 8], F32)
                    drb = Dep()
                    rbb = sb(ph2, "rbb", [32, 8, 128], F32)
                    drbb = Dep()
                    ohr = sb(ph2, "ohr", [32, 512], F32)
                    dohr = Dep()
                    amask = sb(ph2, "amask", [128, 384], F32)
                    damask = Dep()
                    bm = sb(ph2, "bm", [128, 8, 384], F32)
                    dbm = Dep()
                    tv = sb(ph2, "tv", [128, 512], F32)
                    dtv = Dep()
                    esb = sb(ph2, "esb", [128, 8], F32)
                    desb = Dep()
                    dts = deps(8)
                    S.dma("sp", rb[:], P["rel_bias"][:, :], writes=[drb])
                    S.dma("sp", ohr[:], C["ohr"][:, :], writes=[dohr])
                    S.dma("sp", amask[:], C["amask"][:, :], writes=[damask])
                    S.dma("sp", esb[:], bass.AP(tensor=P["ev_sink"].tensor, offset=0, ap=[[0, 128], [1, 8]]), writes=[desb])
                    S.act(esb[:], esb[:], AF.Exp, [desb], [desb])
                    for h in range(8):
                        S.copy("dve", rbb[:, h, :], rb[:, h:h + 1].to_broadcast([32, 128]), [drb], [drbb])
                    for h in range(8):
                        ps, dps = PSA.next()
                        S.mm(ps[:], rbb[:, h, :], ohr[:], True, True, [drbb, dohr], [dps])
                        S.copy("dve", tv[:], ps[:], [dps], [dtv])
                        S.dma("sp", tscr[h], tv[:], reads=[dtv], writes=[dts[h]])
                        S.dma("sp", bm[:, h, :], bass.AP(tensor=tscr.tensor, offset=h * 128 * 512 + 127, ap=[[511, 128], [1, 384]]), reads=[dts[h]], writes=[dbm])
                    for h in range(8):
                        S.tt("dve", bm[:, h, :], bm[:, h, :], amask[:], ALU.add, [dbm, damask], [dbm])
                    et = [sb(ph2, "et%d" % i, [128, 8, 384], BF16) for i in range(5)]
                    det = [Dep() for _ in range(5)]
                    ltmp = [sb(ph2, "ltmp%d" % i, [128, 384], F32) for i in range(2)]
                    dltmp = [Dep(), Dep()]
                    otok = [sb(ph2, "otok%d" % i, [128, 512], BF16) for i in range(2)]
                    dotok = [Dep(), Dep()]
                    den = sb(ph2, "den", [128, 4], F32)
                    dden = Dep()
                    kq = 0

                    def pv_block(n):
                        ot, dot_ = otok[n % 2], dotok[n % 2]
                        for hq in range(2):
                            ps, dps = PSA.next()
                            for hh in range(4):
                                h = hq * 4 + hh
                                js = [j for j in (n - 1, n, n + 1) if 0 <= j < 16]
                                for ji, j in enumerate(js):
                                    c0 = (n - j + 1) * 128
                                    S.mm(ps[:, hh * 65:(hh + 1) * 65], et[j % 5][:, h, c0:c0 + 128], vtok[:, j, hq, :], ji == 0, ji == len(js) - 1,
                                         [det[j % 5], dvtok[j]], [dps])
                            pv = ps[:, 0:260].rearrange("p (a b) -> p a b", b=65)
                            S.tt("dve", den[:], pv[:, :, 64], esb[:, hq * 4:(hq + 1) * 4], ALU.add, [dps, desb], [dden])
                            S.recip(den[:], den[:], [dden], [dden])
                            S.tt("dve", ot[:, hq * 256:(hq + 1) * 256].rearrange("p (a b) -> p a b", b=64), pv[:, :, 0:64],
                                 den[:].unsqueeze(2).to_broadcast([128, 4, 64]), ALU.mult, [dps, dden], [dot_])
                        for fc in range(4):
                            S.tr(psb_t[:, fc * 128:(fc + 1) * 128], ot[:, fc * 128:(fc + 1) * 128], identb[:], [dot_, dcst["identb"]], [dpsb])
                        S.copy("act", yT[:, 4:8, n * 128:(n + 1) * 128], psb_t[:, 0:512].rearrange("p (a b) -> p a b", b=128), [dpsb],
                               [dyT[4 + i][n // 4] for i in range(4)])

                    for j in range(16):
                        e_t, de_t = et[j % 5], det[j % 5]
                        q0 = max(0, j - 1) * 128
                        q1 = min(16, j + 2) * 128
                        c0 = q0 - (j - 1) * 128
                        ncol = q1 - q0
                        for h in range(8):
                            kvh = h // 4
                            ch = h // 2
                            hf = h % 2
                            ksrc = kT if hf == kvh else kTs
                            ps, dps = PSM.next()
                            S.mm(ps[:, 0:ncol], ksrc[hf * 64:(hf + 1) * 64, j * 128:(j + 1) * 128], qT[hf * 64:(hf + 1) * 64, ch, q0:q1], True, True,
                                 [dkT[j // 4], dkTs] + [dqT[ch][b] for b in range(q0 // 512, (q1 - 1) // 512 + 1)], [dps])
                            lt, dlt = ltmp[kq % 2], dltmp[kq % 2]
                            kq += 1
                            S.tt("dve", lt[:, 0:ncol], ps[:, 0:ncol], bm[:, h, c0:c0 + ncol], ALU.add, [dps, dbm], [dlt])
                            S.act(e_t[:, h, c0:c0 + ncol], lt[:, 0:ncol], AF.Exp, [dlt], [de_t])
                        if j >= 2:
                            pv_block(j - 2)
                    pv_block(14)
                    pv_block(15)
                    S.barrier()
                    stage(S, 4)
                for half in range(2):
                    wt, dwt = load_w(w_view("ev_out_w", half * 512, (half + 1) * 512), 8, 512)
                    for d4 in range(4):
                        dc = half * 4 + d4
                        for tb in range(4):
                            tsl = slice(tb * 512, (tb + 1) * 512)
                            ps, dps = PSM.next()
                            for kc in range(8):
                                S.mm(ps[:], wt[:, kc, d4 * 128:(d4 + 1) * 128], yT[:, kc, tsl], kc == 0, kc == 7, [dwt, dyT[kc][tb]], [dps])
                            S.stt("dve", xT[:, dc, tsl], ps[:], modcol[0][:, 16 + dc:17 + dc], xT[:, dc, tsl], ALU.mult, ALU.add,
                                  [dps, dmod[0], dxT[dc][tb]], [dxT[dc][tb]])
                S.barrier()
          S.dead = False
          dump_x(0)

        if p0 <= 1 < p1:
            with ExitStack() as ph:
                modulate(ph, 0, 1, "b")

                def upd(po, dpo, dc, tb):
                    tsl = slice(tb * 512, (tb + 1) * 512)
                    S.stt("dve", xT[:, dc, tsl], po[:], modcol[0][:, 40 + dc:41 + dc], xT[:, dc, tsl], ALU.mult, ALU.add,
                          [dpo, dmod[0], dxT[dc][tb]], [dxT[dc][tb]])

                adab = make_adabufs(ph)
                nbq = list(range(12))

                def after_group():
                    for _ in range(2):
                        if nbq:
                            ada_block(1, nbq.pop(0), adab)

                swiglu(ph, lambda a, b: w_view("ev_ffn_w1", a, b), lambda a, b: w_view("ev_ffn_w3", a, b),
                       lambda f0, n: P["ev_ffn_w2"][f0 * 128:(f0 + n) * 128, :].rearrange("(j p) d -> p j d", p=128), 2816, upd, "f", after_group=after_group)
                while nbq:
                    ada_block(1, nbq.pop(0), adab)
                ada_finish(1)
                S.barrier()
            dump_x(1)

        if p0 <= 2 < p1:
            ssd_phase_inner()
            dump_x(2)

        if p0 <= 3 < p1:
            moe_phase_inner()
            dump_x(3)

        with ExitStack() as ph:
            ot = [sb(ph, "ot%d" % i, [128, 4, 1024], F32) for i in range(2)]
            dot = [Dep(), Dep()]
            dout = Dep()
            for tb in range(4):
                t, dt_ = ot[tb % 2], dot[tb % 2]
                for a in range(4):
                    for fq in range(2):
                        ps, dps = PSM.next()
                        for f4 in range(4):
                            fc = fq * 4 + f4
                            S.tr(ps[:, f4 * 128:(f4 + 1) * 128], xT[:, fc, tb * 512 + a * 128: tb * 512 + (a + 1) * 128], identf[:],
                                 [dxT[fc][tb], dcst["identf"]], [dps])
                        S.copy(evq.next(), t[:, a, fq * 512:(fq + 1) * 512], ps[:], [dps], [dt_])
                S.dma("sp", out_d[tb * 512:(tb + 1) * 512, :].rearrange("(a p) f -> p a f", p=128), t[:], reads=[dt_], writes=[dout])
            S.barrier()
        S.emit_all(block)
    nc._used_inputs = list(P.keys())
    return nc


def ssd_phase(nc, S, sb, P, C, L):
    raise NotImplementedError


def moe_phase(nc, S, sb, P, C, L):
    raise NotImplementedError


_CACHE = {}


def prep_inputs(inputs, b):
    m = {}
    f = lambda a: np.ascontiguousarray(np.asarray(a, dtype=np.float32))
    m["x"] = f(inputs["x"][b])
    m["c"] = f(inputs["c"][b]).reshape(8, 128)
    m["rel_bias"] = f(inputs["rel_bias"])
    for k, shp in PARAM_SHAPES.items():
        if k in m:
            continue
        m[k] = f(inputs[k][0]).reshape(shp)
    return m


def kernel(**inputs):
    if "nc" not in _CACHE:
        _CACHE["nc"] = build()
        _CACHE["consts"] = host_consts()
    nc = _CACHE["nc"]
    consts = _CACHE["consts"]
    in_maps = []
    for b in range(8):
        m = prep_inputs(inputs, b)
        m.update(consts)
        in_maps.append({k: m[k] for k in nc._used_inputs})
    res = run_bass_kernel_spmd(nc, in_maps, core_ids=list(range(8)))
    return np.stack([np.asarray(r["out"], dtype=np.float32) for r in res.results], 0)
```

```python
import numpy as np
import ml_dtypes
from contextlib import ExitStack
import concourse.bass as bass
import concourse.mybir as mybir
from concourse.bass_utils import run_bass_kernel_spmd

F32 = mybir.dt.float32
BF16 = mybir.dt.bfloat16
ALU = mybir.AluOpType
AF = mybir.ActivationFunctionType
ENGS = ("pe", "act", "dve", "pool", "sp")
S_LEN = 2048
D = 1024
EPS = 1e-6
NEG = -30000.0


class Dep:
    __slots__ = ("w", "r", "dsem", "dcnt", "wq")

    def __init__(self):
        self.wq = None
        self.w = None
        self.r = []
        self.dsem = None
        self.dcnt = 0


def deps(n):
    return [Dep() for _ in range(n)]


class Sched:
    def __init__(self, nc, stack):
        self.nc = nc
        self.stack = stack
        self.q = {e: [] for e in ENGS}
        self.cnt = {e: 0 for e in ENGS}
        self.sem = {e: stack.enter_context(nc.semaphore("s_" + e)) for e in ENGS}
        self.known = {e: {} for e in ENGS}
        self.same_wait = {"pe": False, "act": True, "dve": True, "pool": True, "sp": False}
        self.nsem = 0
        self.dma_deps = []
        self.dead = False

    def _waits(self, eng, reads, writes):
        evs = []
        for d in reads:
            if d.w is not None:
                evs.append(d.w)
        for d in writes:
            if d.w is not None:
                evs.append(d.w)
            evs.extend(d.r)
        waits = {}
        kn = self.known[eng]
        for (sem, val, e) in evs:
            if e == eng and not self.same_wait[eng]:
                continue
            if kn.get(id(sem), 0) >= val:
                continue
            cur = waits.get(id(sem))
            if cur is None or cur[1] < val:
                waits[id(sem)] = (sem, val)
        for k, (sem, val) in waits.items():
            kn[k] = val
        return list(waits.values())

    def op(self, eng, fn, reads=(), writes=()):
        if self.dead:
            return
        waits = self._waits(eng, reads, writes)
        self.cnt[eng] += 1
        sem = self.sem[eng]
        ev = (sem, self.cnt[eng], eng)

        def emit(e, waits=waits, fn=fn, sem=sem):
            for (s, v) in waits:
                e.wait_ge(s, v)
            fn(e).then_inc(sem, 1)

        self.q[eng].append(emit)
        for d in writes:
            d.w = ev
            d.r = []
        for d in reads:
            if d not in writes:
                d.r.append(ev)

    def dma(self, eng, out_ap, in_ap, reads=(), writes=(), **kw):
        if self.dead:
            return
        dst = writes[0]
        if dst.w is not None and dst.w[2] is None and dst.w[0] is dst.dsem and not dst.r and dst.wq == eng:
            waits = self._waits(eng, reads, ())
        else:
            waits = self._waits(eng, reads, writes)
        if dst.dsem is None:
            dst.dsem = self.stack.enter_context(self.nc.semaphore("d%d" % self.nsem))
            self.nsem += 1
            self.dma_deps.append(dst)
        dst.dcnt += 16
        dst.wq = eng
        ev = (dst.dsem, dst.dcnt, None)
        dsem = dst.dsem

        def emit(e, waits=waits, dsem=dsem, out_ap=out_ap, in_ap=in_ap, kw=kw):
            for (s, v) in waits:
                e.wait_ge(s, v)
            e.dma_start(out=out_ap, in_=in_ap, **kw).then_inc(dsem, 16)

        self.q[eng].append(emit)
        for d in writes:
            d.w = ev
            d.r = []
        for d in reads:
            d.r.append(ev)

    def barrier(self):
        if self.dead:
            return
        evs = [(self.sem[e], self.cnt[e]) for e in ENGS if self.cnt[e] > 0]
        evs += [(d.dsem, d.dcnt) for d in self.dma_deps]
        for eng in ENGS:
            kn = self.known[eng]
            waits = []
            for (sem, val) in evs:
                if sem is self.sem[eng]:
                    continue
                if kn.get(id(sem), 0) >= val:
                    continue
                kn[id(sem)] = val
                waits.append((sem, val))

            def emit(e, waits=waits):
                for (s, v) in waits:
                    e.wait_ge(s, v)

            self.q[eng].append(emit)

    def emit_all(self, block):
        m = {"pe": block.tensor, "act": block.scalar, "dve": block.vector, "pool": block.gpsimd, "sp": block.sync}
        for eng in ENGS:
            lst = self.q[eng]

            def body(e, lst=lst):
                for f in lst:
                    f(e)

            m[eng](body)

    def mm(self, out, lhsT, rhs, start, stop, r, w):
        self.op("pe", lambda e: e.matmul(out, lhsT=lhsT, rhs=rhs, start=start, stop=stop), r, w)

    def tr(self, out, in_, ident, r, w):
        self.op("pe", lambda e: e.transpose(out, in_, ident), r, w)

    def act(self, out, in_, func, r, w, bias=None, scale=None):
        kw = {}
        if bias is not None:
            kw["bias"] = bias
        if scale is not None:
            kw["scale"] = scale
        self.op("act", lambda e: e.activation(out=out, in_=in_, func=func, **kw), r, w)

    def copy(self, eng, out, in_, r, w):
        if eng == "act":
            self.op("act", lambda e: e.copy(out=out, in_=in_), r, w)
        else:
            self.op(eng, lambda e: e.tensor_copy(out=out, in_=in_), r, w)

    def tt(self, eng, out, in0, in1, op, r, w):
        self.op(eng, lambda e: e.tensor_tensor(out=out, in0=in0, in1=in1, op=op), r, w)

    def ts(self, eng, out, in0, s1, s2, op0, op1, r, w):
        if s2 is None:
            self.op(eng, lambda e: e.tensor_scalar(out=out, in0=in0, scalar1=s1, scalar2=None, op0=op0), r, w)
        else:
            self.op(eng, lambda e: e.tensor_scalar(out=out, in0=in0, scalar1=s1, scalar2=s2, op0=op0, op1=op1), r, w)

    def stt(self, eng, out, in0, scalar, in1, op0, op1, r, w):
        self.op(eng, lambda e: e.scalar_tensor_tensor(out=out, in0=in0, scalar=scalar, in1=in1, op0=op0, op1=op1), r, w)

    def memset(self, eng, ap, val, w):
        self.op(eng, lambda e: e.memset(ap, val), (), w)

    def recip(self, out, in_, r, w):
        self.op("dve", lambda e: e.reciprocal(out=out, in_=in_), r, w)


class _Stop(Exception):
    pass


STAGE = 99


def stage(S, k):
    if STAGE == k or (STAGE == 221 and k == 22):
        S.barrier()
        S.dead = True


class Ring:
    def __init__(self, items):
        self.items = items
        self.i = 0

    def next(self):
        it = self.items[self.i % len(self.items)]
        self.i += 1
        return it


def _bf(a):
    return np.ascontiguousarray(a.astype(np.float32)).astype(ml_dtypes.bfloat16)


def host_consts():
    c = {}
    c["identf"] = np.eye(128, dtype=np.float32)
    c["identb"] = _bf(np.eye(128))
    c["onesb"] = _bf(np.ones((128, 128)))
    c["onesf"] = np.ones((128, 128), np.float32)
    bd = np.zeros((128, 128), np.float32)
    bd[:64, :64] = 1
    bd[64:, 64:] = 1
    c["bd64"] = _bf(bd)
    k = np.arange(128)
    ang = 2 * np.pi * np.outer(k, k) / 128.0
    c["ccsc"] = _bf(np.concatenate([np.cos(ang), -np.sin(ang)], 1) / np.sqrt(128.0))
    n = np.arange(S_LEN)
    jk = np.outer(n, n) % S_LEN
    ang = 2 * np.pi * jk / float(S_LEN)
    c["cs_mat"] = _bf(np.cos(ang) / np.sqrt(float(S_LEN)))
    c["ss_mat"] = _bf(np.sin(ang) / np.sqrt(float(S_LEN)))
    i = np.arange(512)
    rel = 255 - i
    half, max_exact = 16, 8
    na = np.abs(rel)
    large = max_exact + (np.log(np.maximum(na, 1) / max_exact) / np.log(128 / max_exact) * (half - max_exact)).astype(np.int32)
    large = np.minimum(large, half - 1)
    bucket = (rel > 0).astype(np.int32) * half + np.where(na < max_exact, na, large)
    oh = np.zeros((32, 512), np.float32)
    oh[bucket, i] = 1.0
    oh[:, 511] = 0.0
    c["ohr"] = oh
    p = np.arange(128)[:, None]
    cc = np.arange(384)[None, :]
    relm = 128 + p - cc
    c["amask"] = np.where(np.abs(relm) <= 128, 0.0, NEG).astype(np.float32)
    s = np.arange(128)[:, None]
    l = np.arange(128)[None, :]
    c["trif"] = (s <= l).astype(np.float32)
    c["trib"] = (s >= l).astype(np.float32)
    c["maskf"] = _bf((l >= s).astype(np.float32))
    c["maskb"] = _bf((l <= s).astype(np.float32))
    c["negf"] = np.where(l >= s, 0.0, NEG).astype(np.float32)
    c["negb"] = np.where(l <= s, 0.0, NEG).astype(np.float32)
    sl = np.zeros((128, 128), np.float32)
    sl[127, :] = 1
    c["sellast"] = sl
    sf = np.zeros((128, 128), np.float32)
    sf[0, :] = 1
    c["selfirst"] = sf
    ee = np.zeros((8, 8, 128), np.float32)
    for e in range(8):
        ee[e, e, :] = 1
    c["esel"] = ee.reshape(8, 1024)
    return c


CONST_SHAPES = {
    "identf": ([128, 128], F32), "identb": ([128, 128], BF16), "onesb": ([128, 128], BF16), "onesf": ([128, 128], F32),
    "bd64": ([128, 128], BF16), "ccsc": ([128, 256], BF16), "cs_mat": ([2048, 2048], BF16), "ss_mat": ([2048, 2048], BF16),
    "ohr": ([32, 512], F32), "amask": ([128, 384], F32), "trif": ([128, 128], F32), "trib": ([128, 128], F32),
    "maskf": ([128, 128], BF16), "maskb": ([128, 128], BF16), "sellast": ([128, 128], F32), "selfirst": ([128, 128], F32),
    "esel": ([8, 1024], F32), "negf": ([128, 128], F32), "negb": ([128, 128], F32),
}

PARAM_SHAPES = {
    "x": [2048, 1024], "c": [8, 128], "rel_bias": [32, 8],
    "ev_ada_w": [1024, 6144], "ev_ada_b": [1, 6144], "ev_norm1_w": [8, 128], "ev_in_w": [1024, 1280],
    "ev_q_norm_w": [64, 1], "ev_k_norm_w": [64, 1], "ev_sink": [1, 8], "ev_out_w": [1024, 1024], "ev_norm2_w": [8, 128],
    "ev_ffn_w1": [1024, 2816], "ev_ffn_w3": [1024, 2816], "ev_ffn_w2": [2816, 1024],
    "od_ada_w": [1024, 6144], "od_ada_b": [1, 6144], "od_norm1_w": [8, 128], "od_in_w": [1024, 5184],
    "od_conv_w": [120, 128], "od_conv_b": [24, 128], "od_dt_bias_f": [1, 32], "od_dt_bias_b": [1, 32],
    "od_A_log_f": [1, 32], "od_A_log_b": [1, 32], "od_D": [1, 32], "od_gnorm_w": [16, 128], "od_out_w": [2048, 1024],
    "od_norm2_w": [8, 128], "od_router_w": [1024, 8], "od_router_b": [1, 8],
    "od_moe_w1": [8, 1024, 3584], "od_moe_w3": [8, 1024, 3584], "od_moe_w2": [8, 3584, 1024],
}


def build(p0=0, p1=4, dump=False):
    nc = bass.Bass("TRN2", target_bir_lowering=False)
    class _Lazy(dict):
        def __missing__(self, k):
            if k in PARAM_SHAPES:
                v = nc.dram_tensor(k, list(PARAM_SHAPES[k]), F32, kind="ExternalInput").ap()
            else:
                v = nc.dram_tensor(k, list(CONST_SHAPES[k][0]), CONST_SHAPES[k][1], kind="ExternalInput").ap()
            self[k] = v
            return v

    P = _Lazy()
    C = P
    out_d = nc.dram_tensor("out", [2048, 1024], F32, kind="ExternalOutput").ap()
    dump_d = [nc.dram_tensor("dump%d" % i, [2048, 1024], F32, kind="ExternalOutput").ap() for i in range(4)] if dump else None
    tscr = nc.dram_tensor("tscr", [8, 128, 512], F32, kind="Internal").ap()
    ygscr = nc.dram_tensor("ygscr", [2048, 2048], BF16, kind="ExternalOutput" if dump else "Internal").ap()
    acscr = nc.dram_tensor("acscr", [64, 2048], F32, kind="Internal").ap()
    xscr = nc.dram_tensor("xscr", [128, 8, 2048], F32, kind="Internal").ap()

    with ExitStack() as st:
        S = Sched(nc, st)

        def sb(stack, name, shape, dt):
            return stack.enter_context(nc.sbuf_tensor("sb_" + name, shape, dt))

        xT = sb(st, "xT", [128, 8, S_LEN], F32)
        dxT = [deps(4) for _ in range(8)]
        hT = sb(st, "hT", [128, 8, S_LEN], BF16)
        dhT = [deps(4) for _ in range(8)]
        cst = {}
        dcst = {}
        for k in ("identf", "identb", "onesb", "onesf", "bd64"):
            cst[k] = sb(st, "c_" + k, CONST_SHAPES[k][0], CONST_SHAPES[k][1])
            dcst[k] = Dep()
        modcol = [sb(st, "modcol%d" % i, [128, 48], F32) for i in range(2)]
        dmod = [Dep(), Dep()]
        pcol = sb(st, "pcol", [128, 72], F32)
        dpcol = Dep()
        acol = sb(st, "acol", [128, 4, 8], F32)
        dacol = Dep()
        cscol = sb(st, "cscol", [128, 8], F32)
        dcs = Dep()
        epsc = sb(st, "epsc", [128, 1], F32)
        depsc = Dep()
        wring_t = [sb(st, "wring%d" % i, [128, 8, 512], BF16) for i in range(3)]
        wring = Ring([(wring_t[i], Dep()) for i in range(3)])
        psf = [st.enter_context(nc.psum_tensor("psf%d" % i, [128, 512], F32)) for i in range(7)]
        psb_t = st.enter_context(nc.psum_tensor("psb", [128, 1024], BF16))
        dpsb = Dep()
        PSM = Ring([(psf[i], Dep()) for i in range(4)])
        PSA = Ring([(psf[i], Dep()) for i in range(4, 7)])
        block = st.enter_context(nc.Block())
        evq = Ring(["act", "dve"])
        WQ = Ring(["pool"])
        lastw = [None]

        identf, identb, onesb, onesf, bd64 = (cst[k] for k in ("identf", "identb", "onesb", "onesf", "bd64"))

        for k in cst:
            S.dma("sp", cst[k][:], C[k][:, :], writes=[dcst[k]])
        S.memset("dve", epsc[:], EPS, [depsc])

        akc = [0]

        def make_adabufs(stack):
            return dict(adat=[sb(stack, "adat%d_%d" % (akc[0], i), [128, 8, 512], F32) for i in range(2)], dadat=deps(2),
                        modrow=[sb(stack, "modrow%d_%d" % (akc[0], i), [1, 512], F32) for i in range(2)], dmr=deps(2),
                        brow=[sb(stack, "brow%d_%d" % (akc[0], i), [1, 512], F32) for i in range(2)], dbrow=deps(2), k=[0], tag=akc.__setitem__(0, akc[0] + 1))

        def ada_block(layer, nb, B_):
            nm = ("ev", "od")[layer]
            k = B_["k"][0]
            B_["k"][0] += 1
            t, dt_ = B_["adat"][k % 2], B_["dadat"][k % 2]
            mr, dmr_ = B_["modrow"][k % 2], B_["dmr"][k % 2]
            br, dbr_ = B_["brow"][k % 2], B_["dbrow"][k % 2]
            S.dma("act", br[:], P[nm + "_ada_b"][:, nb * 512:(nb + 1) * 512], writes=[dbr_])
            S.dma("sp", t[:], P[nm + "_ada_w"].rearrange("(kc p) f -> p kc f", p=128)[:, :, nb * 512:(nb + 1) * 512], writes=[dt_])
            ps, dps = PSA.next()
            for kc in range(8):
                S.mm(ps[0:1, :], cscol[:, kc:kc + 1], t[:, kc, :], kc == 0, kc == 7, [dcs, dt_], [dps])
            S.tt("dve", mr[:], ps[0:1, :], br[:], ALU.add, [dps, dbr_], [dmr_])
            pc, dpc = PSA.next()
            for j4 in range(4):
                S.mm(pc[:, j4:j4 + 1], mr[0:1, j4 * 128:(j4 + 1) * 128], onesf[0:1, 0:1], True, True, [dmr_, dcst["onesf"]], [dpc])
            S.copy("dve", modcol[layer][:, nb * 4:(nb + 1) * 4], pc[:, 0:4], [dpc], [dmod[layer]])

        def ada_finish(layer):
            for sub in range(2):
                S.stt("dve", acol[:, 2 * layer + sub, :], modcol[layer][:, 8 + 24 * sub:16 + 24 * sub], 1.0,
                      pcol[:, 16 * layer + 8 * sub:16 * layer + 8 * sub + 8], ALU.add, ALU.mult, [dmod[layer], dpcol], [dacol])

        with ExitStack() as ph:
            xst = [sb(ph, "xst%d" % i, [128, 4, 1024], F32) for i in range(2)]
            dxst = [Dep(), Dep()]
            for tb in range(4):
                t, dt_ = xst[tb % 2], dxst[tb % 2]
                S.dma("sp", t[:], P["x"][tb * 512:(tb + 1) * 512, :].rearrange("(a p) f -> p a f", p=128), writes=[dt_])
                for fc in range(8):
                    ps, dps = PSM.next()
                    for a in range(4):
                        S.tr(ps[:, a * 128:(a + 1) * 128], t[:, a, fc * 128:(fc + 1) * 128], identf[:], [dt_, dcst["identf"]], [dps])
                    S.copy(evq.next(), xT[:, fc, tb * 512:(tb + 1) * 512], ps[:], [dps], [dxT[fc][tb]])

            S.barrier()
        with ExitStack() as ph:
            c8 = sb(ph, "c8", [8, 128], F32)
            dc8 = Dep()
            S.dma("sp", c8[:], P["c"][:, :], writes=[dc8])
            ps, dps = PSA.next()
            S.tr(ps[:, 0:8], c8[:], identf[0:8, 0:8], [dc8, dcst["identf"]], [dps])
            S.act(cscol[:], ps[:, 0:8], AF.Silu, [dps], [dcs])
            defer_od = (p0 <= 1 < p1)
            adabufs = make_adabufs(ph)
            for layer in range(2):
                if layer == 1 and defer_od:
                    continue
                for nb in range(12):
                    ada_block(layer, nb, adabufs)
            prow = sb(ph, "prow", [72, 128], F32)
            dprow = Dep()
            for r0, nm in ((0, "ev_norm1_w"), (8, "ev_norm2_w"), (16, "od_norm1_w"), (24, "od_norm2_w"), (32, "od_gnorm_w"), (48, "od_conv_b")):
                n = PARAM_SHAPES[nm][0]
                S.dma("sp", prow[r0:r0 + n, :], P[nm][:, :], writes=[dprow])
            ps, dps = PSA.next()
            S.tr(ps[:, 0:72], prow[:], identf[0:72, 0:72], [dprow, dcst["identf"]], [dps])
            S.copy("dve", pcol[:], ps[:, 0:72], [dps], [dpcol])
            for layer in range(2):
                if layer == 1 and defer_od:
                    continue
                ada_finish(layer)
            S.barrier()

        def modulate(ph, layer, sub, tag, fp32_out=None):
            sq = [sb(ph, "sq%s%d" % (tag, i), [128, 512], BF16) for i in range(3)]
            dsq = deps(3)
            rstd = sb(ph, "rstd" + tag, [128, S_LEN], F32)
            drstd = deps(4)
            tmp = [sb(ph, "mtmp%s%d" % (tag, i), [128, 512], F32) for i in range(2)]
            dtmp = [Dep(), Dep()]
            bcol0 = 0 + 24 * sub
            k = 0
            for tb in range(4):
                tsl = slice(tb * 512, (tb + 1) * 512)
                ps, dps = PSA.next()
                for fc in range(8):
                    i = k % 3
                    k += 1
                    S.act(sq[i][:], xT[:, fc, tsl], AF.Square, [dxT[fc][tb]], [dsq[i]])
                    S.mm(ps[:], onesb[:], sq[i][:], fc == 0, fc == 7, [dsq[i], dcst["onesb"]], [dps])
                S.act(rstd[:, tsl], ps[:], AF.Sqrt, [dps, depsc], [drstd[tb]], bias=epsc[:, 0:1], scale=1.0 / D)
                S.recip(rstd[:, tsl], rstd[:, tsl], [drstd[tb]], [drstd[tb]])
            k = 0
            for tb in range(4):
                tsl = slice(tb * 512, (tb + 1) * 512)
                for fc in range(8):
                    t, dt_ = tmp[k % 2], dtmp[k % 2]
                    k += 1
                    S.stt("dve", t[:], xT[:, fc, tsl], acol[:, 2 * layer + sub, fc:fc + 1], rstd[:, tsl], ALU.mult, ALU.mult,
                          [dxT[fc][tb], dacol, drstd[tb]], [dt_])
                    S.act(hT[:, fc, tsl], t[:], AF.Identity, [dt_, dmod[layer]], [dhT[fc][tb]],
                          bias=modcol[layer][:, bcol0 + fc:bcol0 + fc + 1], scale=1.0)
                    if fp32_out is not None:
                        fp32_out(fc, tb, t, dt_, modcol[layer][:, bcol0 + fc:bcol0 + fc + 1])

        def dbg(name, tile, shape, dt_, reads):
            if not dump:
                return
            d_ = nc.dram_tensor("dbg_" + name, list(shape), dt_, kind="ExternalOutput").ap()
            S.dma("sp", d_, tile[:], reads=reads, writes=[Dep()])

        def load_w(dram_ap_3d, kc_n, ncols):
            t, dt_ = wring.next()
            rd = [lastw[0]] if lastw[0] is not None and lastw[0] is not dt_ else []
            for kc in range(kc_n):
                S.dma(WQ.next(), t[:, kc, 0:ncols], dram_ap_3d[:, kc, :], reads=rd, writes=[dt_])
            lastw[0] = dt_
            return t, dt_

        def w_view(name, c0, c1):
            return P[name].rearrange("(kc p) f -> p kc f", p=128)[:, :, c0:c1]

        def swiglu(ph, w1v, w3v, w2v, F, upd, tag, gate=None, after_group=None):
            if getattr(ph, "_sw", None) is None:
                gT = sb(ph, "gT" + tag, [128, 4, S_LEN], BF16)
                dgT = [deps(4) for _ in range(4)]
                sl_ = [sb(ph, "sil%s%d" % (tag, i), [128, 512], BF16) for i in range(2)]
                dsl = [Dep(), Dep()]
                ph._sw = (gT, dgT, sl_, dsl)
            gT, dgT, sl_, dsl = ph._sw
            nfc_tot = F // 128
            fc0 = 0
            k = 0
            while fc0 < nfc_tot:
                nfc = min(4, nfc_tot - fc0)
                w1t, dw1 = load_w(w1v(fc0 * 128, (fc0 + nfc) * 128), 8, nfc * 128)
                w3t, dw3 = load_w(w3v(fc0 * 128, (fc0 + nfc) * 128), 8, nfc * 128)
                for j in range(nfc):
                    for tb in range(4):
                        tsl = slice(tb * 512, (tb + 1) * 512)
                        p1, dp1 = PSM.next()
                        p3, dp3 = PSM.next()
                        for kc in range(8):
                            S.mm(p1[:], w1t[:, kc, j * 128:(j + 1) * 128], hT[:, kc, tsl], kc == 0, kc == 7, [dw1, dhT[kc][tb]], [dp1])
                        for kc in range(8):
                            S.mm(p3[:], w3t[:, kc, j * 128:(j + 1) * 128], hT[:, kc, tsl], kc == 0, kc == 7, [dw3, dhT[kc][tb]], [dp3])
                        s_, ds_ = sl_[k % 2], dsl[k % 2]
                        k += 1
                        S.act(s_[:], p1[:], AF.Silu, [dp1], [ds_])
                        if gate is None:
                            S.tt("dve", gT[:, j, tsl], p3[:], s_[:], ALU.mult, [dp3, ds_], [dgT[j][tb]])
                        else:
                            gb_, dgb_ = gate(tb)
                            S.tt("dve", gT[:, j, tsl], p3[:], gb_, ALU.mult, [dp3, dgb_], [dgT[j][tb]])
                            S.tt("dve", gT[:, j, tsl], gT[:, j, tsl], s_[:], ALU.mult, [ds_, dgT[j][tb]], [dgT[j][tb]])
                w2t = []
                for half in range(2):
                    w2t.append(load_w(w2v(fc0, nfc)[:, :, half * 512:(half + 1) * 512], nfc, 512))
                for dc in range(8):
                    wt, dwt = w2t[dc // 4]
                    for tb in range(4):
                        tsl = slice(tb * 512, (tb + 1) * 512)
                        po, dpo = PSA.next()
                        for j in range(nfc):
                            S.mm(po[:], wt[:, j, (dc % 4) * 128:(dc % 4 + 1) * 128], gT[:, j, tsl], j == 0, j == nfc - 1, [dwt, dgT[j][tb]], [dpo])
                        upd(po, dpo, dc, tb)
                if after_group is not None:
                    after_group()
                fc0 += nfc

        def dump_x(idx):
            if not dump:
                return
            with ExitStack() as dph:
                ot = [sb(dph, "dot%d_%d" % (idx, i), [128, 4, 1024], F32) for i in range(2)]
                dot = [Dep(), Dep()]
                dd = Dep()
                for tb in range(4):
                    t, dt_ = ot[tb % 2], dot[tb % 2]
                    for a in range(4):
                        for fq in range(2):
                            ps, dps = PSM.next()
                            for f4 in range(4):
                                fc = fq * 4 + f4
                                S.tr(ps[:, f4 * 128:(f4 + 1) * 128], xT[:, fc, tb * 512 + a * 128: tb * 512 + (a + 1) * 128], identf[:],
                                     [dxT[fc][tb], dcst["identf"]], [dps])
                            S.copy(evq.next(), t[:, a, fq * 512:(fq + 1) * 512], ps[:], [dps], [dt_])
                    S.dma("sp", dump_d[idx][tb * 512:(tb + 1) * 512, :].rearrange("(a p) f -> p a f", p=128), t[:], reads=[dt_], writes=[dd])
                S.barrier()


        def moe_phase_inner():
            with ExitStack() as ph:
                rw = sb(ph, "rw", [128, 8, 8], F32)
                drw = Dep()
                S.dma("sp", rw[:], P["od_router_w"].rearrange("(kc p) e -> p kc e", p=128), writes=[drw])
                rbb = sb(ph, "m_rbb", [128, 8], F32)
                drbb = Dep()
                S.dma("sp", rbb[:], bass.AP(tensor=P["od_router_b"].tensor, offset=0, ap=[[0, 128], [1, 8]]), writes=[drbb])
                esel = sb(ph, "esel", [8, 8, 128], F32)
                desel = Dep()
                S.dma("sp", esel[:], P["esel"].rearrange("k (e m) -> k e m", e=8), writes=[desel])
                logit = sb(ph, "logit", [128, 16, 8], F32)
                dlogit = deps(4)
                gtok = sb(ph, "gtok", [128, 16, 8], F32)
                dgtok = deps(4)
                gT8 = sb(ph, "gT8", [8, S_LEN], F32)
                dgT8 = deps(4)
                with ExitStack() as ph2:
                    hfa = sb(ph2, "hfa", [128, 8, 512], F32)
                    dhfa = deps(8)

                    def fp32_out(fc, tb, t, dt_, bcol):
                        S.ts("dve", hfa[:, fc, :], t[:], bcol, None, ALU.add, None, [dt_, dmod[1]], [dhfa[fc]])
                        if fc == 7:
                            pl, dpl = PSM.next()
                            for tt in range(4):
                                for f2 in range(8):
                                    S.mm(pl[:, tt * 8:(tt + 1) * 8], hfa[:, f2, tt * 128:(tt + 1) * 128], rw[:, f2, :], f2 == 0, f2 == 7, [dhfa[f2], drw], [dpl])
                        if fc == 7:
                            S.tt("dve", logit[:, tb * 4:(tb + 1) * 4, :], pl[:, 0:32].rearrange("p (a b) -> p a b", b=8),
                                 rbb[:].unsqueeze(1).to_broadcast([128, 4, 8]), ALU.add, [dpl, drbb], [dlogit[tb]])

                    modulate(ph2, 1, 1, "m", fp32_out=fp32_out)
                    top8 = sb(ph2, "top8", [128, 8], F32)
                    nm1 = sb(ph2, "nm1", [128, 1], F32)
                    ex = sb(ph2, "ex", [128, 8], F32)
                    e2 = sb(ph2, "e2", [128, 1], F32)
                    msk = sb(ph2, "msk", [128, 8], F32)
                    dtp = Dep()
                    for tt in range(16):
                        lg = logit[:, tt, :]
                        dl = dlogit[tt // 4]
                        S.op("dve", lambda e, lg=lg: e.max(out=top8[:], in_=lg), [dl], [dtp])
                        S.ts("dve", nm1[:], top8[:, 0:1], -1.0, None, ALU.mult, None, [dtp], [dtp])
                        S.act(ex[:], lg, AF.Exp, [dl, dtp], [dtp], bias=nm1[:, 0:1], scale=1.0)
                        S.act(e2[:], top8[:, 1:2], AF.Exp, [dtp], [dtp], bias=nm1[:, 0:1], scale=1.0)
                        S.ts("dve", e2[:], e2[:], 1.0, None, ALU.add, None, [dtp], [dtp])
                        S.recip(e2[:], e2[:], [dtp], [dtp])
                        S.ts("dve", msk[:], lg, top8[:, 1:2], None, ALU.is_ge, None, [dl, dtp], [dtp])
                        S.stt("dve", gtok[:, tt, :], ex[:], e2[:, 0:1], msk[:], ALU.mult, ALU.mult, [dtp], [dgtok[tt // 4]])
                    for tb in range(4):
                        ps, dps = PSA.next()
                        for t4 in range(4):
                            S.tr(ps[0:8, t4 * 128:(t4 + 1) * 128], gtok[:, tb * 4 + t4, :], identf[:], [dgtok[tb], dcst["identf"]], [dps])
                        S.copy("dve", gT8[:, tb * 512:(tb + 1) * 512], ps[0:8, :], [dps], [dgT8[tb]])
                    S.barrier()
                gbc = [sb(ph, "gbc%d" % i, [128, 512], F32) for i in range(8)]
                dgbc = deps(8)
                utmp = [sb(ph, "utmp%d" % i, [128, 512], F32) for i in range(2)]
                dutmp = [Dep(), Dep()]
                ucnt = [0]
                for e in range(8):
                    base = (e % 2) * 4
                    for tb in range(4):
                        ps, dps = PSA.next()
                        S.mm(ps[:], esel[:, e, :], gT8[:, tb * 512:(tb + 1) * 512], True, True, [desel, dgT8[tb]], [dps])
                        S.copy("act", gbc[base + tb][:], ps[:], [dps], [dgbc[base + tb]])

                    def upd(po, dpo, dc, tb, base=base):
                        tsl = slice(tb * 512, (tb + 1) * 512)
                        S.stt("dve", xT[:, dc, tsl], po[:], modcol[1][:, 40 + dc:41 + dc], xT[:, dc, tsl], ALU.mult, ALU.add,
                              [dpo, dmod[1], dxT[dc][tb]], [dxT[dc][tb]])

                    def gate(tb, base=base):
                        return gbc[base + tb][:], dgbc[base + tb]

                    w1e = P["od_moe_w1"][e].rearrange("(kc p) f -> p kc f", p=128)
                    w3e = P["od_moe_w3"][e].rearrange("(kc p) f -> p kc f", p=128)
                    w2e = P["od_moe_w2"][e]
                    swiglu(ph, lambda a, b, w=w1e: w[:, :, a:b], lambda a, b, w=w3e: w[:, :, a:b],
                           lambda f0, n, w=w2e: w[f0 * 128:(f0 + n) * 128, :].rearrange("(j p) d -> p j d", p=128), 3584, upd, "m%d" % e, gate=gate)
                S.barrier()


        def ssd_main(ssacc, dssacc):
            with ExitStack() as ph:
                with ExitStack() as ph2:
                    modulate(ph2, 1, 0, "s")
                    dxscr = Dep()
                    for fc in range(8):
                        S.dma("sp", xscr[:, fc, :], xT[:, fc, :], reads=dxT[fc], writes=[dxscr])
                    S.barrier()
                cwcol = sb(ph, "cwcol", [128, 120], F32)
                dcw = Dep()
                dtb = sb(ph, "dtb", [128, 64], F32)
                abc = sb(ph, "abc", [128, 64], F32)
                dbc = sb(ph, "dbc", [128, 32], F32)
                dprm = Dep()
                cst2 = {}
                for k in ("trif", "trib", "sellast", "selfirst", "negf", "negb"):
                    cst2[k] = sb(ph, "c_" + k, [128, 128], F32)
                wdtt = sb(ph, "wdtt", [128, 8, 64], BF16)
                dwdt = Dep()
                for kc in range(8):
                    S.dma("pool", wdtt[:, kc, :], w_view("od_in_w", 5120, 5184)[:, kc, :], writes=[dwdt])
                dcst2 = Dep()
                for k in cst2:
                    S.dma("sp", cst2[k][:], P[k][:, :], writes=[dcst2])
                with ExitStack() as ph2:
                    cwrow = sb(ph2, "cwrow", [120, 128], F32)
                    dcwr = Dep()
                    S.dma("sp", cwrow[:], P["od_conv_w"][:, :], writes=[dcwr])
                    ps, dps = PSA.next()
                    S.tr(ps[:, 0:120], cwrow[:], identf[0:120, 0:120], [dcwr, dcst["identf"]], [dps])
                    S.copy("dve", cwcol[:], ps[:, 0:120], [dps], [dcw])
                    bc = lambda nm, n: bass.AP(tensor=P[nm].tensor, offset=0, ap=[[0, 128], [1, n]])
                    S.dma("sp", dtb[:, 0:32], bc("od_dt_bias_f", 32), writes=[dprm])
                    S.dma("sp", dtb[:, 32:64], bc("od_dt_bias_b", 32), writes=[dprm])
                    S.dma("sp", abc[:, 0:32], bc("od_A_log_f", 32), writes=[dprm])
                    S.dma("sp", abc[:, 32:64], bc("od_A_log_b", 32), writes=[dprm])
                    S.dma("sp", dbc[:], bc("od_D", 32), writes=[dprm])
                    S.act(abc[:], abc[:], AF.Exp, [dprm], [dprm])
                    S.ts("dve", abc[:], abc[:], -1.0, None, ALU.mult, None, [dprm], [dprm])
                    S.barrier()
                def _dtset(i):
                    if i == 0:
                        return [sb(ph, "dtset0_%d" % k, [128, 16, 16], F32) for k in range(5)]
                    return [xT[:, 6, k * 256:(k + 1) * 256].rearrange("p (a b) -> p a b", b=16) for k in range(5)]
                dtsets = [_dtset(0), _dtset(1)]
                ddts = deps(2)
                dacscrs = deps(2)
                ddtmp = Dep()
                a_tok = sb(ph, "a_tok", [128, 16, 16], F32)
                acT = [sb(ph, "acT%d" % i, [16, 512], F32) for i in range(2)]
                dacT = deps(2)
                tmpd = sb(ph, "tmpd", [128, 16], F32)
                dtmpd = Dep()
                dtbg = sb(ph, "dtbg", [128, 16], F32)
                abcg = sb(ph, "abcg", [128, 16], F32)

                def dt_path(g):
                    dt_tok, acum, ea, dec, cdb = dtsets[g % 2]
                    ddt = ddts[g % 2]
                    dacscr = dacscrs[g % 2]
                    r0 = (g % 2) * 16
                    for d in range(2):
                        S.copy("dve", dtbg[:, d * 8:(d + 1) * 8], dtb[:, d * 32 + g * 8:d * 32 + g * 8 + 8], [dprm], [ddtmp])
                        S.copy("dve", abcg[:, d * 8:(d + 1) * 8], abc[:, d * 32 + g * 8:d * 32 + g * 8 + 8], [dprm], [ddtmp])
                    for tt in range(16):
                        ps, dps = PSM.next()
                        for d in range(2):
                            for kc in range(8):
                                S.mm(ps[:, d * 8:(d + 1) * 8], hT[:, kc, tt * 128:(tt + 1) * 128], wdtt[:, kc, d * 32 + g * 8:d * 32 + g * 8 + 8], kc == 0, kc == 7,
                                     [dhT[kc][tt // 4], dwdt], [dps])
                        S.tt("dve", tmpd[:], ps[:, 0:16], dtbg[:], ALU.add, [dps, ddtmp], [dtmpd])
                        S.act(tmpd[:], tmpd[:], AF.Exp, [dtmpd], [dtmpd])
                        S.act(dt_tok[:, tt, :], tmpd[:], AF.Ln, [dtmpd, dcst["onesf"]], [ddt], bias=onesf[:, 0:1], scale=1.0)
                        S.tt("dve", a_tok[:, tt, :], dt_tok[:, tt, :], abcg[:], ALU.mult, [ddt, ddtmp], [ddtmp])
                    for c in range(16):
                        ps, dps = PSM.next()
                        S.mm(ps[:, 0:8], cst2["trif"][:], a_tok[:, c, 0:8], True, True, [dcst2, ddtmp], [dps])
                        S.mm(ps[:, 8:16], cst2["trib"][:], a_tok[:, c, 8:16], True, True, [dcst2, ddtmp], [dps])
                        S.copy("dve", acum[:, c, :], ps[:, 0:16], [dps], [ddt])
                        S.act(ea[:, c, :], ps[:, 0:16], AF.Exp, [dps], [ddt])
                        p2, dp2 = PSM.next()
                        S.mm(p2[:, 0:8], cst2["sellast"][:], acum[:, c, 0:8], True, True, [dcst2, ddt], [dp2])
                        S.mm(p2[:, 8:16], cst2["selfirst"][:], acum[:, c, 8:16], True, True, [dcst2, ddt], [dp2])
                        S.act(cdb[:, c, :], p2[:, 0:16], AF.Exp, [dp2], [ddt])
                        S.tt("dve", tmpd[:], p2[:, 0:16], acum[:, c, :], ALU.subtract, [dp2, ddt], [dtmpd])
                        S.act(dec[:, c, :], tmpd[:], AF.Exp, [dtmpd], [ddt])
                    for c4 in range(4):
                        ps, dps = PSA.next()
                        for c1 in range(4):
                            c = c4 * 4 + c1
                            S.tr(ps[0:16, c1 * 128:(c1 + 1) * 128], acum[:, c, :], identf[:], [ddt, dcst["identf"]], [dps])
                        S.copy("dve", acT[c4 % 2][:], ps[0:16, :], [dps], [dacT[c4 % 2]])
                        S.dma("sp", acscr[r0:r0 + 16, c4 * 512:(c4 + 1) * 512], acT[c4 % 2][:], reads=[dacT[c4 % 2]], writes=[dacscr])

                raw = sb(ph, "craw", [128, S_LEN + 4], BF16)
                draw = Dep()
                S.memset("pool", raw[:, 0:2], 0.0, [draw])
                S.memset("pool", raw[:, S_LEN + 2:S_LEN + 4], 0.0, [draw])

                dgs = [xT[:, 4, i * 320:(i + 1) * 320].bitcast(BF16).rearrange("p (a b) -> p a b", b=128) for i in range(2)]
                ddgs = deps(2)
                kdg = [0]

                def conv_chunk(wt, dwt, wcol0, cchunk, dst_ap, ddst):
                    i = kdg[0] % 2
                    kdg[0] += 1
                    dg, ddg = dgs[i], ddgs[i]
                    for w in range(5):
                        S.ts("dve", dg[:, w, :], identb[:], cwcol[:, w * 24 + cchunk:w * 24 + cchunk + 1], None, ALU.mult, None, [dcst["identb"], dcw], [ddg])
                    for tb in range(4):
                        tsl = slice(tb * 512, (tb + 1) * 512)
                        ps, dps = PSM.next()
                        for kc in range(8):
                            S.mm(ps[:], wt[:, kc, wcol0:wcol0 + 128], hT[:, kc, tsl], kc == 0, kc == 7, [dwt, dhT[kc][tb]], [dps])
                        S.copy("act", raw[:, 2 + tb * 512:2 + (tb + 1) * 512], ps[:], [dps], [draw])
                    for tb in range(4):
                        tsl = slice(tb * 512, (tb + 1) * 512)
                        pc, dpc = PSM.next()
                        for w in range(5):
                            S.mm(pc[:], dg[:, w, :], raw[:, w + tb * 512:w + tb * 512 + 512], w == 0, w == 4, [ddg, draw], [dpc])
                        S.act(dst_ap[:, tsl], pc[:], AF.Silu, [dpc, dpcol], ddst, bias=pcol[:, 48 + cchunk:49 + cchunk], scale=1.0)

                BT = sb(ph, "BT", [128, S_LEN], BF16)
                CT = sb(ph, "CT", [128, S_LEN], BF16)
                dBT, dCT = Dep(), Dep()
                Btok = sb(ph, "Btok", [128, 16, 128], BF16)
                dBtok = Dep()
                cbt = sb(ph, "cbt", [128, 16, 128], BF16)
                dcbt = Dep()
                xsT = sb(ph, "xsT", [128, S_LEN], BF16)
                dxsT = Dep()
                Xd = [sb(ph, "Xd%d" % i, [128, 16, 128], BF16) for i in range(2)]
                dXd = Dep()
                yaccs = [xT[:, 1, :].rearrange("p (a b) -> p a b", b=128), xT[:, 5, :].rearrange("p (a b) -> p a b", b=128)]
                dyaccs = [deps(16), deps(16)]
                arow = [xT[:, 0, i * 1024:(i + 1) * 1024].rearrange("p (a b) -> p a b", b=512) for i in range(2)]
                darow = deps(2)
                Hsr = [xT[:, 3, i * 128:(i + 1) * 128] for i in range(8)]
                dHsr = deps(8)
                lt = [sb(ph, "lt%d" % i, [128, 2, 512], BF16) for i in range(2)]
                dlt = deps(2)
                S_all = sb(ph, "S_all", [128, 2, 16, 128], BF16)
                dSall = [deps(4) for _ in range(2)]
                Hb_all = sb(ph, "Hb_all", [128, 2, 16, 128], BF16)
                dHball = [deps(16) for _ in range(2)]
                S.memset("dve", Hb_all[:, 0, 0, :], 0.0, [dHball[0][0]])
                S.memset("dve", Hb_all[:, 1, 15, :], 0.0, [dHball[1][15]])
                xdr = [sb(ph, "xdr%d" % i, [128, 128], BF16) for i in range(4)]
                dxdr = deps(4)
                szt2 = [sb(ph, "szt%d" % i, [128, 512], BF16) for i in range(2)]
                ygf2 = [xT[:, 3, 1024 + i * 512:1024 + (i + 1) * 512] for i in range(2)]
                sqg2 = [sb(ph, "sqg%d" % i, [128, 512], BF16) for i in range(2)]
                dgate2 = [deps(3) for _ in range(2)]
                ygb = [xT[:, 4, 1024 + i * 256:1024 + (i + 1) * 256].bitcast(BF16) for i in range(2)]
                dygb = deps(2)
                dygscr = Dep()
                kit = [0]
                kyg = [0]
                kxd = [0]
                khs = [0]
                kcc = [0]
                tmpc = [xT[:, 4, 1536:2048], xT[:, 6, 1280:1792]]
                dtmpc = deps(2)

                def prep_conv(g, j, wx, dwx):
                    conv_chunk(wx, dwx, j * 128, g * 4 + j, xsT, [dxsT])

                def prep_evac(g, j, yacc, dyacc):
                    dt_tok = dtsets[g % 2][0]
                    ddt = ddts[g % 2]
                    fcx = g * 4 + j
                    h0 = 8 * g + 2 * j
                    for t8 in range(2):
                        for t1_ in range(8):
                            tt = t8 * 8 + t1_
                            S.tr(psb_t[:, t1_ * 128:(t1_ + 1) * 128], xsT[:, tt * 128:(tt + 1) * 128], identb[:], [dxsT, dcst["identb"]], [dpsb])
                        src8 = psb_t[:, :].rearrange("p (t a b) -> p t a b", t=8, a=2)
                        tsl8 = slice(t8 * 8, (t8 + 1) * 8)
                        for d in range(2):
                            hd0 = d * 8 + 2 * j
                            S.tt("dve", Xd[d][:, tsl8, :].rearrange("p t (a b) -> p t a b", a=2), src8,
                                 dt_tok[:, tsl8, hd0:hd0 + 2].unsqueeze(3).to_broadcast([128, 8, 2, 64]), ALU.mult, [dpsb, ddt], [dXd])
                        S.tt("dve", yacc[:, tsl8, :].rearrange("p t (a b) -> p t a b", a=2), src8,
                             dbc[:, h0:h0 + 2].unsqueeze(1).unsqueeze(3).to_broadcast([128, 8, 2, 64]), ALU.mult, [dpsb, dprm], dyacc[t8 * 8:(t8 + 1) * 8])
                    if fcx == 0:
                        dbg("xsT", xsT, [128, S_LEN], BF16, [dxsT])
                        dbg("Xf", Xd[0], [128, 16, 128], BF16, [dXd])
                        dbg("Xb", Xd[1], [128, 16, 128], BF16, [dXd])
                        dbg("Btok", Btok, [128, 16, 128], BF16, [dBtok])
                        dbg("cbt", cbt, [128, 16, 128], BF16, [dcbt])

                def phase_a(g, j, yacc, dyacc):
                    dt_tok, acum, ea, dec, cdb = dtsets[g % 2]
                    ddt = ddts[g % 2]
                    dacscr = dacscrs[g % 2]
                    r0 = (g % 2) * 16

                    def stage1(d, b4):
                        hd0 = d * 8 + 2 * j
                        negm = cst2["negf" if d == 0 else "negb"]
                        c0 = b4 * 4
                        k = kit[0]
                        kit[0] += 1
                        ar, dar = arow[k % 2], darow[k % 2]
                        lt_, dlt_ = lt[k % 2], dlt[k % 2]
                        for hh in range(2):
                            S.dma("sp", ar[:, hh, :], bass.AP(tensor=acscr.tensor, offset=(r0 + hd0 + hh) * S_LEN + c0 * 128, ap=[[0, 128], [1, 512]]),
                                  reads=[dacscr], writes=[dar])
                        for hh in range(2):
                            av = ar[:, hh, :].rearrange("p (a b) -> p a b", b=128)
                            S.tt("dve", av, av, acum[:, c0:c0 + 4, hd0 + hh:hd0 + hh + 1].to_broadcast([128, 4, 128]), ALU.subtract, [dar, ddt], [dar])
                        av8 = ar[:].rearrange("p h (a b) -> p (h a) b", b=128)
                        S.tt("dve", av8, av8, negm[:].unsqueeze(1).to_broadcast([128, 8, 128]), ALU.min, [dar, dcst2], [dar])
                        S.act(lt_[:], ar[:], AF.Exp, [dar], [dlt_])
                        return (d, b4, lt_, dlt_)

                    def stage2(st_):
                        d, b4, lt_, dlt_ = st_
                        hd0 = d * 8 + 2 * j
                        c0 = b4 * 4
                        S.tt("dve", lt_[:], lt_[:], cbt[:, c0:c0 + 4, :].rearrange("p a b -> p (a b)").unsqueeze(1).to_broadcast([128, 2, 512]), ALU.mult,
                             [dlt_, dcbt], [dlt_])
                        p1, dp1 = PSM.next()
                        for c in range(4):
                            for hh in range(2):
                                S.mm(p1[:, c * 128 + hh * 64:c * 128 + (hh + 1) * 64], lt_[:, hh, c * 128:(c + 1) * 128],
                                     Xd[d][:, c0 + c, hh * 64:(hh + 1) * 64], True, True, [dlt_, dXd], [dp1])
                        p3, dp3 = PSA.next()
                        for c in range(4):
                            kx = kxd[0] % 4
                            kxd[0] += 1
                            S.tt("pool", xdr[kx][:].rearrange("p (a b) -> p a b", b=64), Xd[d][:, c0 + c, :].rearrange("p (a b) -> p a b", b=64),
                                 dec[:, c0 + c, hd0:hd0 + 2].unsqueeze(2).to_broadcast([128, 2, 64]), ALU.mult, [dXd, ddt], [dxdr[kx]])
                            S.mm(p3[:, c * 128:(c + 1) * 128], Btok[:, c0 + c, :], xdr[kx][:], True, True, [dBtok, dxdr[kx]], [dp3])
                        S.tt("dve", yacc[:, c0:c0 + 4, :], p1[:].rearrange("p (a b) -> p a b", b=128), yacc[:, c0:c0 + 4, :], ALU.add,
                             [dp1] + dyacc[c0:c0 + 4], dyacc[c0:c0 + 4])
                        S.copy("act", S_all[:, d, c0:c0 + 4, :], p3[:].rearrange("p (a b) -> p a b", b=128), [dp3], [dSall[d][b4]])

                    prev_st = None
                    for (d, b4) in [(d, b4) for d in range(2) for b4 in range(4)]:
                        st_ = stage1(d, b4)
                        if prev_st is not None:
                            stage2(prev_st)
                        prev_st = st_
                    stage2(prev_st)

                def phase_b(g, j):
                    dt_tok, acum, ea, dec, cdb = dtsets[g % 2]
                    ddt = ddts[g % 2]
                    hprev = [None, None]
                    for step in range(16):
                        for d in range(2):
                            hd0 = d * 8 + 2 * j
                            c = step if d == 0 else 15 - step
                            kh = khs[0] % 8
                            khs[0] += 1
                            hn, dhn = Hsr[kh], dHsr[kh]
                            if step == 0:
                                S.copy("dve", hn, S_all[:, d, c, :], [dSall[d][c // 4]], [dhn])
                            else:
                                hp, dhp = hprev[d]
                                for hh in range(2):
                                    S.stt("dve", hn[:, hh * 64:(hh + 1) * 64], hp[:, hh * 64:(hh + 1) * 64], cdb[:, c, hd0 + hh:hd0 + hh + 1],
                                          S_all[:, d, c, hh * 64:(hh + 1) * 64], ALU.mult, ALU.add, [dhp, ddt, dSall[d][c // 4]], [dhn])
                            if step < 15:
                                cn = c + 1 if d == 0 else c - 1
                                S.copy("act", Hb_all[:, d, cn, :], hn, [dhn], [dHball[d][cn]])
                            hprev[d] = (hn, dhn)

                def phase_c(g, j, yacc, dyacc):
                    dt_tok, acum, ea, dec, cdb = dtsets[g % 2]
                    ddt = ddts[g % 2]
                    for d in range(2):
                        hd0 = d * 8 + 2 * j
                        for b4 in range(4):
                            c0 = b4 * 4
                            p2, dp2 = PSM.next()
                            for c in range(4):
                                csl = slice((c0 + c) * 128, (c0 + c + 1) * 128)
                                S.mm(p2[:, c * 128:(c + 1) * 128], CT[:, csl], Hb_all[:, d, c0 + c, :], True, True, [dCT, dHball[d][c0 + c]], [dp2])
                            kc_ = kcc[0] % 2
                            kcc[0] += 1
                            tc_, dtc_ = tmpc[kc_], dtmpc[kc_]
                            S.tt("dve", tc_.rearrange("p (c a b) -> p c a b", c=4, a=2), p2[:].rearrange("p (c a b) -> p c a b", c=4, a=2),
                                 ea[:, c0:c0 + 4, hd0:hd0 + 2].unsqueeze(3).to_broadcast([128, 4, 2, 64]), ALU.mult, [dp2, ddt], [dtc_])
                            S.tt("dve", yacc[:, c0:c0 + 4, :], yacc[:, c0:c0 + 4, :], tc_.rearrange("p (c x) -> p c x", c=4), ALU.add,
                                 [dtc_] + dyacc[c0:c0 + 4], dyacc[c0:c0 + 4])

                def gating(g, j, wz, dwz, yacc, dyacc):
                    fcx = g * 4 + j
                    if fcx == 0:
                        dbg("yacc", xT[:, 1, :], [128, S_LEN], F32, dyacc)

                    def g1(tb):
                        tsl = slice(tb * 512, (tb + 1) * 512)
                        py, dpy = PSM.next()
                        for t4 in range(4):
                            tt = tb * 4 + t4
                            S.tr(py[:, t4 * 128:(t4 + 1) * 128], yacc[:, tt, :], identf[:], [dyacc[tt], dcst["identf"]], [dpy])
                        pz, dpz = PSM.next()
                        for kc in range(8):
                            S.mm(pz[:], wz[:, kc, j * 128:(j + 1) * 128], hT[:, kc, tsl], kc == 0, kc == 7, [dwz, dhT[kc][tb]], [dpz])
                        ig = kyg[0] % 2
                        kyg[0] += 1
                        szt, ygf, sqg = szt2[ig], ygf2[ig], sqg2[ig]
                        dsz_, dyg_, dsq_ = dgate2[ig]
                        S.act(szt[:], pz[:], AF.Silu, [dpz], [dsz_])
                        S.tt("dve", ygf, py[:], szt[:], ALU.mult, [dpy, dsz_], [dyg_])
                        S.act(sqg[:], ygf, AF.Square, [dyg_], [dsq_])
                        return (tb, ig)

                    def g2(st_):
                        tb, ig = st_
                        tsl = slice(tb * 512, (tb + 1) * 512)
                        szt, ygf, sqg = szt2[ig], ygf2[ig], sqg2[ig]
                        dsz_, dyg_, dsq_ = dgate2[ig]
                        pq, dpq = PSA.next()
                        S.mm(pq[:], onesb[:], sqg[:], True, True, [dsq_, dcst["onesb"]], [dpq])
                        if fcx == 0:
                            S.copy("dve", ssacc[:, tsl], pq[:], [dpq], [dssacc[tb]])
                        else:
                            S.tt("dve", ssacc[:, tsl], pq[:], ssacc[:, tsl], ALU.add, [dpq, dssacc[tb]], [dssacc[tb]])
                        S.ts("dve", ygb[ig][:], ygf, pcol[:, 32 + fcx:33 + fcx], None, ALU.mult, None, [dyg_, dpcol], [dygb[ig]])
                        S.dma("sp", ygscr[fcx * 128:(fcx + 1) * 128, tsl], ygb[ig][:], reads=[dygb[ig]], writes=[dygscr])

                    prev = None
                    for tb in range(4):
                        st_ = g1(tb)
                        if prev is not None:
                            g2(prev)
                        prev = st_
                    g2(prev)

                BTs = [BT, xT[:, 2, 0:1024].bitcast(BF16)]
                CTs = [CT, xT[:, 2, 1024:2048].bitcast(BF16)]
                Btoks = [Btok, xT[:, 7, 0:1024].bitcast(BF16).rearrange("p (a b) -> p a b", b=128)]
                cbts = [cbt, xT[:, 7, 1024:2048].bitcast(BF16).rearrange("p (a b) -> p a b", b=128)]
                dBTs, dCTs, dBtoks, dcbts = deps(2), deps(2), deps(2), deps(2)
                (wzt, dwz), (wxt, dwx), (wbct, dwbc) = wring.items

                def load_into(t, dt_, view, ncols, col0=0):
                    for kc in range(8):
                        S.dma("pool", t[:, kc, col0:col0 + ncols], view[:, kc, :], writes=[dt_])

                def group_prologue(g):
                    s_ = g % 2
                    load_into(wxt, dwx, w_view("od_in_w", 2048 + g * 512, 2048 + (g + 1) * 512), 512)
                    load_into(wbct, dwbc, w_view("od_in_w", 4096 + g * 128, 4096 + (g + 1) * 128), 128, 0)
                    load_into(wbct, dwbc, w_view("od_in_w", 4608 + g * 128, 4608 + (g + 1) * 128), 128, 128)
                    conv_chunk(wbct, dwbc, 0, 16 + g, BTs[s_], [dBTs[s_]])
                    conv_chunk(wbct, dwbc, 128, 20 + g, CTs[s_], [dCTs[s_]])
                    if g == 0:
                        dbg("BT", BTs[0], [128, S_LEN], BF16, [dBTs[0]])
                        dbg("CT", CTs[0], [128, S_LEN], BF16, [dCTs[0]])
                    for t8 in range(2):
                        for t1_ in range(8):
                            tt = t8 * 8 + t1_
                            S.tr(psb_t[:, t1_ * 128:(t1_ + 1) * 128], BTs[s_][:, tt * 128:(tt + 1) * 128], identb[:], [dBTs[s_], dcst["identb"]], [dpsb])
                        S.copy("act", Btoks[s_][:, t8 * 8:(t8 + 1) * 8, :], psb_t[:, :].rearrange("p (a b) -> p a b", b=128), [dpsb], [dBtoks[s_]])
                    for c in range(16):
                        ps, dps = PSM.next()
                        csl = slice(c * 128, (c + 1) * 128)
                        S.mm(ps[:, 0:128], BTs[s_][:, csl], CTs[s_][:, csl], True, True, [dBTs[s_], dCTs[s_]], [dps])
                        S.copy("act", cbts[s_][:, c, :], ps[:, 0:128], [dps], [dcbts[s_]])

                dt_path(0)
                group_prologue(0)
                for g in range(4):
                    s_ = g % 2
                    BT, CT, Btok, cbt = BTs[s_], CTs[s_], Btoks[s_], cbts[s_]
                    dBT, dCT, dBtok, dcbt = dBTs[s_], dCTs[s_], dBtoks[s_], dcbts[s_]
                    if g == 0:
                        for nm_, t_ in zip(("dt", "acum", "ea", "dec", "cdb"), dtsets[0]):
                            dbg(nm_, t_, [128, 16, 16], F32, [ddts[0]])
                    load_into(wzt, dwz, w_view("od_in_w", g * 512, (g + 1) * 512), 512)
                    prep_conv(g, 0, wxt, dwx)
                    prep_evac(g, 0, yaccs[0], dyaccs[0])
                    for j in range(4):
                        ya, dya = yaccs[j % 2], dyaccs[j % 2]
                        phase_a(g, j, ya, dya)
                        if j < 3:
                            prep_conv(g, j + 1, wxt, dwx)
                        if j == 2 and g < 3:
                            group_prologue(g + 1)
                        phase_b(g, j)
                        if j < 3:
                            prep_evac(g, j + 1, yaccs[(j + 1) % 2], dyaccs[(j + 1) % 2])
                        elif g < 3:
                            dt_path(g + 1)
                        phase_c(g, j, ya, dya)
                        gating(g, j, wzt, dwz, ya, dya)
                S.barrier()
                for fc in range(8):
                    S.dma("sp", xT[:, fc, :], xscr[:, fc, :], reads=[], writes=dxT[fc])

        def ssd_out(ssacc, dssacc):
            with ExitStack() as ph2:
                rs = sb(ph2, "rs_o", [128, S_LEN], F32)
                drs = Dep()
                S.act(rs[:], ssacc[:], AF.Sqrt, dssacc + [depsc], [drs], bias=epsc[:, 0:1], scale=1.0 / 2048)
                S.recip(rs[:], rs[:], [drs], [drs])
                dbg("rs", rs, [128, S_LEN], F32, [drs])
                ygt = [sb(ph2, "ygt%d" % i, [128, 16, 512], BF16) for i in range(2)]
                dygt = deps(2)
                ot1 = [sb(ph2, "ot1_%d" % i, [128, 512], F32) for i in range(2)]
                dot1 = deps(2)
                ko = 0
                for tb in range(4):
                    tsl = slice(tb * 512, (tb + 1) * 512)
                    yt, dyt = ygt[tb % 2], dygt[tb % 2]
                    for kc in range(16):
                        S.dma("sp", yt[:, kc, :], ygscr[kc * 128:(kc + 1) * 128, tsl], reads=[], writes=[dyt])
                    for half in range(2):
                        wa, dwa = load_w(P["od_out_w"][0:1024, half * 512:(half + 1) * 512].rearrange("(kc p) f -> p kc f", p=128), 8, 512)
                        wb_, dwb_ = load_w(P["od_out_w"][1024:2048, half * 512:(half + 1) * 512].rearrange("(kc p) f -> p kc f", p=128), 8, 512)
                        for d4 in range(4):
                            dc = half * 4 + d4
                            ps, dps = PSM.next()
                            for kc in range(16):
                                wt_, dwt_ = (wa, dwa) if kc < 8 else (wb_, dwb_)
                                S.mm(ps[:], wt_[:, kc % 8, d4 * 128:(d4 + 1) * 128], yt[:, kc, :], kc == 0, kc == 15, [dwt_, dyt], [dps])
                            o1, do1 = ot1[ko % 2], dot1[ko % 2]
                            ko += 1
                            S.tt("dve", o1[:], ps[:], rs[:, tsl], ALU.mult, [dps, drs], [do1])
                            S.stt("dve", xT[:, dc, tsl], o1[:], modcol[1][:, 16 + dc:17 + dc], xT[:, dc, tsl], ALU.mult, ALU.add,
                                  [do1, dmod[1], dxT[dc][tb]], [dxT[dc][tb]])
                S.barrier()


        def ssd_phase_inner():
            with ExitStack() as pho:
                ssacc = sb(pho, "ssacc", [128, S_LEN], F32)
                dssacc = deps(4)
                ssd_main(ssacc, dssacc)
                ssd_out(ssacc, dssacc)

        if p0 <= 0 < p1:
          if True:
            with ExitStack() as ph:
                with ExitStack() as ph2:
                    modulate(ph2, 0, 0, "a")
                    S.barrier()
                    stage(S, 1)
                qT = sb(ph, "qT", [128, 4, S_LEN], BF16)
                dqT = [deps(4) for _ in range(4)]
                kT = sb(ph, "kT", [128, S_LEN], BF16)
                dkT = deps(4)
                kTs = sb(ph, "kTs", [128, S_LEN], BF16)
                dkTs = Dep()
                vtok = sb(ph, "vtok", [128, 16, 2, 65], BF16)
                dvtok = deps(16)
                wqk = sb(ph, "wqk", [128, 2], F32)
                dwqk = Dep()
                ph_u = ExitStack()
                ph.enter_context(ph_u)
                uT = sb(ph_u, "uT", [128, 4, S_LEN], BF16)
                duT = [deps(4) for _ in range(4)]
                S.dma("sp", wqk[0:64, 0:1], P["ev_q_norm_w"][:, :], writes=[dwqk])
                S.dma("sp", wqk[64:128, 0:1], P["ev_q_norm_w"][:, :], writes=[dwqk])
                S.dma("sp", wqk[0:64, 1:2], P["ev_k_norm_w"][:, :], writes=[dwqk])
                S.dma("sp", wqk[64:128, 1:2], P["ev_k_norm_w"][:, :], writes=[dwqk])
                S.op("act", lambda e: e.mul(out=wqk[:, 0:1], in_=wqk[:, 0:1], mul=0.125), [dwqk], [dwqk])
                S.memset("dve", vtok[:], 1.0, dvtok)
                with ExitStack() as ph2:
                    sqb = [sb(ph2, "qsq%d" % i, [128, 512], BF16) for i in range(2)]
                    dsqb = [Dep(), Dep()]
                    qraw = [sb(ph2, "qraw%d" % i, [128, 512], F32) for i in range(2)]
                    dqraw = [Dep(), Dep()]
                    qrs = [sb(ph2, "qrs%d" % i, [128, 512], F32) for i in range(2)]
                    dqrs = [Dep(), Dep()]
                    wt, dwt = load_w(w_view("ev_in_w", 0, 512), 8, 512)
                    for oc in range(4):
                        for tb in range(4):
                            tsl = slice(tb * 512, (tb + 1) * 512)
                            ps, dps = PSM.next()
                            for kc in range(8):
                                S.mm(ps[:], wt[:, kc, oc * 128:(oc + 1) * 128], hT[:, kc, tsl], kc == 0, kc == 7, [dwt, dhT[kc][tb]], [dps])
                            S.copy(evq.next(), uT[:, oc, tsl], ps[:], [dps], [duT[oc][tb]])
                    stage(S, 21)
                    wt, dwt = load_w(w_view("ev_in_w", 512, 1024), 8, 512)
                    wt2, dwt2 = load_w(w_view("ev_in_w", 1024, 1280), 8, 256)
                    pend = []
                    kk = 0

                    def qk_stage2(ps, dps, ii, dst_ap, ddst, wcol):
                        if STAGE == 221:
                            return
                        pa, dpa = PSA.next()
                        S.mm(pa[:], bd64[:], sqb[ii][:], True, True, [dsqb[ii], dcst["bd64"]], [dpa])
                        S.act(qrs[ii][:], pa[:], AF.Sqrt, [dpa, depsc], [dqrs[ii]], bias=epsc[:, 0:1], scale=1.0 / 64)
                        S.recip(qrs[ii][:], qrs[ii][:], [dqrs[ii]], [dqrs[ii]])
                        S.stt("dve", dst_ap, qraw[ii][:], wqk[:, wcol:wcol + 1], qrs[ii][:], ALU.mult, ALU.mult, [dqraw[ii], dwqk, dqrs[ii]], [ddst])

                    for oc in range(5):
                        for tb in range(4):
                            tsl = slice(tb * 512, (tb + 1) * 512)
                            ps, dps = PSM.next()
                            for kc in range(8):
                                if oc < 4:
                                    S.mm(ps[:], wt[:, kc, oc * 128:(oc + 1) * 128], hT[:, kc, tsl], kc == 0, kc == 7, [dwt, dhT[kc][tb]], [dps])
                                else:
                                    S.mm(ps[:], wt2[:, kc, 0:128], hT[:, kc, tsl], kc == 0, kc == 7, [dwt2, dhT[kc][tb]], [dps])
                            ii = kk % 2
                            kk += 1
                            S.copy("act", qraw[ii][:], ps[:], [dps], [dqraw[ii]])
                            S.act(sqb[ii][:], qraw[ii][:], AF.Square, [dqraw[ii]], [dsqb[ii]])
                            if pend:
                                pend.pop(0)()
                            if oc < 4:
                                pend.append(lambda ps=ps, dps=dps, ii=ii, oc=oc, tb=tb, tsl=tsl: qk_stage2(ps, dps, ii, qT[:, oc, tsl], dqT[oc][tb], 0))
                            else:
                                pend.append(lambda ps=ps, dps=dps, ii=ii, tb=tb, tsl=tsl: qk_stage2(ps, dps, ii, kT[:, tsl], dkT[tb], 1))
                    while pend:
                        pend.pop(0)()
                    stage(S, 22)
                    S.dma("sp", kTs[64:128, :], kT[0:64, :], reads=dkT, writes=[dkTs])
                    S.dma("sp", kTs[0:64, :], kT[64:128, :], reads=dkT, writes=[dkTs])
                    for tt in range(16):
                        ps, dps = PSM.next()
                        for kc in range(8):
                            S.mm(ps[:, 0:128], hT[:, kc, tt * 128:(tt + 1) * 128], wt2[:, kc, 128:256], kc == 0, kc == 7, [dwt2, dhT[kc][tt // 4]], [dps])
                        S.copy(evq.next(), vtok[:, tt, :, 0:64], ps[:, 0:128].rearrange("p (a b) -> p a b", a=2), [dps], [dvtok[tt]])
                    S.barrier()
                    stage(S, 2)
                yT, dyT = hT, dhT
                with ExitStack() as ph2:
                    ccsc = sb(ph2, "ccsc", [128, 256], BF16)
                    dccsc = Dep()
                    S.dma("sp", ccsc[:], C["ccsc"][:, :], writes=[dccsc])
                    NGP = 4
                    ab = sb(ph2, "ab", [128, NGP, 16, 256], BF16)
                    dab = [deps(16) for _ in range(NGP)]
                    cst_t = [sb(ph2, "cs_t%d" % i, [128, 2, 512], BF16) for i in range(2)]
                    sst_t = [sb(ph2, "ss_t%d" % i, [128, 2, 512], BF16) for i in range(2)]
                    dcs_t = [Dep(), Dep()]
                    dss_t = [Dep(), Dep()]
                    csv = C["cs_mat"].rearrange("(t p) n -> p t n", p=128)
                    ssv = C["ss_mat"].rearrange("(t p) n -> p t n", p=128)
                    k = 0
                    for gp in range(4 // NGP):
                        for g2 in range(NGP):
                            g = gp * NGP + g2
                            for tt in range(16):
                                ps, dps = PSA.next()
                                S.mm(ps[:, 0:256], uT[:, g, tt * 128:(tt + 1) * 128], ccsc[:], True, True, [duT[g][tt // 4], dccsc], [dps])
                                S.copy(evq.next(), ab[:, g2, tt, :], ps[:, 0:256], [dps], [dab[g2][tt]])
                        for nb in range(4):
                            accs = [PSM.next() for _ in range(NGP)]
                            for qq in range(8):
                                i = k % 2
                                k += 1
                                S.dma("sp", cst_t[i][:], csv[:, qq * 2:(qq + 1) * 2, nb * 512:(nb + 1) * 512], writes=[dcs_t[i]])
                                S.dma("act", sst_t[i][:], ssv[:, qq * 2:(qq + 1) * 2, nb * 512:(nb + 1) * 512], writes=[dss_t[i]])
                                for g2 in range(NGP):
                                    ps, dps = accs[g2]
                                    for t4 in range(2):
                                        tt = qq * 2 + t4
                                        S.mm(ps[:], ab[:, g2, tt, 0:128], cst_t[i][:, t4, :], tt == 0, False, [dab[g2][tt], dcs_t[i]], [dps])
                                        S.mm(ps[:], ab[:, g2, tt, 128:256], sst_t[i][:, t4, :], False, tt == 15, [dab[g2][tt], dss_t[i]], [dps])
                            for g2 in range(NGP):
                                ps, dps = accs[g2]
                                S.copy(evq.next(), yT[:, gp * NGP + g2, nb * 512:(nb + 1) * 512], ps[:], [dps], [dyT[gp * NGP + g2][nb]])
                    S.barrier()
                    stage(S, 3)
                ph_u.close()
                with ExitStack() as ph2:
                    rb = sb(ph2, "rb", [32, 8], F32)
                    drb = Dep()
                    rbb = sb(ph2, "rbb", [32, 8, 128], F32)
                    drbb = Dep()
                    ohr = sb(ph2, "ohr", [32, 512], F32)
                    dohr = Dep()
                    amask = sb(ph2, "amask", [128, 384], F32)
                    damask = Dep()
                    bm = sb(ph2, "bm", [128, 8, 384], F32)
                    dbm = Dep()
                    tv = sb(ph2, "tv", [128, 512], F32)
                    dtv = Dep()
                    esb = sb(ph2, "esb", [128, 8], F32)
                    desb = Dep()
                    dts = deps(8)
                    S.dma("sp", rb[:], P["rel_bias"][:, :], writes=[drb])
                    S.dma("sp", ohr[:], C["ohr"][:, :], writes=[dohr])
                    S.dma("sp", amask[:], C["amask"][:, :], writes=[damask])
                    S.dma("sp", esb[:], bass.AP(tensor=P["ev_sink"].tensor, offset=0, ap=[[0, 128], [1, 8]]), writes=[desb])
                    S.act(esb[:], esb[:], AF.Exp, [desb], [desb])
                    for h in range(8):
                        S.copy("dve", rbb[:, h, :], rb[:, h:h + 1].to_broadcast([32, 128]), [drb], [drbb])
                    for h in range(8):
                        ps, dps = PSA.next()
                        S.mm(ps[:], rbb[:, h, :], ohr[:], True, True, [drbb, dohr], [dps])
                        S.copy("dve", tv[:], ps[:], [dps], [dtv])
                        S.dma("sp", tscr[h], tv[:], reads=[dtv], writes=[dts[h]])
                        S.dma("sp", bm[:, h, :], bass.AP(tensor=tscr.tensor, offset=h * 128 * 512 + 127, ap=[[511, 128], [1, 384]]), reads=[dts[h]], writes=[dbm])
                    for h in range(8):
                        S.tt("dve", bm[:, h, :], bm[:, h, :], amask[:], ALU.add, [dbm, damask], [dbm])
                    et = [sb(ph2, "et%d" % i, [128, 8, 384], BF16) for i in range(5)]
                    det = [Dep() for _ in range(5)]
                    ltmp = [sb(ph2, "ltmp%d" % i, [128, 384], F32) for i in range(2)]
                    dltmp = [Dep(), Dep()]
                    otok = [sb(ph2, "otok%d" % i, [128, 512], BF16) for i in range(2)]
                    dotok = [Dep(), Dep()]
                    den = sb(ph2, "den", [128, 4], F32)
                    dden = Dep()
                    kq = 0

                    def pv_block(n):
                        ot, dot_ = otok[n % 2], dotok[n % 2]
                        for hq in range(2):
                            ps, dps = PSA.next()
                            for hh in range(4):
                                h = hq * 4 + hh
                                js = [j for j in (n - 1, n, n + 1) if 0 <= j < 16]
                                for ji, j in enumerate(js):
                                    c0 = (n - j + 1) * 128
                                    S.mm(ps[:, hh * 65:(hh + 1) * 65], et[j % 5][:, h, c0:c0 + 128], vtok[:, j, hq, :], ji == 0, ji == len(js) - 1,
                                         [det[j % 5], dvtok[j]], [dps])
                            pv = ps[:, 0:260].rearrange("p (a b) -> p a b", b=65)
                            S.tt("dve", den[:], pv[:, :, 64], esb[:, hq * 4:(hq + 1) * 4], ALU.add, [dps, desb], [dden])
                            S.recip(den[:], den[:], [dden], [dden])
                            S.tt("dve", ot[:, hq * 256:(hq + 1) * 256].rearrange("p (a b) -> p a b", b=64), pv[:, :, 0:64],
                                 den[:].unsqueeze(2).to_broadcast([128, 4, 64]), ALU.mult, [dps, dden], [dot_])
                        for fc in range(4):
                            S.tr(psb_t[:, fc * 128:(fc + 1) * 128], ot[:, fc * 128:(fc + 1) * 128], identb[:], [dot_, dcst["identb"]], [dpsb])
                        S.copy("act", yT[:, 4:8, n * 128:(n + 1) * 128], psb_t[:, 0:512].rearrange("p (a b) -> p a b", b=128), [dpsb],
                               [dyT[4 + i][n // 4] for i in range(4)])

                    for j in range(16):
                        e_t, de_t = et[j % 5], det[j % 5]
                        q0 = max(0, j - 1) * 128
                        q1 = min(16, j + 2) * 128
                        c0 = q0 - (j - 1) * 128
                        ncol = q1 - q0
                        for h in range(8):
                            kvh = h // 4
                            ch = h // 2
                            hf = h % 2
                            ksrc = kT if hf == kvh else kTs
                            ps, dps = PSM.next()
                            S.mm(ps[:, 0:ncol], ksrc[hf * 64:(hf + 1) * 64, j * 128:(j + 1) * 128], qT[hf * 64:(hf + 1) * 64, ch, q0:q1], True, True,
                                 [dkT[j // 4], dkTs] + [dqT[ch][b] for b in range(q0 // 512, (q1 - 1) // 512 + 1)], [dps])
                            lt, dlt = ltmp[kq % 2], dltmp[kq % 2]
                            kq += 1
                            S.tt("dve", lt[:, 0:ncol], ps[:, 0:ncol], bm[:, h, c0:c0 + ncol], ALU.add, [dps, dbm], [dlt])
                            S.act(e_t[:, h, c0:c0 + ncol], lt[:, 0:ncol], AF.Exp, [dlt], [de_t])
                        if j >= 2:
                            pv_block(j - 2)
                    pv_block(14)
                    pv_block(15)
                    S.barrier()
                    stage(S, 4)
                for half in range(2):
                    wt, dwt = load_w(w_view("ev_out_w", half * 512, (half + 1) * 512), 8, 512)
                    for d4 in range(4):
                        dc = half * 4 + d4
                        for tb in range(4):
                            tsl = slice(tb * 512, (tb + 1) * 512)
                            ps, dps = PSM.next()
                            for kc in range(8):
                                S.mm(ps[:], wt[:, kc, d4 * 128:(d4 + 1) * 128], yT[:, kc, tsl], kc == 0, kc == 7, [dwt, dyT[kc][tb]], [dps])
                            S.stt("dve", xT[:, dc, tsl], ps[:], modcol[0][:, 16 + dc:17 + dc], xT[:, dc, tsl], ALU.mult, ALU.add,
                                  [dps, dmod[0], dxT[dc][tb]], [dxT[dc][tb]])
                S.barrier()
          S.dead = False
          dump_x(0)

        if p0 <= 1 < p1:
            with ExitStack() as ph:
                modulate(ph, 0, 1, "b")

                def upd(po, dpo, dc, tb):
                    tsl = slice(tb * 512, (tb + 1) * 512)
                    S.stt("dve", xT[:, dc, tsl], po[:], modcol[0][:, 40 + dc:41 + dc], xT[:, dc, tsl], ALU.mult, ALU.add,
                          [dpo, dmod[0], dxT[dc][tb]], [dxT[dc][tb]])

                adab = make_adabufs(ph)
                nbq = list(range(12))

                def after_group():
                    for _ in range(2):
                        if nbq:
                            ada_block(1, nbq.pop(0), adab)

                swiglu(ph, lambda a, b: w_view("ev_ffn_w1", a, b), lambda a, b: w_view("ev_ffn_w3", a, b),
                       lambda f0, n: P["ev_ffn_w2"][f0 * 128:(f0 + n) * 128, :].rearrange("(j p) d -> p j d", p=128), 2816, upd, "f", after_group=after_group)
                while nbq:
                    ada_block(1, nbq.pop(0), adab)
                ada_finish(1)
                S.barrier()
            dump_x(1)

        if p0 <= 2 < p1:
            ssd_phase_inner()
            dump_x(2)

        if p0 <= 3 < p1:
            moe_phase_inner()
            dump_x(3)

        with ExitStack() as ph:
            ot = [sb(ph, "ot%d" % i, [128, 4, 1024], F32) for i in range(2)]
            dot = [Dep(), Dep()]
            dout = Dep()
            for tb in range(4):
                t, dt_ = ot[tb % 2], dot[tb % 2]
                for a in range(4):
                    for fq in range(2):
                        ps, dps = PSM.next()
                        for f4 in range(4):
                            fc = fq * 4 + f4
                            S.tr(ps[:, f4 * 128:(f4 + 1) * 128], xT[:, fc, tb * 512 + a * 128: tb * 512 + (a + 1) * 128], identf[:],
                                 [dxT[fc][tb], dcst["identf"]], [dps])
                        S.copy(evq.next(), t[:, a, fq * 512:(fq + 1) * 512], ps[:], [dps], [dt_])
                S.dma("sp", out_d[tb * 512:(tb + 1) * 512, :].rearrange("(a p) f -> p a f", p=128), t[:], reads=[dt_], writes=[dout])
            S.barrier()
        S.emit_all(block)
    nc._used_inputs = list(P.keys())
    return nc


def ssd_phase(nc, S, sb, P, C, L):
    raise NotImplementedError


def moe_phase(nc, S, sb, P, C, L):
    raise NotImplementedError


_CACHE = {}


def prep_inputs(inputs, b):
    m = {}
    f = lambda a: np.ascontiguousarray(np.asarray(a, dtype=np.float32))
    m["x"] = f(inputs["x"][b])
    m["c"] = f(inputs["c"][b]).reshape(8, 128)
    m["rel_bias"] = f(inputs["rel_bias"])
    for k, shp in PARAM_SHAPES.items():
        if k in m:
            continue
        m[k] = f(inputs[k][0]).reshape(shp)
    return m


def kernel(**inputs):
    if "nc" not in _CACHE:
        _CACHE["nc"] = build()
        _CACHE["consts"] = host_consts()
    nc = _CACHE["nc"]
    consts = _CACHE["consts"]
    in_maps = []
    for b in range(8):
        m = prep_inputs(inputs, b)
        m.update(consts)
        in_maps.append({k: m[k] for k in nc._used_inputs})
    res = run_bass_kernel_spmd(nc, in_maps, core_ids=list(range(8)))
    return np.stack([np.asarray(r["out"], dtype=np.float32) for r in res.results], 0)
```
